# Optimizing a Trainium2 kernel written in Bass

```python
import jax
import jax.numpy as jnp
from jax import lax
import numpy as np

D_MODEL = 2048
BATCH = 2
SEQ = 4096
DEPTH = 1

ATT_HEADS = 8
ATT_KV_HEADS = 2
HEAD_DIM = 128
IDX_HEADS = 8
IDX_DIM = 64
TOPK_MAX = 256
Q_BLOCK = 128
ROPE_THETA = 10000.0
ATT_SCALE = HEAD_DIM ** -0.5
IDX_SCALE = (IDX_HEADS * IDX_DIM) ** -0.5
HG_HEADS = 8
HG_KEY_DIM = 128
HG_VAL_DIM = 128
HG_CHUNK = 64
N_GROUPS = 4
EXPERTS_PER_GROUP = 8
N_EXPERTS = N_GROUPS * EXPERTS_PER_GROUP
EXPERT_FF = D_MODEL // 4
EPS = 1e-6

IN_SPLITS = (ATT_HEADS * HEAD_DIM, ATT_KV_HEADS * HEAD_DIM, ATT_KV_HEADS * HEAD_DIM,
             IDX_HEADS * IDX_DIM, IDX_DIM, IDX_HEADS,
             HG_HEADS * HG_KEY_DIM, HG_HEADS * HG_KEY_DIM, HG_HEADS * HG_VAL_DIM, HG_HEADS * HG_VAL_DIM,
             D_MODEL, D_MODEL)
IN_WIDTH = sum(IN_SPLITS)

kernel_name = 'hybrid_dsa_hgrn2_hmoe_block'


def rmsnorm(x, g):
    xf = x.astype(jnp.float32)
    y = xf * lax.rsqrt(jnp.mean(xf * xf, axis=-1, keepdims=True) + EPS)
    return (y * g.astype(jnp.float32)).astype(x.dtype)


def ada_norm(x, g, shift, scale):
    return rmsnorm(x, g) * (1 + scale[:, None]) + shift[:, None]


def split_cols(z):
    parts = []
    off = 0
    for w in IN_SPLITS:
        parts.append(z[..., off:off + w])
        off += w
    return parts


def rope_tables(positions, dim):
    inv = jnp.power(ROPE_THETA, -jnp.arange(0, dim, 2, dtype=jnp.float32) / dim)
    ang = positions.astype(jnp.float32)[..., None] * inv
    return jnp.cos(ang), jnp.sin(ang)


def apply_rope(x, cos, sin):
    xf = x.astype(jnp.float32)
    half = x.shape[-1] // 2
    x1, x2 = xf[..., :half], xf[..., half:]
    c, s = cos[:, :, None], sin[:, :, None]
    return jnp.concatenate([x1 * c - x2 * s, x2 * c + x1 * s], axis=-1).astype(x.dtype)


def dsa_attention(q, k, v, q_idx, k_idx, w_idx):
    b, s = q.shape[0], q.shape[1]
    n_blk = s // Q_BLOCK
    topk = min(TOPK_MAX, s // 4)
    k_idx32 = k_idx.astype(jnp.float32)
    key_pos = jnp.arange(s)

    def to_blocks(a):
        return jnp.moveaxis(a.reshape((b, n_blk, Q_BLOCK) + a.shape[2:]), 1, 0)

    def block(args):
        qb, qib, wib, start = args
        t = start + jnp.arange(Q_BLOCK)
        causal = key_pos[None, :] <= t[:, None]
        dots = jnp.einsum('bqhd,bsd->bqhs', qib.astype(jnp.float32), k_idx32)
        score = jnp.einsum('bqh,bqhs->bqs', wib.astype(jnp.float32) * IDX_SCALE, jax.nn.relu(dots))
        score = jnp.where(causal[None], score, -jnp.inf)
        _, sel = lax.top_k(score, topk)
        valid = sel <= t[None, :, None]
        k_sel = jax.vmap(lambda kb, ib: kb[ib])(k, sel)
        v_sel = jax.vmap(lambda vb, ib: vb[ib])(v, sel)
        logits = jnp.einsum('bqhgd,bqkhd->bhgqk', qb.astype(jnp.float32), k_sel.astype(jnp.float32)) * ATT_SCALE
        logits = jnp.where(valid[:, None, None], logits, -jnp.inf)
        p = jax.nn.softmax(logits, axis=-1)
        return jnp.einsum('bhgqk,bqkhd->bqhgd', p.astype(v.dtype), v_sel)

    starts = jnp.arange(n_blk) * Q_BLOCK
    out = lax.map(block, (to_blocks(q), to_blocks(q_idx), to_blocks(w_idx), starts))
    return jnp.moveaxis(out, 0, 1).reshape(b, s, -1)


def hgrn2_recurrence(q, f_logit, i, lb):
    b, s, h, dk = q.shape
    dv = i.shape[-1]
    n_c = s // HG_CHUNK
    f32 = jnp.float32
    lb = lb.astype(f32)
    q = jax.nn.silu(q.astype(f32))
    f = lb + (1.0 - lb) * jax.nn.sigmoid(f_logit.astype(f32))
    log_f = jnp.log(f)
    k = 1.0 - f

    def chunks(a):
        return a.reshape((b, n_c, HG_CHUNK) + a.shape[2:])

    q, log_f, k, v = chunks(q), chunks(log_f), chunks(k), chunks(i.astype(f32))
    cum = jnp.cumsum(log_f, axis=2)
    cum_last = cum[:, :, -1]
    q_dec = q * jnp.exp(cum)
    k_dec = k * jnp.exp(-cum)
    att = jnp.einsum('bnthk,bnshk->bnhts', q_dec, k_dec)
    tril = jnp.tril(jnp.ones((HG_CHUNK, HG_CHUNK), dtype=bool))
    att = jnp.where(tril, att, 0.0)
    intra = jnp.einsum('bnhts,bnshv->bnthv', att, v)
    k_to_end = k * jnp.exp(cum_last[:, :, None] - cum)
    upd = jnp.einsum('bnshk,bnshv->bnhkv', k_to_end, v)

    def step(state, inp):
        decay, u = inp
        return decay[..., None] * state + u, state

    init = jnp.zeros((b, h, dk, dv), f32)
    _, s_prev = lax.scan(step, init, (jnp.moveaxis(jnp.exp(cum_last), 1, 0), jnp.moveaxis(upd, 1, 0)))
    s_prev = jnp.moveaxis(s_prev, 0, 1)
    inter = jnp.einsum('bnthk,bnhkv->bnthv', q_dec, s_prev)
    return (intra + inter).reshape(b, s, h, dv)


def hier_moe(h, w_rg, b_rg, w_re, b_re, w_gate, w_up, w_down):
    b, s, _ = h.shape
    g_logit = (h @ w_rg).astype(jnp.float32) + b_rg.astype(jnp.float32)
    p_grp, grp = lax.top_k(jax.nn.softmax(g_logit, axis=-1), 1)
    e_logit = ((h @ w_re).astype(jnp.float32) + b_re.astype(jnp.float32)).reshape(b, s, N_GROUPS, EXPERTS_PER_GROUP)
    e_in_grp = jnp.take_along_axis(e_logit, grp[..., None], axis=2)[:, :, 0]
    p_e, e_idx = lax.top_k(jax.nn.softmax(e_in_grp, axis=-1), 2)
    p_e = p_e / jnp.sum(p_e, axis=-1, keepdims=True)
    w_tok = p_grp * p_e
    comb = jnp.sum(jax.nn.one_hot(e_idx, EXPERTS_PER_GROUP, dtype=jnp.float32) * w_tok[..., None], axis=2)
    y = jnp.zeros_like(h)
    for gi in range(N_GROUPS):
        cg = jnp.where(grp == gi, comb, 0.0).astype(h.dtype)
        lo, hi = gi * EXPERTS_PER_GROUP, (gi + 1) * EXPERTS_PER_GROUP
        a = jnp.einsum('bsd,edf->bsef', h, w_gate[lo:hi])
        u = jnp.einsum('bsd,edf->bsef', h, w_up[lo:hi])
        y = y + jnp.einsum('bsef,efd->bsd', jax.nn.silu(a) * u * cg[..., None], w_down[lo:hi])
    return y


def setup_inputs(seed: int = 0) -> dict:
    key = jax.random.key(seed)
    ks = jax.random.split(key, 24)
    f32 = jnp.float32

    def nrm(k, shape, scale):
        return jax.random.normal(k, shape, f32) * scale

    d = D_MODEL
    hgw = HG_HEADS * HG_KEY_DIM
    return {
        'x': nrm(ks[0], (BATCH, SEQ, d), 1.0),
        'c': nrm(ks[1], (BATCH, d), 1.0),
        'positions': (jnp.arange(SEQ, dtype=jnp.int32)[None, :]
                      + jax.random.randint(ks[2], (BATCH, 1), 0, 1024, dtype=jnp.int32)),
        'w_ada': nrm(ks[3], (DEPTH, d, 6 * d), 0.5 * d ** -0.5),
        'b_ada': nrm(ks[4], (DEPTH, 6 * d), 0.02),
        'g_norm1': 1.0 + nrm(ks[5], (DEPTH, d), 0.02),
        'w_in': nrm(ks[6], (DEPTH, d, IN_WIDTH), d ** -0.5),
        'g_head': 1.0 + nrm(ks[7], (DEPTH, HG_HEADS, HG_VAL_DIM), 0.02),
        'hg_lower_bounds': 1.0 + nrm(ks[8], (DEPTH + 1, hgw), 0.1),
        'w_attn_up': nrm(ks[9], (DEPTH, ATT_HEADS * HEAD_DIM, d), (ATT_HEADS * HEAD_DIM) ** -0.5),
        'w_hgrn_up': nrm(ks[10], (DEPTH, HG_HEADS * HG_VAL_DIM, d), (HG_HEADS * HG_VAL_DIM) ** -0.5),
        'w_out': nrm(ks[11], (DEPTH, d, d), d ** -0.5),
        'g_norm2': 1.0 + nrm(ks[12], (DEPTH, d), 0.02),
        'w_router_group': nrm(ks[13], (DEPTH, d, N_GROUPS), d ** -0.5),
        'b_router_group': nrm(ks[14], (DEPTH, N_GROUPS), 0.01),
        'w_router_expert': nrm(ks[15], (DEPTH, d, N_EXPERTS), d ** -0.5),
        'b_router_expert': nrm(ks[16], (DEPTH, N_EXPERTS), 0.01),
        'w_exp_gate': nrm(ks[17], (DEPTH, N_EXPERTS, d, EXPERT_FF), d ** -0.5),
        'w_exp_up': nrm(ks[18], (DEPTH, N_EXPERTS, d, EXPERT_FF), d ** -0.5),
        'w_exp_down': nrm(ks[19], (DEPTH, N_EXPERTS, EXPERT_FF, d), EXPERT_FF ** -0.5),
        'g_final': 1.0 + nrm(ks[20], (d,), 0.02),
    }


def reference(x, c, positions, w_ada, b_ada, g_norm1, w_in, g_head, hg_lower_bounds, w_attn_up, w_hgrn_up,
              w_out, g_norm2, w_router_group, b_router_group, w_router_expert, b_router_expert,
              w_exp_gate, w_exp_up, w_exp_down, g_final):
    b, s, _ = x.shape
    cos_a, sin_a = rope_tables(positions, HEAD_DIM)
    cos_i, sin_i = rope_tables(positions, IDX_DIM)
    lb_all = jnp.cumsum(jax.nn.softmax(hg_lower_bounds.astype(jnp.float32), axis=0), axis=0)
    c_act = jax.nn.silu(c)
    for l in range(DEPTH):
        mod = c_act @ w_ada[l] + b_ada[l]
        sh1, sc1, gt1, sh2, sc2, gt2 = jnp.split(mod, 6, axis=-1)
        h = ada_norm(x, g_norm1[l], sh1, sc1)
        aq, ak, av, iq, ik, iw, hq, hf, hi, hog, ga, gh = split_cols(h @ w_in[l])
        aq = apply_rope(aq.reshape(b, s, ATT_HEADS, HEAD_DIM), cos_a, sin_a)
        aq = aq.reshape(b, s, ATT_KV_HEADS, ATT_HEADS // ATT_KV_HEADS, HEAD_DIM)
        ak = apply_rope(ak.reshape(b, s, ATT_KV_HEADS, HEAD_DIM), cos_a, sin_a)
        av = av.reshape(b, s, ATT_KV_HEADS, HEAD_DIM)
        iq = apply_rope(iq.reshape(b, s, IDX_HEADS, IDX_DIM), cos_i, sin_i)
        ik = apply_rope(ik[:, :, None], cos_i, sin_i)[:, :, 0]
        y_att = dsa_attention(aq, ak, av, iq, ik, iw)
        o_h = hgrn2_recurrence(hq.reshape(b, s, HG_HEADS, HG_KEY_DIM),
                               hf.reshape(b, s, HG_HEADS, HG_KEY_DIM),
                               hi.reshape(b, s, HG_HEADS, HG_VAL_DIM),
                               lb_all[l].reshape(HG_HEADS, HG_KEY_DIM))
        y_hg = rmsnorm(o_h, g_head[l]).astype(x.dtype).reshape(b, s, -1) * jax.nn.silu(hog)
        merged = (jax.nn.sigmoid(ga) * (y_att @ w_attn_up[l])
                  + jax.nn.sigmoid(gh) * (y_hg @ w_hgrn_up[l]))
        x = x + gt1[:, None] * (merged @ w_out[l])
        h2 = ada_norm(x, g_norm2[l], sh2, sc2)
        x = x + gt2[:, None] * hier_moe(h2, w_router_group[l], b_router_group[l], w_router_expert[l],
                                        b_router_expert[l], w_exp_gate[l], w_exp_up[l], w_exp_down[l])
    return rmsnorm(x, g_final)
```

```python
import contextlib
import math
import numpy as np
import concourse.bass as bass
import concourse.mybir as mybir
from concourse.bass_utils import run_bass_kernel_spmd

F32 = mybir.dt.float32
BF16 = mybir.dt.bfloat16
I32 = mybir.dt.int32
AF = mybir.ActivationFunctionType
ALU = mybir.AluOpType
AX = mybir.AxisListType

D = 2048
NU = 32
NQ = 8
NOWN = 8
EPS = 1e-6
ATT_SCALE = 128 ** -0.5
IDX_SCALE = 512 ** -0.5
NEG = -1.0e30
NBIS = 14
COMB_OFF = 228288
TWO_PI = 2.0 * math.pi

O_AQ, O_AK, O_AV, O_IQ, O_IK, O_IW = 0, 1024, 1280, 1536, 2048, 2112
O_HQ, O_HF, O_HI, O_HOG, O_GA, O_GH = 2120, 3144, 4168, 5192, 6216, 8264


class Buf:
    __slots__ = ("name", "lw", "rd")

    def __init__(self, name=""):
        self.name = name
        self.lw = None
        self.rd = {}


def bufs(n, name=""):
    return [Buf(f"{name}{i}") for i in range(n)]


class Prog:
    STREAMS = ["pe", "act", "dve", "pool", "sp"]
    NDMASEM = 8

    def __init__(self, nc):
        self.nc = nc
        self.ops = []
        self.last = {}
        self.dma_open = []
        self.fence = {}

    def op(self, eng, fn, reads=(), writes=(), dma=False):
        idx = len(self.ops)
        deps = set()
        for b in reads:
            if b.lw is not None:
                self._dep(deps, idx, eng, dma, b.lw, "raw")
        for b in writes:
            if b.lw is not None:
                self._dep(deps, idx, eng, dma, b.lw, "waw")
            for r in b.rd.values():
                self._dep(deps, idx, eng, dma, r, "war")
        if eng in self.fence:
            deps |= self.fence.pop(eng)
        for b in reads:
            b.rd[("dma", idx) if dma else eng] = idx
        for b in writes:
            b.lw = idx
            b.rd = {}
        self.ops.append(dict(eng=eng, fn=fn, deps=deps, dma=dma, sig=False))
        if dma:
            self.dma_open.append(idx)
        else:
            self.last[eng] = idx
        return idx

    def _dep(self, deps, idx, eng, dma, pidx, kind):
        if pidx == idx:
            return
        p = self.ops[pidx]
        if (not dma) and (not p["dma"]) and p["eng"] == eng:
            if eng == "pe":
                return
        deps.add(pidx)

    def barrier(self):
        import os
        if os.environ.get("MK_NOBAR", "0") == "1":
            return
        f = set(self.last.values()) | set(self.dma_open)
        self.dma_open = []
        for s in self.STREAMS:
            self.fence[s] = set(f) | self.fence.get(s, set())

    def pe(self, fn, reads=(), writes=()):
        return self.op("pe", fn, reads, writes)

    def act(self, fn, reads=(), writes=()):
        return self.op("act", fn, reads, writes)

    def dve(self, fn, reads=(), writes=()):
        return self.op("dve", fn, reads, writes)

    def pool(self, fn, reads=(), writes=()):
        return self.op("pool", fn, reads, writes)

    def dma(self, eng, out, in_, reads=(), writes=(), **kw):
        return self.op(eng, lambda e: e.dma_start(out=out, in_=in_, **kw), reads, writes, dma=True)

    def emit(self, final_wait_ops=()):
        nc = self.nc
        ops = self.ops
        for o in ops:
            for d in o["deps"]:
                ops[d]["sig"] = True
        for i in final_wait_ops:
            ops[i]["sig"] = True
        ordn = {s: 0 for s in self.STREAMS}
        dcount = {s: 0 for s in self.STREAMS}
        for o in ops:
            s = o["eng"]
            if o["dma"]:
                k = dcount[s]
                dcount[s] += 1
                o["slot"] = k % self.NDMASEM
                o["use"] = k // self.NDMASEM + 1
            elif o["sig"]:
                ordn[s] += 1
                o["ord"] = ordn[s]
        with contextlib.ExitStack() as st:
            esem = {s: st.enter_context(nc.semaphore(f"e_{s}")) for s in self.STREAMS}
            dsem = {s: [st.enter_context(nc.semaphore(f"d_{s}{k}")) for k in range(self.NDMASEM)]
                    for s in self.STREAMS if dcount[s] > 0}
            block = st.enter_context(nc.Block())

            def target(pidx):
                p = ops[pidx]
                if p["dma"]:
                    return (dsem[p["eng"]][p["slot"]], 16 * p["use"], ("d", p["eng"], p["slot"]))
                return (esem[p["eng"]], p["ord"], ("e", p["eng"]))

            def run_stream(s, e):
                waited = {}
                for idx, o in enumerate(ops):
                    if o["eng"] != s:
                        continue
                    tg = {}
                    for d in o["deps"]:
                        sem, val, key = target(d)
                        if key not in tg or tg[key][1] < val:
                            tg[key] = (sem, val)
                    if o["dma"] and o["use"] > 1:
                        key = ("d", s, o["slot"])
                        val = 16 * (o["use"] - 1)
                        if key not in tg or tg[key][1] < val:
                            tg[key] = (dsem[s][o["slot"]], val)
                    for key, (sem, val) in tg.items():
                        if waited.get(key, 0) >= val:
                            continue
                        e.wait_ge(sem, val)
                        waited[key] = val
                    ins = o["fn"](e)
                    if o["dma"]:
                        ins.then_inc(dsem[s][o["slot"]], 16)
                    elif o["sig"]:
                        ins.then_inc(esem[s], 1)
                if s == "sp":
                    for i in final_wait_ops:
                        sem, val, key = target(i)
                        e.wait_ge(sem, val)

            @block.tensor
            def _(e):
                run_stream("pe", e)

            @block.scalar
            def _(e):
                run_stream("act", e)

            @block.vector
            def _(e):
                run_stream("dve", e)

            @block.gpsimd
            def _(e):
                run_stream("pool", e)

            @block.sync
            def _(e):
                run_stream("sp", e)


class Arena:
    BASE = 16640
    LIMIT = 228288

    def __init__(self, nc):
        self.nc = nc
        self.off = self.BASE
        self.n = 0

    def alloc(self, shape, dt, name="t"):
        esz = 4 if dt in (F32, I32) else 2
        size = esz * int(np.prod(shape[1:]))
        size = (size + 63) // 64 * 64
        t = self.nc.alloc_sbuf_tensor_at(f"a{self.n}_{name}", list(shape), dt, offset=self.off)
        self.n += 1
        self.off += size
        assert self.off <= self.LIMIT, f"SBUF arena overflow at {name}: {self.off}"
        return t

    def mark(self):
        return self.off

    def release(self, m):
        self.off = m


def build(upto=99, dbg=False):
    nc = bass.Bass("TRN2", target_bir_lowering=False)

    def din(name, shape, dt=F32):
        return nc.dram_tensor(name, list(shape), dt, kind="ExternalInput").ap()

    def dscr(name, shape, dt=F32):
        return nc.dram_tensor(name, list(shape), dt).ap()

    xa = din("xa", [NU * 128, D])
    xo = din("xo", [1024, D])
    posr = din("posr", [128, NU * 128], I32)
    poso = din("poso", [128, 1024], I32)
    vmask_d = din("vmask", [128, NU])
    smask_d = din("smask", [128, 512])
    cst_d = din("cst", [128, 640])
    ccol_d = din("ccol", [128, 16])
    lbc_d = din("lbc", [128, 16])
    w_ada = din("w_ada", [D, 6 * D])
    b_ada = din("b_ada", [1, 6 * D])
    grows = din("grows", [4, D])
    w_in = din("w_in", [D, 10312])
    w1a = din("w1a", [D, 768])
    w2s = din("w2s", [D, 1536])
    if upto >= 5:
        w_au = din("w_au", [1024, D])
        w_hu = din("w_hu", [1024, D])
        w_out = din("w_out", [D, D])
        w_r = din("w_r", [D, 36])
        b_r = din("b_r", [1, 36])
    if upto >= 6:
        w_eg = din("w_eg", [32, D, 512])
        w_eu = din("w_eu", [32, D, 512])
        w_ed = din("w_ed", [32, 512, D])
    out_d = nc.dram_tensor("out", [1024, D], F32, kind="ExternalOutput").ap()

    hTs = dscr("hTs", [NU, 128, D], BF16)
    modscr = dscr("modscr", [4, 128, D])
    x1s = dscr("x1s", [NOWN, 128, D])

    dbg_out = {}

    def dbg_tensor(name, shape, dt=F32):
        t = nc.dram_tensor("dbg_" + name, list(shape), dt, kind="ExternalOutput").ap()
        dbg_out[name] = t
        return t

    P = Prog(nc)
    A = Arena(nc)
    final_ops = []

    PS = [nc.alloc_psum_tensor(f"ps{k}", [128, 512], F32) for k in range(8)]
    PSB = bufs(8, "ps")

    def psf(k):
        return PS[k][:]

    def psb(k):
        return PS[k][:].bitcast(BF16)

    cst = A.alloc([128, 640], F32, "cst")
    b_cst = Buf("cst")
    P.dma("sp", cst[:], cst_d, writes=[b_cst])
    ident_f = cst[:, 0:128]
    cmask = cst[:, 128:256]
    tmask = cst[:, 256:384]
    inv_a = cst[:, 384:385]
    inv_i = cst[:, 385:386]
    sgn_a = cst[:, 386:387]
    sgn_i = cst[:, 387:388]
    ones_row = cst[0:1, 388:516]
    bisc = cst[:, 516:516 + NBIS]
    rmask = cst[:, 540:604]
    ident_b = A.alloc([128, 128], BF16, "identb")
    b_idb = Buf("identb")
    P.dve(lambda e: e.tensor_copy(out=ident_b[:], in_=ident_f), reads=[b_cst], writes=[b_idb])
    vmask = A.alloc([128, NU], F32, "vmask")
    b_vmask = Buf("vmask")
    P.dma("sp", vmask[:], vmask_d, writes=[b_vmask])

    def bcast_rows(dst_ap, b_dst, row_dram_ap, n, tmp_row, b_tmp, bank):
        for c0 in range(0, n, 512):
            w = min(512, n - c0)
            P.dma("sp", tmp_row[0:1, 0:w], row_dram_ap[:, c0:c0 + w], writes=[b_tmp])
            P.pe(lambda e, w=w: e.matmul(psf(bank)[:, 0:w], lhsT=ones_row, rhs=tmp_row[0:1, 0:w], start=True, stop=True),
                 reads=[b_cst, b_tmp], writes=[PSB[bank]])
            P.act(lambda e, c0=c0, w=w: e.activation(out=dst_ap[:, c0:c0 + w], in_=psf(bank)[:, 0:w], func=AF.Copy),
                  writes=[PSB[bank], b_dst])

    KT = A.alloc([128, 2, NU * 128], BF16, "KT")
    ikT = A.alloc([128, NU * 128], BF16, "ikT")
    Vx = A.alloc([128, NU, 2, 132], BF16, "Vx")
    b_KT, b_ikT, b_Vx = bufs(NQ, "KT"), bufs(NQ, "ikT"), bufs(NU, "Vx")
    m_p1 = A.mark()
    A1 = A.alloc([128, D], F32, "A1")
    B1 = A.alloc([128, D], F32, "B1")
    b_A1, b_B1 = Buf("A1"), Buf("B1")
    ccol = A.alloc([128, 16], F32, "ccol")
    csil = A.alloc([128, 16], F32, "csil")
    crep = A.alloc([128, 16, 128], BF16, "crep")
    b_ccol, b_csil, b_crep = Buf(), Buf(), Buf()
    P.dma("sp", ccol[:], ccol_d, writes=[b_ccol])
    P.act(lambda e: e.activation(out=csil[:], in_=ccol[:], func=AF.Silu), reads=[b_ccol], writes=[b_csil])
    for ck in range(16):
        P.dve(lambda e, ck=ck: e.tensor_copy(out=crep[:, ck, :], in_=csil[:, ck:ck + 1].to_broadcast([128, 128])),
              reads=[b_csil], writes=[b_crep])
    wada_t = [A.alloc([128, 16, 256], BF16, f"wada{i}") for i in range(2)]
    b_wada = bufs(2, "wada")
    brow = [A.alloc([1, 512], F32, f"brow{i}") for i in range(2)]
    b_brow = bufs(2, "brow")
    stage = [A.alloc([128, 512], F32, "stage0")] * 2
    b_stage = [Buf("stage")] * 2
    nst = [0]

    MW = 256
    NB8 = D // MW
    gsm = A.alloc([128, MW], F32, "gsm")
    b_gsm = Buf()
    mod_dma_done = set()

    def mod_dma(blk):
        if blk in mod_dma_done or blk >= 6 * NB8:
            return
        mod_dma_done.add(blk)
        s = blk % 2
        P.dma("pool", wada_t[s][:], w_ada[:, blk * MW:(blk + 1) * MW].rearrange("(c p) f -> p c f", p=128),
              writes=[b_wada[s]])

    def mod_block(blk, consume, grow=None):
        s = blk % 2
        mod_dma(blk)
        P.dma("sp", brow[s][0:1, 0:MW], b_ada[:, blk * MW:(blk + 1) * MW], writes=[b_brow[s]])
        bank = 4 + blk % 2
        for ck in range(16):
            P.pe(lambda e, ck=ck, s=s, bank=bank: e.matmul(psf(bank)[:, 0:MW], lhsT=crep[:, ck, :], rhs=wada_t[s][:, ck, :],
                                                            start=(ck == 0), stop=False),
                 reads=[b_crep, b_wada[s]], writes=[PSB[bank]])
        P.pe(lambda e, s=s, bank=bank: e.matmul(psf(bank)[:, 0:MW], lhsT=ones_row, rhs=brow[s][0:1, 0:MW], start=False, stop=True),
             reads=[b_cst, b_brow[s]], writes=[PSB[bank]])
        mod_dma(blk + 1)
        if grow is not None:
            c0 = (blk % NB8) * MW
            P.dma("sp", brow[s][0:1, MW:2 * MW], grows[grow:grow + 1, c0:c0 + MW], writes=[b_brow[s]])
            P.pe(lambda e, s=s: e.matmul(psf(3)[:, 0:MW], lhsT=ones_row, rhs=brow[s][0:1, MW:2 * MW], start=True, stop=True),
                 reads=[b_cst, b_brow[s]], writes=[PSB[3]])
            P.act(lambda e: e.activation(out=gsm[:], in_=psf(3)[:, 0:MW], func=AF.Copy), writes=[PSB[3], b_gsm])
        consume(bank)

    def to_scr(slot, c0, bank, with_g):
        k = nst[0] % 2
        nst[0] += 1
        if with_g:
            P.dve(lambda e: e.scalar_tensor_tensor(out=stage[k][:, 0:MW], in0=psf(bank)[:, 0:MW], scalar=1.0, in1=gsm[:],
                                                   op0=ALU.add, op1=ALU.mult),
                  reads=[b_gsm], writes=[PSB[bank], b_stage[k]])
        else:
            P.act(lambda e: e.activation(out=stage[k][:, 0:MW], in_=psf(bank)[:, 0:MW], func=AF.Copy), writes=[PSB[bank], b_stage[k]])
        P.dma("act", modscr[slot, :, c0:c0 + MW], stage[k][:, 0:MW], reads=[b_stage[k]])

    for q in range(NB8):
        mod_block(q, lambda bank, q=q: P.act(
            lambda e: e.activation(out=B1[:, q * MW:(q + 1) * MW], in_=psf(bank)[:, 0:MW], func=AF.Copy),
            writes=[PSB[bank], b_B1]))
    for q in range(NB8):
        mod_block(NB8 + q, lambda bank, q=q: P.dve(
            lambda e: e.scalar_tensor_tensor(out=A1[:, q * MW:(q + 1) * MW], in0=psf(bank)[:, 0:MW], scalar=1.0,
                                             in1=gsm[:], op0=ALU.add, op1=ALU.mult),
            reads=[b_gsm], writes=[PSB[bank], b_A1]), grow=0)
    pending_mod = []
    for q in range(NB8):
        pending_mod.append(lambda q=q: mod_block(2 * NB8 + q, lambda bank, q=q: to_scr(0, q * MW, bank, False)))
    for q in range(NB8):
        pending_mod.append(lambda q=q: mod_block(3 * NB8 + q, lambda bank, q=q: to_scr(2, q * MW, bank, False)))
    for q in range(NB8):
        pending_mod.append(lambda q=q: mod_block(4 * NB8 + q, lambda bank, q=q: to_scr(1, q * MW, bank, True), grow=1))
    for q in range(NB8):
        pending_mod.append(lambda q=q: mod_block(5 * NB8 + q, lambda bank, q=q: to_scr(3, q * MW, bank, False)))

    xt = [A.alloc([128, D], F32, f"xt{i}") for i in range(2)]
    b_xt = bufs(2, "xt")
    hb = [A.alloc([128, D], BF16, f"hb{i}") for i in range(4)]
    b_hb = bufs(4, "hb")
    NS0 = [dict(junk=hb[i], b_junk=b_hb[i], xn=None, b_xn=None,
                st1=A.alloc([128, 8], F32, f"st1_{i}"), b_ssq=Buf(), b_rstd=Buf()) for i in range(4)]

    def norm_tile(NS, x_ap, bx, Abc, bA, Bbc, bB, hb_ap, b_hbk):
        junk, b_junk, xn, b_xn, st1, b_ssq, b_rstd = (NS["junk"], NS["b_junk"], NS["xn"], NS["b_xn"], NS["st1"],
                                                      NS["b_ssq"], NS["b_rstd"])
        P.dve(lambda e: e.scalar_tensor_tensor(out=junk[:], in0=x_ap, scalar=1.0, in1=x_ap, op0=ALU.mult, op1=ALU.mult,
                                               accum_out=st1[:, 0:1]),
              reads=[bx], writes=[b_junk, b_ssq])
        P.act(lambda e: e.activation(out=st1[:, 1:2], in_=st1[:, 0:1], func=AF.Sqrt, scale=1.0 / D, bias=EPS),
              reads=[b_ssq], writes=[b_rstd])
        P.dve(lambda e: e.reciprocal(out=st1[:, 2:3], in_=st1[:, 1:2]), reads=[b_rstd], writes=[b_rstd])
        if xn is None:
            P.dve(lambda e: e.scalar_tensor_tensor(out=x_ap, in0=x_ap, scalar=st1[:, 2:3], in1=Abc, op0=ALU.mult, op1=ALU.mult),
                  reads=[bx, b_rstd, bA], writes=[bx])
            P.dve(lambda e: e.tensor_tensor(out=hb_ap, in0=x_ap, in1=Bbc, op=ALU.add), reads=[bx, bB], writes=[b_hbk])
        else:
            P.dve(lambda e: e.scalar_tensor_tensor(out=xn[:], in0=x_ap, scalar=st1[:, 2:3], in1=Abc, op0=ALU.mult, op1=ALU.mult),
                  reads=[bx, b_rstd, bA], writes=[b_xn])
            P.pool(lambda e: e.tensor_tensor(out=hb_ap, in0=xn[:], in1=Bbc, op=ALU.add), reads=[b_xn, bB], writes=[b_hbk])

    def transpose_tile(hb_ap, b_hbk, dst_ap, b_dst, banks):
        for half in range(2):
            bank = banks[half]
            for k in range(8):
                ck = half * 8 + k
                P.pe(lambda e, k=k, ck=ck, bank=bank: e.transpose(out=psb(bank)[:, k * 128:(k + 1) * 128],
                                                                   in_=hb_ap[:, ck * 128:(ck + 1) * 128], identity=ident_b[:]),
                     reads=[b_hbk, b_idb], writes=[PSB[bank]])
            P.act(lambda e, half=half, bank=bank: e.activation(out=dst_ap[:, half * 1024:(half + 1) * 1024], in_=psb(bank),
                                                                func=AF.Copy),
                  writes=[PSB[bank], b_dst])


    def rope_scratch():
        t0 = A.alloc([128, 512], F32, "rp_t0")
        t2 = A.alloc([128, 512], F32, "rp_t2")
        return dict(pos_i=t0[:].bitcast(I32), ang=A.alloc([128, 512], F32, "rp_ang")[:],
                    ki=t2[:].bitcast(I32), kf=t0[:],
                    r=A.alloc([128, 512], F32, "rp_r")[:], m=t2[:],
                    rc=A.alloc([128, 512], F32, "rp_rc")[:], b=Buf("rp"))

    def rope_tables(RS, pos_dram_ap, inv_col, sgn_col, cos_ap, sin_ap, b_tab):
        rp_pos_i, rp_ang, rp_ki, rp_kf, rp_r, rp_m, rp_rc, b_rp = (RS["pos_i"], RS["ang"], RS["ki"], RS["kf"], RS["r"],
                                                                    RS["m"], RS["rc"], RS["b"])
        P.dma("sp", rp_pos_i, pos_dram_ap, writes=[b_rp])
        P.dve(lambda e: e.tensor_copy(out=rp_ang, in_=rp_pos_i), reads=[b_rp], writes=[b_rp])
        P.dve(lambda e: e.tensor_scalar(out=rp_ang, in0=rp_ang, scalar1=inv_col, scalar2=None, op0=ALU.mult),
              reads=[b_rp, b_cst], writes=[b_rp])
        P.dve(lambda e: e.tensor_scalar(out=rp_ki, in0=rp_ang, scalar1=1.0 / TWO_PI, scalar2=None, op0=ALU.mult),
              reads=[b_rp], writes=[b_rp])
        P.dve(lambda e: e.tensor_copy(out=rp_kf, in_=rp_ki), reads=[b_rp], writes=[b_rp])
        P.dve(lambda e: e.scalar_tensor_tensor(out=rp_r, in0=rp_kf, scalar=-TWO_PI, in1=rp_ang, op0=ALU.mult, op1=ALU.add),
              reads=[b_rp], writes=[b_rp])
        P.dve(lambda e: e.tensor_scalar(out=rp_m, in0=rp_r, scalar1=math.pi, scalar2=-TWO_PI, op0=ALU.is_gt, op1=ALU.mult),
              reads=[b_rp], writes=[b_rp])
        P.dve(lambda e: e.tensor_tensor(out=rp_r, in0=rp_r, in1=rp_m, op=ALU.add), reads=[b_rp], writes=[b_rp])
        P.dve(lambda e: e.tensor_scalar(out=rp_m, in0=rp_r, scalar1=-math.pi, scalar2=TWO_PI, op0=ALU.is_lt, op1=ALU.mult),
              reads=[b_rp], writes=[b_rp])
        P.dve(lambda e: e.tensor_tensor(out=rp_r, in0=rp_r, in1=rp_m, op=ALU.add), reads=[b_rp], writes=[b_rp])
        P.dve(lambda e: e.tensor_scalar(out=rp_m, in0=rp_r, scalar1=math.pi / 2, scalar2=-TWO_PI, op0=ALU.is_gt, op1=ALU.mult),
              reads=[b_rp], writes=[b_rp])
        P.dve(lambda e: e.scalar_tensor_tensor(out=rp_rc, in0=rp_r, scalar=math.pi / 2, in1=rp_m, op0=ALU.add, op1=ALU.add),
              reads=[b_rp], writes=[b_rp])
        P.act(lambda e: e.activation(out=sin_ap, in_=rp_r, func=AF.Sin), reads=[b_rp], writes=[b_tab])
        P.act(lambda e: e.activation(out=cos_ap, in_=rp_rc, func=AF.Sin), reads=[b_rp], writes=[b_tab])
        P.dve(lambda e: e.tensor_scalar(out=sin_ap, in0=sin_ap, scalar1=sgn_col, scalar2=None, op0=ALU.mult),
              reads=[b_tab, b_cst], writes=[b_tab])

    def rope_evac(dst_ap, b_dst, bank_a, bank_s, cos_ap, sin_ap, b_tab, t1, t2, b_t):
        P.dve(lambda e: e.tensor_tensor(out=t1, in0=psf(bank_a), in1=cos_ap, op=ALU.mult), reads=[b_tab], writes=[PSB[bank_a], b_t[0]])
        P.dve(lambda e: e.tensor_tensor(out=t2, in0=psf(bank_s), in1=sin_ap, op=ALU.mult), reads=[b_tab], writes=[PSB[bank_s], b_t[1]])
        P.pool(lambda e: e.tensor_tensor(out=dst_ap, in0=t1, in1=t2, op=ALU.add), reads=[b_t[0], b_t[1]], writes=[b_dst])

    if upto >= 2:
        RS1 = rope_scratch()
        w1a_t = A.alloc([128, 16, 768], BF16, "w1a")
        wv_t = A.alloc([128, 16, 256], BF16, "wv")
        b_w1a, b_wv = Buf(), Buf()
        P.dma("pool", w1a_t[:], w1a.rearrange("(c p) f -> p c f", p=128), writes=[b_w1a])
        P.dma("pool", wv_t[:], w_in[:, O_AV:O_AV + 256].rearrange("(c p) f -> p c f", p=128), writes=[b_wv])
        hTq = [A.alloc([128, 4, 16, 128], BF16, f"hTq{i}") for i in range(2)]
        b_hTq4 = [bufs(4, f"hTq{i}_") for i in range(2)]
        tabs = [A.alloc([128, 512], F32, f"tab{i}") for i in range(4)]
        b_tabs = [Buf("tab_a"), Buf("tab_i")]
        rt = [A.alloc([128, 512], F32, f"rt{i}") for i in range(2)] * 2
        b_rt = bufs(2) * 2
        def norm_quad(qq):
            for r in range(4):
                u = 4 * qq + r
                k_ = u % 2
                P.dma("sp", xt[k_][:], xa[u * 128:(u + 1) * 128, :], writes=[b_xt[k_]])
                norm_tile(NS0[r], xt[k_][:], b_xt[k_], A1[:], b_A1, B1[:], b_B1, hb[r][:], b_hb[r])

        def trans_quad(qq):
            for r in range(4):
                u = 4 * qq + r
                transpose_tile(hb[r][:], b_hb[r], hTq[qq % 2][:, r, :, :].rearrange("p c t -> p (c t)"), b_hTq4[qq % 2][r], (6, 7))
                P.dma("act", hTs[u], hTq[qq % 2][:, r, :, :].rearrange("p c t -> p (c t)"), reads=[b_hTq4[qq % 2][r]])

        def norm_one(qq, r):
            u = 4 * qq + r
            k_ = u % 2
            P.dma("sp", xt[k_][:], xa[u * 128:(u + 1) * 128, :], writes=[b_xt[k_]])
            norm_tile(NS0[r], xt[k_][:], b_xt[k_], A1[:], b_A1, B1[:], b_B1, hb[r][:], b_hb[r])

        def projA(q, s, gi):
            ca, cs = [(0, 2), (1, 3), (4, 5)][gi]
            banks = (2 * (gi % 2), 2 * (gi % 2) + 1)
            for bi, cc in enumerate((ca, cs)):
                for ck in range(16):
                    P.pe(lambda e, ck=ck, cc=cc, bank=banks[bi]: e.matmul(
                        psf(bank), lhsT=w1a_t[:, ck, cc * 128:(cc + 1) * 128], rhs=hTq[s][:, :, ck, :],
                        start=(ck == 0), stop=(ck == 15)),
                        reads=[b_w1a] + b_hTq4[s], writes=[PSB[banks[bi]]])

        def evacA(q, gi):
            banks = (2 * (gi % 2), 2 * (gi % 2) + 1)
            k2 = 2 * (gi % 2)
            if gi < 2:
                rope_evac(KT[:, gi, q * 512:(q + 1) * 512], b_KT[q], banks[0], banks[1], tabs[0][:], tabs[1][:], b_tabs[0],
                          rt[k2][:], rt[k2 + 1][:], (b_rt[k2], b_rt[k2 + 1]))
            else:
                rope_evac(ikT[:, q * 512:(q + 1) * 512], b_ikT[q], banks[0], banks[1], tabs[2][:], tabs[3][:], b_tabs[1],
                          rt[k2][:], rt[k2 + 1][:], (b_rt[k2], b_rt[k2 + 1]))

        def projV(q, s):
            for r in range(4):
                u = 4 * q + r
                bank = 4 + (u % 2)
                for ck in range(16):
                    P.pe(lambda e, ck=ck, r=r, bank=bank: e.matmul(psf(bank)[:, 0:256], lhsT=hTq[s][:, r, ck, :], rhs=wv_t[:, ck, :],
                                                                    start=(ck == 0), stop=(ck == 15)),
                         reads=[b_wv, b_hTq4[s][r]], writes=[PSB[bank]])
                P.act(lambda e, u=u, bank=bank: e.activation(out=Vx[:, u, :, 0:128],
                                                             in_=psf(bank)[:, 0:256].rearrange("p (g d) -> p g d", g=2),
                                                             func=AF.Copy, scale=vmask[:, u:u + 1]),
                      reads=[b_vmask], writes=[PSB[bank], b_Vx[u]])
                P.pool(lambda e, u=u: e.tensor_copy(out=Vx[:, u, :, 128:129],
                                                    in_=vmask[:, u:u + 1].unsqueeze(1).to_broadcast([128, 2, 1])),
                       reads=[b_vmask], writes=[b_Vx[u]])

        norm_quad(0)
        trans_quad(0)
        for q in range(NQ):
            s = q % 2
            nxt = q + 1 < NQ
            rope_tables(RS1, posr[:, q * 512:(q + 1) * 512], inv_a, sgn_a, tabs[0][:], tabs[1][:], b_tabs[0])
            rope_tables(RS1, posr[:, q * 512:(q + 1) * 512], inv_i, sgn_i, tabs[2][:], tabs[3][:], b_tabs[1])
            projA(q, s, 0)
            if nxt:
                norm_one(q + 1, 0)
            projA(q, s, 1)
            evacA(q, 0)
            if nxt:
                norm_one(q + 1, 1)
            projA(q, s, 2)
            evacA(q, 1)
            if nxt:
                norm_one(q + 1, 2)
            projV(q, s)
            evacA(q, 2)
            if nxt:
                norm_one(q + 1, 3)
            for _ in range(5):
                if pending_mod:
                    pending_mod.pop(0)()
            if nxt:
                trans_quad(q + 1)

    while pending_mod:
        pending_mod.pop(0)()

    A.release(m_p1)
    P.barrier()
    o_n = A.alloc([128, NOWN, 1024], BF16, "o_n")
    b_on = bufs(NOWN, "o_n")
    m_p1b = A.mark()
    if upto >= 3:
        ghbc = A.alloc([128, 1024], F32, "ghbc")
        b_ghbc = Buf()
        tmp_row2 = A.alloc([1, 512], F32, "tmprow2")
        b_tmprow2 = Buf()
        import os
        _skip = os.environ.get("MK_SKIP", "")
        if "g" not in _skip:
            bcast_rows(ghbc[:], b_ghbc, grows[3:4, :], 1024, tmp_row2, b_tmprow2, 7)
        lbt = A.alloc([128, 40], F32, "lbt")
        b_lbt = Buf()
        P.dma("sp", lbt[:, 0:16], lbc_d, writes=[b_lbt])
        P.dve(lambda e: e.tensor_tensor(out=lbt[:, 16:24], in0=lbt[:, 0:8], in1=lbt[:, 8:16], op=ALU.subtract),
              reads=[b_lbt], writes=[b_lbt])
        P.act(lambda e: e.activation(out=lbt[:, 16:24], in_=lbt[:, 16:24], func=AF.Sigmoid), reads=[b_lbt], writes=[b_lbt])
        P.dve(lambda e: e.tensor_scalar(out=lbt[:, 24:32], in0=lbt[:, 16:24], scalar1=-1.0, scalar2=1.0, op0=ALU.mult, op1=ALU.add),
              reads=[b_lbt], writes=[b_lbt])
        P.dve(lambda e: e.tensor_scalar(out=lbt[:, 32:40], in0=lbt[:, 16:24], scalar1=-1.0, scalar2=None, op0=ALU.add),
              reads=[b_lbt], writes=[b_lbt])
        rm512 = A.alloc([128, 8, 64], F32, "rm512")
        b_rm = Buf()
        P.dve(lambda e: e.tensor_copy(out=rm512[:], in_=rmask.unsqueeze(1).to_broadcast([128, 8, 64])), reads=[b_cst], writes=[b_rm])
        whf = A.alloc([128, 16, 512], BF16, "whf")
        whi = A.alloc([128, 16, 512], BF16, "whi")
        whq = A.alloc([128, 16, 512], BF16, "whq")
        b_whf, b_whi, b_whq = Buf(), Buf(), Buf()
        hTqB = [A.alloc([128, 4, 16, 128], BF16, f"hTqb{i}") for i in range(2)]
        b_hTqB = bufs(2)
        bA = [A.alloc([128, 512], F32, f"hgA{i}") for i in range(4)]
        bK = [A.alloc([128, 512], F32, f"hgK{i}") for i in range(4)]
        bC = [A.alloc([128, 512], F32, f"hgC{i}") for i in range(4)]
        b_bA, b_bK, b_bC = bufs(4), bufs(4), bufs(4)
        kdT = A.alloc([128, 4, 512], BF16, "kdT")
        b_kdT = bufs(4)
        dec = A.alloc([128, 4, 8], F32, "dec")
        b_dec = Buf()
        ep = A.alloc([128, 4, 128], F32, "ep")
        b_ep = Buf()
        v_t = [A.alloc([128, 512], BF16, f"v_t{i}") for i in range(2)]
        b_vt = bufs(2)
        kd_t = [A.alloc([128, 4, 128], BF16, f"kd_t{i}") for i in range(2)]
        b_kdt = bufs(2)
        qs = A.alloc([128, 4, 128], F32, "qs")
        b_qs = Buf()
        qdT = A.alloc([128, 4, 128], BF16, "qdT")
        q0 = A.alloc([128, 4, 128], BF16, "q0")
        q1 = A.alloc([128, 4, 128], BF16, "q1")
        b_qd, b_q0, b_q1 = Buf(), Buf(), Buf()
        attm = A.alloc([128, 4, 128], BF16, "attm")
        b_attm = Buf()
        Sst = A.alloc([128, 4, 128], F32, "Sst")
        b_S = Buf()
        Sbf = [A.alloc([128, 4, 128], BF16, f"Sbf{i}") for i in range(2)]
        b_Sbf = bufs(2)
        tmpU = A.alloc([128, 4, 128], F32, "tmpU")
        b_tmpU = Buf()
        o_sb = A.alloc([128, 4, 128], F32, "o_sb")
        b_osb = Buf()
        junk4 = A.alloc([128, 128], BF16, "junk4")
        b_junk4 = Buf()
        st4 = A.alloc([128, 12], F32, "st4")
        b_st4 = Buf()
        if "m" not in _skip:
            P.pool(lambda e: e.memset(q0[:], 0.0), writes=[b_q0])
            P.pool(lambda e: e.memset(q1[:], 0.0), writes=[b_q1])
        import os
        for hg in range(int(os.environ.get("MK_NHG", 2))):
            P.dma("pool", whf[:], w_in[:, O_HF + hg * 512:O_HF + (hg + 1) * 512].rearrange("(c p) f -> p c f", p=128), writes=[b_whf])
            P.dma("pool", whi[:], w_in[:, O_HI + hg * 512:O_HI + (hg + 1) * 512].rearrange("(c p) f -> p c f", p=128), writes=[b_whi])
            P.dma("pool", whq[:], w_in[:, O_HQ + hg * 512:O_HQ + (hg + 1) * 512].rearrange("(c p) f -> p c f", p=128), writes=[b_whq])
            P.pool(lambda e: e.memset(Sst[:], 0.0), writes=[b_S])
            for q in range(int(os.environ.get("MK_NQ1B", NQ))):
                s = q % 2
                for r in range(4):
                    P.dma("sp", hTqB[s][:, r, :, :], hTs[4 * q + r].rearrange("p (c t) -> p c t", c=16), writes=[b_hTqB[s]])
                for h in range(4):
                    for ck in range(16):
                        P.pe(lambda e, ck=ck, h=h, s=s: e.matmul(psf(h), lhsT=whf[:, ck, h * 128:(h + 1) * 128], rhs=hTqB[s][:, :, ck, :],
                                                                  start=(ck == 0), stop=(ck == 15)),
                             reads=[b_whf, b_hTqB[s]], writes=[PSB[h]])
                for h in range(4):
                    P.act(lambda e, h=h: e.activation(out=bA[h][:], in_=psf(h), func=AF.Sigmoid), writes=[PSB[h], b_bA[h]])
                for h in range(4):
                    H = 4 * hg + h
                    P.dve(lambda e, h=h, H=H: e.tensor_scalar(out=bK[h][:], in0=bA[h][:], scalar1=-1.0, scalar2=lbt[:, 32 + H:33 + H],
                                                               op0=ALU.add, op1=ALU.mult),
                          reads=[b_bA[h], b_lbt], writes=[b_bK[h]])
                for h in range(4):
                    H = 4 * hg + h
                    P.act(lambda e, h=h, H=H: e.activation(out=bA[h][:], in_=bA[h][:], func=AF.Ln, scale=lbt[:, 24 + H:25 + H],
                                                            bias=lbt[:, 16 + H:17 + H]),
                          reads=[b_bA[h], b_lbt], writes=[b_bA[h]])
                for h in range(4):
                    P.dve(lambda e, h=h: e.tensor_tensor_scan(out=bC[h][:], data0=rm512[:].rearrange("p c t -> p (c t)"), data1=bA[h][:],
                                                              initial=0.0, op0=ALU.mult, op1=ALU.add),
                          reads=[b_bA[h], b_rm], writes=[b_bC[h]])
                for h in range(4):
                    P.act(lambda e, h=h: e.activation(out=bA[h][:], in_=bC[h][:], func=AF.Exp, scale=-1.0), reads=[b_bC[h]], writes=[b_bA[h]])
                for h in range(4):
                    P.act(lambda e, h=h: e.activation(out=dec[:, h, :], in_=bC[h][:].rearrange("p (c t) -> p c t", t=64)[:, :, 63],
                                                      func=AF.Exp),
                          reads=[b_bC[h]], writes=[b_dec])
                for h in range(4):
                    P.act(lambda e, h=h: e.activation(out=ep[:, h, :], in_=bC[h][:, 384:512], func=AF.Exp), reads=[b_bC[h]], writes=[b_ep])
                for h in range(4):
                    P.dve(lambda e, h=h: e.tensor_tensor(out=kdT[:, h, :], in0=bK[h][:], in1=bA[h][:], op=ALU.mult),
                          reads=[b_bK[h], b_bA[h]], writes=[b_kdT[h]])
                for r in range(4):
                    u = 4 * q + r
                    k2 = u % 2
                    own = (r == 3)
                    for ck in range(16):
                        P.pe(lambda e, ck=ck, r=r, s=s: e.matmul(psf(4), lhsT=hTqB[s][:, r, ck, :], rhs=whi[:, ck, :],
                                                                  start=(ck == 0), stop=(ck == 15)),
                             reads=[b_whi, b_hTqB[s]], writes=[PSB[4]])
                    P.act(lambda e, u=u, k2=k2: e.activation(out=v_t[k2][:], in_=psf(4), func=AF.Copy, scale=vmask[:, u:u + 1]),
                          reads=[b_vmask], writes=[PSB[4], b_vt[k2]])
                    for h in range(4):
                        P.pe(lambda e, h=h, r=r: e.transpose(out=psb(5)[:, h * 128:(h + 1) * 128], in_=kdT[:, h, r * 128:(r + 1) * 128],
                                                             identity=ident_b[:]),
                             reads=[b_kdT[h], b_idb], writes=[PSB[5]])
                    P.dve(lambda e, k2=k2: e.tensor_copy(out=kd_t[k2][:].rearrange("p h d -> p (h d)"), in_=psb(5)[:, 0:512]),
                          writes=[PSB[5], b_kdt[k2]])
                    if own:
                        i = q
                        for h in range(4):
                            for ck in range(16):
                                P.pe(lambda e, ck=ck, h=h, s=s: e.matmul(psf(7)[:, h * 128:(h + 1) * 128], lhsT=whq[:, ck, h * 128:(h + 1) * 128],
                                                                          rhs=hTqB[s][:, 3, ck, :], start=(ck == 0), stop=(ck == 15)),
                                     reads=[b_whq, b_hTqB[s]], writes=[PSB[7]])
                        P.act(lambda e: e.activation(out=qs[:].rearrange("p h d -> p (h d)"), in_=psf(7), func=AF.Silu),
                              writes=[PSB[7], b_qs])
                        P.dve(lambda e: e.tensor_tensor(out=qdT[:], in0=qs[:], in1=ep[:], op=ALU.mult), reads=[b_qs, b_ep], writes=[b_qd])
                        P.pool(lambda e: e.tensor_copy(out=q0[:, :, 0:64], in_=qdT[:, :, 0:64]), reads=[b_qd], writes=[b_q0])
                        P.pool(lambda e: e.tensor_copy(out=q1[:, :, 64:128], in_=qdT[:, :, 64:128]), reads=[b_qd], writes=[b_q1])
                        for h in range(4):
                            P.pe(lambda e, h=h: e.matmul(psf(7)[:, h * 128:(h + 1) * 128], lhsT=kdT[:, h, 384:512], rhs=qdT[:, h, :],
                                                         start=True, stop=True),
                                 reads=[b_kdT[h], b_qd], writes=[PSB[7]])
                        P.dve(lambda e: e.tensor_tensor(out=attm[:], in0=psf(7).rearrange("p (h d) -> p h d", h=4),
                                                        in1=tmask.unsqueeze(1).to_broadcast([128, 4, 128]), op=ALU.mult),
                              reads=[b_cst], writes=[PSB[7], b_attm])
                    for c in range(2):
                        if own:
                            P.act(lambda e, c=c: e.activation(out=Sbf[c][:], in_=Sst[:], func=AF.Copy), reads=[b_S], writes=[b_Sbf[c]])
                        for h in range(4):
                            P.pe(lambda e, h=h, c=c, k2=k2: e.matmul(psf(6)[:, h * 128:(h + 1) * 128], lhsT=kd_t[k2][64 * c:64 * c + 64, h, :],
                                                                      rhs=v_t[k2][64 * c:64 * c + 64, h * 128:(h + 1) * 128], start=True, stop=True),
                                 reads=[b_kdt[k2], b_vt[k2]], writes=[PSB[6]])
                        cq = 2 * r + c
                        P.dve(lambda e: e.tensor_tensor(out=tmpU[:], in0=psf(6).rearrange("p (h d) -> p h d", h=4), in1=Sst[:], op=ALU.add),
                              reads=[b_S], writes=[PSB[6], b_tmpU])
                        P.dve(lambda e, cq=cq: e.tensor_tensor(out=Sst[:], in0=tmpU[:], in1=dec[:, :, cq:cq + 1].to_broadcast([128, 4, 128]),
                                                               op=ALU.mult),
                              reads=[b_tmpU, b_dec], writes=[b_S])
                    if own:
                        for h in range(4):
                            P.pe(lambda e, h=h, k2=k2: e.matmul(psf(7)[:, h * 128:(h + 1) * 128], lhsT=attm[:, h, :],
                                                                rhs=v_t[k2][:, h * 128:(h + 1) * 128], start=True, stop=False),
                                 reads=[b_attm, b_vt[k2]], writes=[PSB[7]])
                            P.pe(lambda e, h=h: e.matmul(psf(7)[:, h * 128:(h + 1) * 128], lhsT=q0[:, h, :], rhs=Sbf[0][:, h, :],
                                                         start=False, stop=False),
                                 reads=[b_q0, b_Sbf[0]], writes=[PSB[7]])
                            P.pe(lambda e, h=h: e.matmul(psf(7)[:, h * 128:(h + 1) * 128], lhsT=q1[:, h, :], rhs=Sbf[1][:, h, :],
                                                         start=False, stop=True),
                                 reads=[b_q1, b_Sbf[1]], writes=[PSB[7]])
                        P.act(lambda e: e.activation(out=o_sb[:].rearrange("p h d -> p (h d)"), in_=psf(7), func=AF.Copy),
                              writes=[PSB[7], b_osb])
                        for h in range(4):
                            P.dve(lambda e, h=h: e.scalar_tensor_tensor(out=junk4[:], in0=o_sb[:, h, :], scalar=1.0, in1=o_sb[:, h, :],
                                                                        op0=ALU.mult, op1=ALU.mult, accum_out=st4[:, h:h + 1]),
                                  reads=[b_osb], writes=[b_junk4, b_st4])
                        P.act(lambda e: e.activation(out=st4[:, 4:8], in_=st4[:, 0:4], func=AF.Sqrt, scale=1.0 / 128, bias=EPS),
                              reads=[b_st4], writes=[b_st4])
                        P.dve(lambda e: e.reciprocal(out=st4[:, 8:12], in_=st4[:, 4:8]), reads=[b_st4], writes=[b_st4])
                        for h in range(4):
                            H = 4 * hg + h
                            P.dve(lambda e, h=h, H=H, i=i: e.scalar_tensor_tensor(out=o_n[:, i, H * 128:(H + 1) * 128], in0=o_sb[:, h, :],
                                                                                   scalar=st4[:, 8 + h:9 + h], in1=ghbc[:, H * 128:(H + 1) * 128],
                                                                                   op0=ALU.mult, op1=ALU.mult),
                                  reads=[b_osb, b_st4, b_ghbc], writes=[b_on[i]])

    if dbg and upto == 3:
        t = dbg_tensor("o_n", [128, NOWN, 1024], BF16)
        final_ops.append(P.dma("sp", t, o_n[:], reads=b_on))
        t = dbg_tensor("ikT", [128, NU * 128], BF16)
        final_ops.append(P.dma("sp", t, ikT[:], reads=b_ikT))
        t = dbg_tensor("KT", [128, 2, NU * 128], BF16)
        final_ops.append(P.dma("sp", t, KT[:], reads=b_KT))

    A.release(m_p1b)
    P.barrier()
    y_att = A.alloc([128, NOWN, 1024], BF16, "y_att")
    b_yatt = bufs(NOWN, "yatt")
    m_p2 = A.mark()
    if upto >= 4:
        QT = A.alloc([128, 8, 1024], BF16, "QT")
        iqT = A.alloc([128, 4, 1024], BF16, "iqT")
        b_QT, b_iqT = bufs(8, "QT"), bufs(4, "iqT")
        iwt = A.alloc([128, NOWN, 24], F32, "iwt")
        b_iwt = bufs(NOWN, "iwt")
        smask_t = A.alloc([128, 512], F32, "smask")
        b_smask = Buf()
        P.dma("sp", smask_t[:], smask_d, writes=[b_smask])
        m_p2a = A.mark()
        hTo = A.alloc([128, NOWN, 16, 128], BF16, "hTo")
        b_hTo = bufs(NOWN, "hTo")
        for i in range(NOWN):
            P.dma("sp", hTo[:, i, :, :], hTs[4 * i + 3].rearrange("p (c t) -> p c t", c=16), writes=[b_hTo[i]])
        RS2 = rope_scratch()
        otab = [A.alloc([128, 1024], F32, f"otab{i}") for i in range(4)]
        b_otab = [Buf("otab_a"), Buf("otab_i")]
        for hf2 in range(2):
            rope_tables(RS2, poso[:, hf2 * 512:(hf2 + 1) * 512], inv_a, sgn_a, otab[0][:, hf2 * 512:(hf2 + 1) * 512],
                        otab[1][:, hf2 * 512:(hf2 + 1) * 512], b_otab[0])
            rope_tables(RS2, poso[:, hf2 * 512:(hf2 + 1) * 512], inv_i, sgn_i, otab[2][:, hf2 * 512:(hf2 + 1) * 512],
                        otab[3][:, hf2 * 512:(hf2 + 1) * 512], b_otab[1])
        wiw = A.alloc([128, 16, 8], BF16, "wiw")
        b_wiw = Buf()
        P.dma("pool", wiw[:], w_in[:, O_IW:O_IW + 8].rearrange("(c p) f -> p c f", p=128), writes=[b_wiw])
        for i in range(NOWN):
            for ck in range(16):
                P.pe(lambda e, ck=ck, i=i: e.matmul(psf(7)[:, 0:8], lhsT=hTo[:, i, ck, :], rhs=wiw[:, ck, :], start=(ck == 0), stop=(ck == 15)),
                     reads=[b_hTo[i], b_wiw], writes=[PSB[7]])
            P.act(lambda e, i=i: e.activation(out=iwt[:, i, 0:8], in_=psf(7)[:, 0:8], func=AF.Copy), writes=[PSB[7], b_iwt[i]])
            P.act(lambda e, i=i: e.activation(out=iwt[:, i, 8:16], in_=iwt[:, i, 0:8], func=AF.Abs, scale=IDX_SCALE),
                  reads=[b_iwt[i]], writes=[b_iwt[i]])
            P.dve(lambda e, i=i: e.tensor_scalar(out=iwt[:, i, 16:24], in0=iwt[:, i, 0:8], scalar1=0.0, scalar2=2.0,
                                                 op0=ALU.is_ge, op1=ALU.mult), reads=[b_iwt[i]], writes=[b_iwt[i]])
            P.dve(lambda e, i=i: e.tensor_scalar(out=iwt[:, i, 16:24], in0=iwt[:, i, 16:24], scalar1=-1.0, scalar2=None,
                                                 op0=ALU.add), reads=[b_iwt[i]], writes=[b_iwt[i]])
        wq = [[A.alloc([128, 16, 256], BF16, f"wq{k}_{t}") for t in range(2)] for k in range(2)]
        b_wq = [[Buf(), Buf()] for k in range(2)]
        rt2 = [A.alloc([128, 512], F32, f"rt2_{i}") for i in range(4)]
        b_rt2 = bufs(4)
        groups = [("q", g) for g in range(4)] + [("i", g) for g in range(2)]
        cnt = 0
        for gi, (kind, g) in enumerate(groups):
            k = gi % 2
            if kind == "q":
                src_a = w_in[:, O_AQ + g * 256:O_AQ + (g + 1) * 256]
                src_s = w2s[:, g * 256:(g + 1) * 256]
            else:
                src_a = w_in[:, O_IQ + g * 256:O_IQ + (g + 1) * 256]
                src_s = w2s[:, 1024 + g * 256:1024 + (g + 1) * 256]
            P.dma("pool", wq[k][0][:], src_a.rearrange("(c p) f -> p c f", p=128), writes=[b_wq[k][0]])
            P.dma("pool", wq[k][1][:], src_s.rearrange("(c p) f -> p c f", p=128), writes=[b_wq[k][1]])
            for cc in range(2):
                for hf2 in range(2):
                    banks = (2 * (cnt % 2), 2 * (cnt % 2) + 1)
                    k2 = 2 * (cnt % 2)
                    cnt += 1
                    for t in range(2):
                        for ck in range(16):
                            P.pe(lambda e, ck=ck, cc=cc, hf2=hf2, t=t, k=k, bank=banks[t]: e.matmul(
                                psf(bank), lhsT=wq[k][t][:, ck, cc * 128:(cc + 1) * 128],
                                rhs=hTo[:, 4 * hf2:4 * hf2 + 4, ck, :], start=(ck == 0), stop=(ck == 15)),
                                reads=[b_wq[k][t]] + b_hTo[4 * hf2:4 * hf2 + 4], writes=[PSB[banks[t]]])
                    if kind == "q":
                        hd = 2 * g + cc
                        rope_evac(QT[:, hd, hf2 * 512:(hf2 + 1) * 512], b_QT[hd], banks[0], banks[1],
                                  otab[0][:, hf2 * 512:(hf2 + 1) * 512], otab[1][:, hf2 * 512:(hf2 + 1) * 512], b_otab[0],
                                  rt2[k2][:], rt2[k2 + 1][:], (b_rt2[k2], b_rt2[k2 + 1]))
                    else:
                        chn = 2 * g + cc
                        rope_evac(iqT[:, chn, hf2 * 512:(hf2 + 1) * 512], b_iqT[chn], banks[0], banks[1],
                                  otab[2][:, hf2 * 512:(hf2 + 1) * 512], otab[3][:, hf2 * 512:(hf2 + 1) * 512], b_otab[1],
                                  rt2[k2][:], rt2[k2 + 1][:], (b_rt2[k2], b_rt2[k2 + 1]))
        A.release(m_p2a)
        P.barrier()
        scoreL = [A.alloc([128, 4096], F32, f"score{i}") for i in range(2)]
        b_scoreL = bufs(2, "score")
        mask01L = [A.alloc([128, 4096], BF16, f"mask01_{i}") for i in range(2)]
        b_mask01L = bufs(2, "mask01")
        maskTL = [A.alloc([128, 32, 128], BF16, f"maskT{i}") for i in range(2)]
        b_maskTL = bufs(2, "maskT")
        bsL = [A.alloc([128, 8 + NBIS], F32, f"bs{i}") for i in range(2)]
        b_bsL = bufs(2, "bs")
        sacc = [A.alloc([128, 512], F32, f"sacc{i}") for i in range(2)]
        b_sacc = bufs(2)
        rl = [A.alloc([128, 512], F32, f"rl{i}") for i in range(2)]
        b_rl = bufs(2)
        Et = [A.alloc([128, 4, 128], BF16, f"Et{i}") for i in range(2)]
        b_Et = bufs(2)
        PT = [A.alloc([128, 4, 128], BF16, f"PT{i}") for i in range(2)]
        b_PT = bufs(2)
        rc = A.alloc([128, 4], F32, "rc")
        b_rc = Buf()
        cnts = dict(ndot=0, nst=0)

        def indexer(i):
            score, b_score, bs, b_bs = scoreL[i % 2], b_scoreL[i % 2], bsL[i % 2], b_bsL[i % 2]
            nk = 512 * (i + 1)
            for kq in range(i + 1):
                for h in range(8):
                    bank = cnts["ndot"] % 2
                    k = cnts["ndot"] % 2
                    cnts["ndot"] += 1
                    pb = (h % 2) * 64
                    P.pe(lambda e, h=h, kq=kq, bank=bank, pb=pb: e.matmul(
                        psf(bank), lhsT=iqT[pb:pb + 64, h // 2, i * 128:(i + 1) * 128], rhs=ikT[pb:pb + 64, kq * 512:(kq + 1) * 512],
                        start=True, stop=True),
                        reads=[b_iqT[h // 2], b_ikT[kq]], writes=[PSB[bank]])
                    P.act(lambda e, h=h, bank=bank, k=k: e.activation(out=rl[k][:], in_=psf(bank), func=AF.Relu,
                                                                       scale=iwt[:, i, 8 + h:9 + h]),
                          reads=[b_iwt[i]], writes=[PSB[bank], b_rl[k]])
                    dst = score[:, kq * 512:(kq + 1) * 512] if h == 7 else sacc[h % 2][:]
                    b_dst = b_score if h == 7 else b_sacc[h % 2]
                    if h == 0:
                        P.dve(lambda e, k=k, dst=dst: e.tensor_scalar(out=dst, in0=rl[k][:], scalar1=iwt[:, i, 16:17], scalar2=None,
                                                                       op0=ALU.mult),
                              reads=[b_rl[k], b_iwt[i]], writes=[b_dst])
                    else:
                        P.dve(lambda e, k=k, h=h, dst=dst: e.scalar_tensor_tensor(
                            out=dst, in0=rl[k][:], scalar=iwt[:, i, 16 + h:17 + h], in1=sacc[(h - 1) % 2][:], op0=ALU.mult, op1=ALU.add),
                            reads=[b_rl[k], b_iwt[i], b_sacc[(h - 1) % 2]], writes=[b_dst])
            P.dve(lambda e: e.tensor_reduce(out=bs[:, 0:1], in_=score[:, 0:nk], axis=AX.X, op=ALU.max, apply_absolute_value=True),
                  reads=[b_score], writes=[b_bs])
            P.dve(lambda e: e.tensor_tensor(out=score[:, nk - 128:nk], in0=score[:, nk - 128:nk], in1=cmask, op=ALU.add),
                  reads=[b_score, b_cst], writes=[b_score])
            P.dve(lambda e: e.tensor_tensor(out=score[:, 0:512], in0=score[:, 0:512], in1=smask_t[:], op=ALU.add),
                  reads=[b_score, b_smask], writes=[b_score])
            P.dve(lambda e: e.tensor_scalar(out=bs[:, 8:8 + NBIS], in0=bisc, scalar1=bs[:, 0:1], scalar2=None, op0=ALU.mult),
                  reads=[b_bs, b_cst], writes=[b_bs])
            P.dve(lambda e: e.tensor_scalar(out=bs[:, 1:2], in0=bs[:, 0:1], scalar1=-1.0, scalar2=None, op0=ALU.mult),
                  reads=[b_bs], writes=[b_bs])

        def bis_step(i, k, step):
            score, b_score, bs, b_bs = scoreL[i % 2], b_scoreL[i % 2], bsL[i % 2], b_bsL[i % 2]
            mask01, b_mask01 = mask01L[i % 2], b_mask01L[i % 2]
            nk = 512 * (i + 1)
            if step == 0:
                P.dve(lambda e: e.tensor_tensor(out=bs[:, 2:3], in0=bs[:, 1:2], in1=bs[:, 8 + k:9 + k], op=ALU.add),
                      reads=[b_bs], writes=[b_bs])
            elif step == 1:
                P.dve(lambda e: e.tensor_scalar(out=mask01[:, 0:nk], in0=score[:, 0:nk], scalar1=bs[:, 2:3], scalar2=0.0,
                                                op0=ALU.is_ge, op1=ALU.add, accum_out=bs[:, 3:4]),
                      reads=[b_score, b_bs], writes=[b_mask01, b_bs])
            elif step == 2:
                P.dve(lambda e: e.tensor_scalar(out=bs[:, 4:5], in0=bs[:, 3:4], scalar1=256.0, scalar2=bs[:, 8 + k:9 + k],
                                                op0=ALU.is_ge, op1=ALU.mult),
                      reads=[b_bs], writes=[b_bs])
            else:
                P.dve(lambda e: e.tensor_tensor(out=bs[:, 1:2], in0=bs[:, 1:2], in1=bs[:, 4:5], op=ALU.add), reads=[b_bs], writes=[b_bs])

        def make_mask(i):
            score, b_score, bs, b_bs = scoreL[i % 2], b_scoreL[i % 2], bsL[i % 2], b_bsL[i % 2]
            mask01, b_mask01 = mask01L[i % 2], b_mask01L[i % 2]
            maskT, b_maskT = maskTL[i % 2], b_maskTL[i % 2]
            nk = 512 * (i + 1)
            nkt = 4 * (i + 1)
            P.dve(lambda e: e.tensor_scalar(out=mask01[:, 0:nk], in0=score[:, 0:nk], scalar1=bs[:, 1:2], scalar2=None, op0=ALU.is_ge),
                  reads=[b_score, b_bs], writes=[b_mask01])
            for k0 in range(0, nkt, 8):
                n = min(8, nkt - k0)
                for kk in range(n):
                    kt = k0 + kk
                    P.pe(lambda e, kk=kk, kt=kt: e.transpose(out=psb(2)[:, kk * 128:(kk + 1) * 128], in_=mask01[:, kt * 128:(kt + 1) * 128],
                                                             identity=ident_b[:]),
                         reads=[b_mask01, b_idb], writes=[PSB[2]])
                P.act(lambda e, k0=k0, n=n: e.activation(out=maskT[:, k0:k0 + n, :].rearrange("p k q -> p (k q)"),
                                                         in_=psb(2)[:, 0:n * 128], func=AF.Copy),
                      writes=[PSB[2], b_maskT])

        lnr = A.alloc([128, 8], F32, "lnr")
        b_lnr = Buf()
        Et2 = [A.alloc([128, 2, 128], BF16, f"Et2_{i}") for i in range(3)]
        b_Et2 = bufs(3)
        PT2 = [A.alloc([128, 2, 128], BF16, f"PT2_{i}") for i in range(3)]
        b_PT2 = bufs(3)

        def attention(i, use_dve=False):
            maskT, b_maskT = maskTL[i % 2], b_maskTL[i % 2]
            nkt = 4 * (i + 1)
            for hp in range(4):
                g = hp // 2
                accb = (4, 5) if hp % 2 == 0 else (7, 2)
                steps = []
                for kt in range(nkt):
                    sb_ = (3, 6)[cnts["nst"] % 2]
                    k = cnts["nst"] % 3
                    cnts["nst"] += 1
                    steps.append((kt, sb_, k))
                for j in range(nkt + 2):
                    if j < nkt:
                        kt, sb_, k = steps[j]
                        P.pe(lambda e, kt=kt, g=g, hp=hp, sb_=sb_: e.matmul(psf(sb_)[:, 0:256], lhsT=KT[:, g, kt * 128:(kt + 1) * 128],
                                                                             rhs=QT[:, 2 * hp:2 * hp + 2, i * 128:(i + 1) * 128],
                                                                             start=True, stop=True),
                             reads=[b_KT[kt // 4]] + b_QT[2 * hp:2 * hp + 2], writes=[PSB[sb_]])
                        P.act(lambda e, sb_=sb_, k=k: e.activation(out=Et2[k][:].rearrange("p h q -> p (h q)"), in_=psf(sb_)[:, 0:256],
                                                                   func=AF.Exp, scale=ATT_SCALE),
                              writes=[PSB[sb_], b_Et2[k]])
                        P.op("dve" if (use_dve and kt % 2 == 1) else "pool",
                             lambda e, kt=kt, k=k: e.tensor_tensor(out=PT2[k][:], in0=Et2[k][:],
                                                                   in1=maskT[:, kt, :].unsqueeze(1).to_broadcast([128, 2, 128]), op=ALU.mult),
                             reads=[b_Et2[k], b_maskT], writes=[b_PT2[k]])
                    if j >= 2:
                        kt, sb_, k = steps[j - 2]
                        for hh in range(2):
                            P.pe(lambda e, hh=hh, kt=kt, g=g, k=k, ab=accb[hh]: e.matmul(
                                psf(ab)[:, 0:129], lhsT=PT2[k][:, hh, :], rhs=Vx[:, kt, g, 0:129], start=(kt == 0), stop=(kt == nkt - 1)),
                                reads=[b_PT2[k], b_Vx[kt]], writes=[PSB[accb[hh]]])
                for hh in range(2):
                    hd = 2 * hp + hh
                    ab = accb[hh]
                    P.act(lambda e, ab=ab, hd=hd: e.activation(out=lnr[:, hd:hd + 1], in_=psf(ab)[:, 128:129], func=AF.Ln),
                          writes=[PSB[ab], b_lnr])
                    P.act(lambda e, hd=hd: e.activation(out=lnr[:, hd:hd + 1], in_=lnr[:, hd:hd + 1], func=AF.Exp, scale=-1.0),
                          reads=[b_lnr], writes=[b_lnr])
                    P.act(lambda e, ab=ab, hd=hd: e.activation(out=y_att[:, i, hd * 128:(hd + 1) * 128], in_=psf(ab)[:, 0:128],
                                                               func=AF.Copy, scale=lnr[:, hd:hd + 1]),
                          reads=[b_lnr], writes=[PSB[ab], b_yatt[i]])

        NP2 = NOWN // 2
        for pr in range(NP2 + 1):
            if pr < NP2:
                for i in (2 * pr, 2 * pr + 1):
                    indexer(i)
            if pr >= 1:
                for i in (2 * pr - 2, 2 * pr - 1):
                    attention(i, use_dve=(pr == NP2))
            if pr < NP2:
                tiles = (2 * pr, 2 * pr + 1)
                for k in range(NBIS):
                    for step in range(4):
                        for i in tiles:
                            bis_step(i, k, step)
                for i in tiles:
                    make_mask(i)

    if dbg and upto == 4:
        t = dbg_tensor("y_att", [128, NOWN, 1024], BF16)
        final_ops.append(P.dma("sp", t, y_att[:], reads=b_yatt))
        if upto >= 4:
            t = dbg_tensor("ikT", [128, NU * 128], BF16)
            final_ops.append(P.dma("sp", t, ikT[:], reads=b_ikT))
            t = dbg_tensor("rl0", [128, 512])
            final_ops.append(P.dma("sp", t, rl[0][:], reads=[b_rl[0]]))
            t = dbg_tensor("rl1", [128, 512])
            final_ops.append(P.dma("sp", t, rl[1][:], reads=[b_rl[1]]))
            t = dbg_tensor("sacc0", [128, 512])
            final_ops.append(P.dma("sp", t, sacc[0][:], reads=[b_sacc[0]]))
            t = dbg_tensor("score", [128, 4096])
            final_ops.append(P.dma("sp", t, scoreL[1][:], reads=[b_scoreL[1]]))
            t = dbg_tensor("bs", [128, 8 + NBIS])
            final_ops.append(P.dma("sp", t, bsL[1][:], reads=[b_bsL[1]]))
            t = dbg_tensor("mask01", [128, 4096], BF16)
            final_ops.append(P.dma("sp", t, mask01L[1][:], reads=[b_mask01L[1]]))
            t = dbg_tensor("maskT", [128, 32, 128], BF16)
            final_ops.append(P.dma("sp", t, maskTL[1][:], reads=[b_maskTL[1]]))
            t = dbg_tensor("iwt", [128, NOWN, 24])
            final_ops.append(P.dma("sp", t, iwt[:], reads=b_iwt))
            t = dbg_tensor("QT", [128, 8, 1024], BF16)
            final_ops.append(P.dma("sp", t, QT[:], reads=b_QT))
            t = dbg_tensor("iqT", [128, 4, 1024], BF16)
            final_ops.append(P.dma("sp", t, iqT[:], reads=b_iqT))

    P.barrier()
    R_L, R_M, R_H = 19584, 61056, 93824
    h2T = None
    comb = None
    if upto >= 5:
        A.off = R_L
        yaT = A.alloc([128, 8, 1024], BF16, "yaT")
        yhT = A.alloc([128, 8, 1024], BF16, "yhT")
        G1bc = A.alloc([128, D], F32, "G1bc")
        b_yaT, b_yhT, b_G1 = bufs(NOWN, "yaT"), Buf("yhT"), Buf("G1")
        assert A.off <= R_M
        A.off = R_H
        onT = A.alloc([128, 8, 1024], BF16, "onT")
        b_onT = bufs(NOWN, "onT")
        hTo4 = A.alloc([128, NOWN, 16, 128], BF16, "hTo4")
        b_hTo4 = bufs(NOWN, "hTo4")
        whog = [A.alloc([128, 16, 256], BF16, f"whog{i}") for i in range(2)]
        b_whog = bufs(2)
        sil4 = [A.alloc([128, 512], F32, f"sil4_{i}") for i in range(2)]
        b_sil4 = bufs(2)
        wg4 = [dict(ga=A.alloc([128, 16, 256], BF16, f"wga{i}"), gh=A.alloc([128, 16, 256], BF16, f"wgh{i}"),
                    au=A.alloc([128, 8, 256], BF16, f"wau{i}"), hu=A.alloc([128, 8, 256], BF16, f"whu{i}")) for i in range(2)]
        b_wg4 = [dict(ga=Buf(), gh=Buf(), au=Buf(), hu=Buf()) for i in range(2)]
        sg4 = [sil4[0], sil4[1]] + [A.alloc([128, 512], F32, f"sg4_{i}") for i in range(2)]
        b_sg4 = bufs(4)
        mm4 = [A.alloc([128, 512], F32, f"mm4_{i}") for i in range(4)]
        b_mm4 = bufs(4)
        P.dma("sp", G1bc[:], modscr[0], writes=[b_G1])
        for i in range(NOWN):
            P.dma("sp", hTo4[:, i, :, :], hTs[4 * i + 3].rearrange("p (c t) -> p c t", c=16), writes=[b_hTo4[i]])
        for i in range(NOWN):
            for (src, dstT, b_src, b_dstT, bank) in ((y_att, yaT, b_yatt, b_yaT, 0), (o_n, onT, b_on, b_onT, 1)):
                for c in range(8):
                    P.pe(lambda e, c=c, i=i, src=src, bank=bank: e.transpose(out=psb(bank)[:, c * 128:(c + 1) * 128],
                                                                             in_=src[:, i, c * 128:(c + 1) * 128], identity=ident_b[:]),
                         reads=[b_src[i], b_idb], writes=[PSB[bank]])
                P.act(lambda e, i=i, dstT=dstT, bank=bank: e.activation(out=dstT[:, :, i * 128:(i + 1) * 128],
                                                                        in_=psb(bank).rearrange("p (c t) -> p c t", c=8), func=AF.Copy),
                      writes=[PSB[bank], b_dstT[i]])
        n4 = 0
        for g4 in range(4):
            k4 = g4 % 2
            P.dma("pool", whog[k4][:], w_in[:, O_HOG + g4 * 256:O_HOG + (g4 + 1) * 256].rearrange("(c p) f -> p c f", p=128),
                  writes=[b_whog[k4]])
            for cc in range(2):
                chn = 2 * g4 + cc
                for hf4 in range(2):
                    bank = 2 + n4 % 2
                    kk = n4 % 2
                    n4 += 1
                    for ck in range(16):
                        P.pe(lambda e, ck=ck, cc=cc, hf4=hf4, k4=k4, bank=bank: e.matmul(
                            psf(bank), lhsT=whog[k4][:, ck, cc * 128:(cc + 1) * 128], rhs=hTo4[:, 4 * hf4:4 * hf4 + 4, ck, :],
                            start=(ck == 0), stop=(ck == 15)),
                            reads=[b_whog[k4]] + b_hTo4[4 * hf4:4 * hf4 + 4], writes=[PSB[bank]])
                    P.act(lambda e, bank=bank, kk=kk: e.activation(out=sil4[kk][:], in_=psf(bank), func=AF.Silu),
                          writes=[PSB[bank], b_sil4[kk]])
                    P.dve(lambda e, kk=kk, chn=chn, hf4=hf4: e.tensor_tensor(out=yhT[:, chn, hf4 * 512:(hf4 + 1) * 512], in0=sil4[kk][:],
                                                                             in1=onT[:, chn, hf4 * 512:(hf4 + 1) * 512], op=ALU.mult),
                          reads=[b_sil4[kk]] + b_onT[4 * hf4:4 * hf4 + 4], writes=[b_yhT])
        P.barrier()
        mT_t = nc.alloc_sbuf_tensor_at("mergedT", [128, 16, 1024], BF16, offset=R_M)
        b_mT = bufs(NOWN, "mT")
        n4 = 0
        for g4 in range(8):
            k4 = g4 % 2
            W = wg4[k4]
            BW = b_wg4[k4]
            P.dma("pool", W["ga"][:], w_in[:, O_GA + g4 * 256:O_GA + (g4 + 1) * 256].rearrange("(c p) f -> p c f", p=128), writes=[BW["ga"]])
            P.dma("pool", W["gh"][:], w_in[:, O_GH + g4 * 256:O_GH + (g4 + 1) * 256].rearrange("(c p) f -> p c f", p=128), writes=[BW["gh"]])
            P.dma("pool", W["au"][:], w_au[:, g4 * 256:(g4 + 1) * 256].rearrange("(c p) f -> p c f", p=128), writes=[BW["au"]])
            P.dma("pool", W["hu"][:], w_hu[:, g4 * 256:(g4 + 1) * 256].rearrange("(c p) f -> p c f", p=128), writes=[BW["hu"]])
            for cc in range(2):
                Dc = 2 * g4 + cc
                for hf4 in range(2):
                    kk = n4 % 2
                    n4 += 1
                    bk = [4 * kk + t for t in range(4)]
                    for ck in range(16):
                        P.pe(lambda e, ck=ck, cc=cc, hf4=hf4, W=W, bank=bk[0]: e.matmul(
                            psf(bank), lhsT=W["ga"][:, ck, cc * 128:(cc + 1) * 128], rhs=hTo4[:, 4 * hf4:4 * hf4 + 4, ck, :],
                            start=(ck == 0), stop=(ck == 15)),
                            reads=[BW["ga"]] + b_hTo4[4 * hf4:4 * hf4 + 4], writes=[PSB[bk[0]]])
                    for ck in range(16):
                        P.pe(lambda e, ck=ck, cc=cc, hf4=hf4, W=W, bank=bk[1]: e.matmul(
                            psf(bank), lhsT=W["gh"][:, ck, cc * 128:(cc + 1) * 128], rhs=hTo4[:, 4 * hf4:4 * hf4 + 4, ck, :],
                            start=(ck == 0), stop=(ck == 15)),
                            reads=[BW["gh"]] + b_hTo4[4 * hf4:4 * hf4 + 4], writes=[PSB[bk[1]]])
                    for fc in range(8):
                        P.pe(lambda e, fc=fc, cc=cc, hf4=hf4, W=W, bank=bk[2]: e.matmul(
                            psf(bank), lhsT=W["au"][:, fc, cc * 128:(cc + 1) * 128], rhs=yaT[:, fc, hf4 * 512:(hf4 + 1) * 512],
                            start=(fc == 0), stop=(fc == 7)),
                            reads=[BW["au"]] + b_yaT[4 * hf4:4 * hf4 + 4], writes=[PSB[bk[2]]])
                    for fc in range(8):
                        P.pe(lambda e, fc=fc, cc=cc, hf4=hf4, W=W, bank=bk[3]: e.matmul(
                            psf(bank), lhsT=W["hu"][:, fc, cc * 128:(cc + 1) * 128], rhs=yhT[:, fc, hf4 * 512:(hf4 + 1) * 512],
                            start=(fc == 0), stop=(fc == 7)),
                            reads=[BW["hu"], b_yhT], writes=[PSB[bk[3]]])
                    P.act(lambda e, kk=kk, bank=bk[0]: e.activation(out=sg4[2 * kk][:], in_=psf(bank), func=AF.Sigmoid),
                          writes=[PSB[bk[0]], b_sg4[2 * kk]])
                    P.act(lambda e, kk=kk, bank=bk[1]: e.activation(out=sg4[2 * kk + 1][:], in_=psf(bank), func=AF.Sigmoid),
                          writes=[PSB[bk[1]], b_sg4[2 * kk + 1]])
                    P.dve(lambda e, kk=kk, bank=bk[2]: e.tensor_tensor(out=mm4[2 * kk][:], in0=psf(bank), in1=sg4[2 * kk][:], op=ALU.mult),
                          reads=[b_sg4[2 * kk]], writes=[PSB[bk[2]], b_mm4[2 * kk]])
                    P.dve(lambda e, kk=kk, bank=bk[3]: e.tensor_tensor(out=mm4[2 * kk + 1][:], in0=psf(bank), in1=sg4[2 * kk + 1][:], op=ALU.mult),
                          reads=[b_sg4[2 * kk + 1]], writes=[PSB[bk[3]], b_mm4[2 * kk + 1]])
                    P.pool(lambda e, kk=kk, Dc=Dc, hf4=hf4: e.tensor_tensor(out=mT_t[:, Dc, hf4 * 512:(hf4 + 1) * 512], in0=mm4[2 * kk][:],
                                                                            in1=mm4[2 * kk + 1][:], op=ALU.add),
                           reads=[b_mm4[2 * kk], b_mm4[2 * kk + 1]], writes=b_mT[4 * hf4:4 * hf4 + 4])
        P.barrier()
        A.off = R_L
        h2T = A.alloc([128, NOWN, 16, 128], BF16, "h2T")
        b_h2T = bufs(NOWN, "h2T")
        G1b = A.alloc([128, D], F32, "G1b")
        b_G1b = Buf()
        assert A.off <= R_M
        A.off = R_H
        wo_t = A.alloc([128, 16, D], BF16, "wo_t")
        b_wo = bufs(4, "wo")
        for n in range(4):
            P.dma("pool", wo_t[:, :, n * 512:(n + 1) * 512], w_out[:, n * 512:(n + 1) * 512].rearrange("(c p) f -> p c f", p=128),
                  writes=[b_wo[n]])
        P.dma("sp", G1b[:], modscr[0], writes=[b_G1b])
        A2bc = A.alloc([128, D], F32, "A2bc")
        B2bc = A.alloc([128, D], F32, "B2bc")
        b_A2, b_B2 = Buf(), Buf()
        P.dma("sp", A2bc[:], modscr[1], writes=[b_A2])
        P.dma("sp", B2bc[:], modscr[2], writes=[b_B2])
        xo_t = [A.alloc([128, D], F32, "xo_t0")] * 2
        b_xo = [Buf("xo")] * 2
        x1_t = [A.alloc([128, D], F32, f"x1_t{i}") for i in range(2)]
        b_x1 = bufs(2)
        hb4 = [A.alloc([128, D], BF16, f"hb4_{i}") for i in range(2)]
        b_hb4 = bufs(2)
        NS4c = dict(xn=A.alloc([128, D], F32, "xn4"), b_xn=Buf(), st1=A.alloc([128, 8], F32, "st14"), b_ssq=Buf(), b_rstd=Buf())
        NS4 = [dict(junk=hb4[k_], b_junk=b_hb4[k_], **NS4c) for k_ in range(2)]
        wr_t = A.alloc([128, 16, 36], BF16, "wr_t")
        b_wr = Buf()
        P.dma("pool", wr_t[:], w_r.rearrange("(c p) f -> p c f", p=128), writes=[b_wr])
        brbc = A.alloc([128, 36], F32, "brbc")
        b_brbc = Buf()
        tmp_row4 = A.alloc([1, 512], F32, "tmprow4")
        b_tmprow4 = Buf()
        bcast_rows(brbc[:], b_brbc, b_r, 36, tmp_row4, b_tmprow4, 7)
        comb = nc.alloc_sbuf_tensor_at("comb", [128, NOWN, 32], F32, offset=COMB_OFF)
        b_comb = bufs(NOWN, "comb")
        rt4 = A.alloc([128, 928], F32, "rt4")
        b_rt4 = Buf()
        lgall = A.alloc([128, NOWN, 36], F32, "lgall")
        b_lgall = Buf()
        n4 = 0
        for i in range(NOWN):
            s4 = i % 2
            P.dma("sp", xo_t[s4][:], xo[i * 128:(i + 1) * 128, :], writes=[b_xo[s4]])
            for n in range(4):
                bank = n4 % 2
                kk = n4 % 2
                n4 += 1
                for Dc in range(16):
                    P.pe(lambda e, Dc=Dc, i=i, n=n, bank=bank: e.matmul(psf(bank), lhsT=mT_t[:, Dc, i * 128:(i + 1) * 128],
                                                                         rhs=wo_t[:, Dc, n * 512:(n + 1) * 512], start=(Dc == 0), stop=(Dc == 15)),
                         reads=[b_mT[i], b_wo[n]], writes=[PSB[bank]])
                P.dve(lambda e, n=n, bank=bank, s4=s4: e.tensor_tensor(out=x1_t[s4][:, n * 512:(n + 1) * 512], in0=psf(bank),
                                                                       in1=G1b[:, n * 512:(n + 1) * 512], op=ALU.mult),
                      reads=[b_G1b], writes=[PSB[bank], b_x1[s4]])
                P.pool(lambda e, n=n, s4=s4: e.tensor_tensor(out=x1_t[s4][:, n * 512:(n + 1) * 512], in0=x1_t[s4][:, n * 512:(n + 1) * 512],
                                                             in1=xo_t[s4][:, n * 512:(n + 1) * 512], op=ALU.add),
                       reads=[b_x1[s4], b_xo[s4]], writes=[b_x1[s4]])
            P.dma("pool", x1s[i], x1_t[s4][:], reads=[b_x1[s4]])
            norm_tile(NS4[s4], x1_t[s4][:], b_x1[s4], A2bc[:], b_A2, B2bc[:], b_B2, hb4[s4][:], b_hb4[s4])
            transpose_tile(hb4[s4][:], b_hb4[s4], h2T[:, i, :, :].rearrange("p c t -> p (c t)"), b_h2T[i], (2, 3))
            for ck in range(16):
                P.pe(lambda e, ck=ck, i=i: e.matmul(psf(6)[:, 0:36], lhsT=h2T[:, i, ck, :], rhs=wr_t[:, ck, :], start=(ck == 0), stop=(ck == 15)),
                     reads=[b_h2T[i], b_wr], writes=[PSB[6]])
            P.dve(lambda e, i=i: e.tensor_tensor(out=lgall[:, i, :], in0=psf(6)[:, 0:36], in1=brbc[:], op=ALU.add),
                  reads=[b_brbc], writes=[PSB[6], b_lgall])
        T8 = NOWN
        gl = lgall[:, :, 0:4]
        el = lgall[:, :, 4:36]
        rB = lambda c0, n: rt4[:, c0:c0 + n]

        def R3(c0, a, b_):
            return rt4[:, c0:c0 + a * b_].rearrange("p (a b) -> p a b", a=a)
        gmax, gsum, pg, m1, m2, w1, w2 = rB(0, 8), rB(8, 8), rB(16, 8), rB(24, 8), rB(32, 8), rB(40, 8), rB(48, 8)
        gd, ge, pen = R3(64, 8, 4), R3(96, 8, 4), R3(128, 8, 4)
        elm, is1, is2 = R3(160, 8, 32), R3(416, 8, 32), R3(672, 8, 32)
        D1 = lambda fn, **kw: P.dve(fn, reads=[b_rt4, b_lgall], writes=[b_rt4])
        D1(lambda e: e.tensor_reduce(out=gmax, in_=gl, axis=AX.X, op=ALU.max))
        D1(lambda e: e.tensor_tensor(out=gd, in0=gl, in1=gmax.unsqueeze(2).to_broadcast([128, T8, 4]), op=ALU.subtract))
        P.act(lambda e: e.activation(out=ge, in_=gd, func=AF.Exp), reads=[b_rt4], writes=[b_rt4])
        D1(lambda e: e.tensor_reduce(out=gsum, in_=ge, axis=AX.X, op=ALU.add))
        D1(lambda e: e.reciprocal(out=pg, in_=gsum))
        D1(lambda e: e.tensor_scalar(out=pen, in0=gd, scalar1=0.0, scalar2=-NEG, op0=ALU.is_lt, op1=ALU.mult))
        D1(lambda e: e.tensor_tensor(out=elm.rearrange("p t (g x) -> p t g x", g=4), in0=el.rearrange("p t (g x) -> p t g x", g=4),
                                     in1=pen.unsqueeze(3).to_broadcast([128, T8, 4, 8]), op=ALU.subtract))
        D1(lambda e: e.tensor_reduce(out=m1, in_=elm, axis=AX.X, op=ALU.max))
        D1(lambda e: e.tensor_tensor(out=is1, in0=elm, in1=m1.unsqueeze(2).to_broadcast([128, T8, 32]), op=ALU.is_equal))
        D1(lambda e: e.scalar_tensor_tensor(out=elm, in0=is1, scalar=NEG, in1=elm, op0=ALU.mult, op1=ALU.add))
        D1(lambda e: e.tensor_reduce(out=m2, in_=elm, axis=AX.X, op=ALU.max))
        D1(lambda e: e.tensor_tensor(out=is2, in0=elm, in1=m2.unsqueeze(2).to_broadcast([128, T8, 32]), op=ALU.is_equal))
        D1(lambda e: e.tensor_tensor(out=w2, in0=m2, in1=m1, op=ALU.subtract))
        P.act(lambda e: e.activation(out=w2, in_=w2, func=AF.Exp), reads=[b_rt4], writes=[b_rt4])
        D1(lambda e: e.tensor_scalar(out=w2, in0=w2, scalar1=1.0, scalar2=None, op0=ALU.add))
        D1(lambda e: e.reciprocal(out=w1, in_=w2))
        D1(lambda e: e.tensor_scalar(out=w2, in0=w1, scalar1=-1.0, scalar2=1.0, op0=ALU.mult, op1=ALU.add))
        D1(lambda e: e.tensor_tensor(out=w1, in0=w1, in1=pg, op=ALU.mult))
        D1(lambda e: e.tensor_tensor(out=w2, in0=w2, in1=pg, op=ALU.mult))
        D1(lambda e: e.tensor_tensor(out=is1, in0=is1, in1=w1.unsqueeze(2).to_broadcast([128, T8, 32]), op=ALU.mult))
        D1(lambda e: e.tensor_tensor(out=is2, in0=is2, in1=w2.unsqueeze(2).to_broadcast([128, T8, 32]), op=ALU.mult))
        P.dve(lambda e: e.tensor_tensor(out=comb[:], in0=is1, in1=is2, op=ALU.add), reads=[b_rt4], writes=b_comb)

    if dbg and upto == 5:
        t = dbg_tensor("x1", [NOWN, 128, D])
        final_ops.append(P.dma("sp", t, x1s, reads=[]))
        t = dbg_tensor("h2T", [128, NOWN, 16, 128], BF16)
        final_ops.append(P.dma("sp", t, h2T[:], reads=b_h2T))
        t = dbg_tensor("comb", [128, NOWN, 32])
        final_ops.append(P.dma("sp", t, comb[:], reads=b_comb))
        t = dbg_tensor("mT", [128, 16, 1024], BF16)
        final_ops.append(P.dma("sp", t, mT_t[:], reads=b_mT))

    P.barrier()
    if upto >= 6:
        A.off = R_L + 32768
        G2bc = A.alloc([128, D], F32, "G2bc")
        b_G2 = Buf()
        P.dma("sp", G2bc[:], modscr[3], writes=[b_G2])
        A.off = R_M
        Y = A.alloc([128, NOWN, D], F32, "Y")
        b_Y = [[Buf() for n in range(4)] for i in range(NOWN)]
        m_p6 = A.mark()
        ring = [A.alloc([128, 8192], BF16, f"ring{i}") for i in range(5)]
        b_ring = bufs(5, "ring")
        actT = A.alloc([128, 4, 1024], BF16, "actT")
        b_act = bufs(2, "act")
        sa6 = [A.alloc([128, 512], F32, f"sa6_{i}") for i in range(2)]
        b_sa6 = bufs(2)
        import os
        NEXP = int(os.environ.get("MK_NEXP", 32))
        n6 = 0
        nd6 = 0
        for ex in range(NEXP):
            sl = [(3 * ex + t) % 5 for t in range(3)]
            Wg = ring[sl[0]][:].rearrange("p (c f) -> p c f", c=16)
            Wu = ring[sl[1]][:].rearrange("p (c f) -> p c f", c=16)
            Wd = ring[sl[2]][:].rearrange("p (c d) -> p c d", c=4)
            P.dma("pool", Wg, w_eg[ex].rearrange("(c p) f -> p c f", p=128), writes=[b_ring[sl[0]]])
            P.dma("pool", Wu, w_eu[ex].rearrange("(c p) f -> p c f", p=128), writes=[b_ring[sl[1]]])
            P.dma("pool", Wd, w_ed[ex].rearrange("(c p) d -> p c d", p=128), writes=[b_ring[sl[2]]])
            for hf6 in range(2):
                for fc in range(4):
                    kk = n6 % 2
                    n6 += 1
                    ba, bu = kk, 2 + kk
                    for ck in range(16):
                        P.pe(lambda e, ck=ck, fc=fc, hf6=hf6, Wg=Wg, ba=ba: e.matmul(
                            psf(ba), lhsT=Wg[:, ck, fc * 128:(fc + 1) * 128], rhs=h2T[:, 4 * hf6:4 * hf6 + 4, ck, :],
                            start=(ck == 0), stop=(ck == 15)),
                            reads=[b_ring[sl[0]]] + b_h2T[4 * hf6:4 * hf6 + 4], writes=[PSB[ba]])
                    for ck in range(16):
                        P.pe(lambda e, ck=ck, fc=fc, hf6=hf6, Wu=Wu, bu=bu: e.matmul(
                            psf(bu), lhsT=Wu[:, ck, fc * 128:(fc + 1) * 128], rhs=h2T[:, 4 * hf6:4 * hf6 + 4, ck, :],
                            start=(ck == 0), stop=(ck == 15)),
                            reads=[b_ring[sl[1]]] + b_h2T[4 * hf6:4 * hf6 + 4], writes=[PSB[bu]])
                    P.act(lambda e, ba=ba, kk=kk: e.activation(out=sa6[kk][:], in_=psf(ba), func=AF.Silu), writes=[PSB[ba], b_sa6[kk]])
                    P.dve(lambda e, bu=bu, kk=kk, fc=fc, hf6=hf6: e.tensor_tensor(out=actT[:, fc, hf6 * 512:(hf6 + 1) * 512], in0=psf(bu),
                                                                                  in1=sa6[kk][:], op=ALU.mult),
                          reads=[b_sa6[kk]], writes=[PSB[bu], b_act[hf6]])
            for i in range(NOWN):
                for n in range(4):
                    bd = 4 + nd6 % 4
                    nd6 += 1
                    for fc in range(4):
                        P.pe(lambda e, fc=fc, i=i, n=n, Wd=Wd, bd=bd: e.matmul(psf(bd), lhsT=actT[:, fc, i * 128:(i + 1) * 128],
                                                                                rhs=Wd[:, fc, n * 512:(n + 1) * 512], start=(fc == 0), stop=(fc == 3)),
                             reads=[b_act[i // 4], b_ring[sl[2]]], writes=[PSB[bd]])
                    if ex == 0:
                        P.dve(lambda e, i=i, n=n, bd=bd, ex=ex: e.tensor_scalar(out=Y[:, i, n * 512:(n + 1) * 512], in0=psf(bd),
                                                                                scalar1=comb[:, i, ex:ex + 1], scalar2=None, op0=ALU.mult),
                              reads=[b_comb[i]], writes=[PSB[bd], b_Y[i][n]])
                    else:
                        P.dve(lambda e, i=i, n=n, bd=bd, ex=ex: e.scalar_tensor_tensor(out=Y[:, i, n * 512:(n + 1) * 512], in0=psf(bd),
                                                                                       scalar=comb[:, i, ex:ex + 1], in1=Y[:, i, n * 512:(n + 1) * 512],
                                                                                       op0=ALU.mult, op1=ALU.add),
                              reads=[b_comb[i], b_Y[i][n]], writes=[PSB[bd], b_Y[i][n]])
        A.release(m_p6)
        P.barrier()
        gfbc = A.alloc([128, D], F32, "gfbc")
        b_gf = Buf()
        tmp_row6 = A.alloc([1, 512], F32, "tmprow6")
        b_tmprow6 = Buf()
        bcast_rows(gfbc[:], b_gf, grows[2:3, :], D, tmp_row6, b_tmprow6, 0)
        x1l = [A.alloc([128, D], F32, f"x1l{i}") for i in range(2)]
        b_x1l = bufs(2)
        of6 = [A.alloc([128, D], F32, f"of6_{i}") for i in range(2)]
        b_of6 = bufs(2)
        junk6 = A.alloc([128, D], BF16, "junk6")
        b_junk6 = Buf()
        st6 = A.alloc([128, 8], F32, "st6")
        b_st6 = Buf()
        for i in range(NOWN):
            k6 = i % 2
            P.dma("sp", x1l[k6][:], x1s[i], writes=[b_x1l[k6]])
            P.dve(lambda e, i=i: e.tensor_tensor(out=Y[:, i, :], in0=Y[:, i, :], in1=G2bc[:], op=ALU.mult),
                  reads=[b_G2] + b_Y[i], writes=b_Y[i])
            P.pool(lambda e, i=i, k6=k6: e.tensor_tensor(out=x1l[k6][:], in0=x1l[k6][:], in1=Y[:, i, :], op=ALU.add),
                   reads=[b_x1l[k6]] + b_Y[i], writes=[b_x1l[k6]])
            P.dve(lambda e, k6=k6: e.scalar_tensor_tensor(out=junk6[:], in0=x1l[k6][:], scalar=1.0, in1=x1l[k6][:], op0=ALU.mult, op1=ALU.mult,
                                                          accum_out=st6[:, 0:1]),
                  reads=[b_x1l[k6]], writes=[b_junk6, b_st6])
            P.act(lambda e: e.activation(out=st6[:, 1:2], in_=st6[:, 0:1], func=AF.Sqrt, scale=1.0 / D, bias=EPS), reads=[b_st6], writes=[b_st6])
            P.dve(lambda e: e.reciprocal(out=st6[:, 2:3], in_=st6[:, 1:2]), reads=[b_st6], writes=[b_st6])
            P.dve(lambda e, k6=k6: e.scalar_tensor_tensor(out=of6[k6][:], in0=x1l[k6][:], scalar=st6[:, 2:3], in1=gfbc[:], op0=ALU.mult, op1=ALU.mult),
                  reads=[b_x1l[k6], b_st6, b_gf], writes=[b_of6[k6]])
            final_ops.append(P.dma("act", out_d[i * 128:(i + 1) * 128, :], of6[k6][:], reads=[b_of6[k6]]))

    if dbg and upto == 2:
        t = dbg_tensor("KT", [128, 2, NU * 128], BF16)
        final_ops.append(P.dma("sp", t, KT[:], reads=b_KT))
        t = dbg_tensor("ikT", [128, NU * 128], BF16)
        final_ops.append(P.dma("sp", t, ikT[:], reads=b_ikT))
        t = dbg_tensor("Vx", [128, NU, 2, 132], BF16)
        final_ops.append(P.dma("sp", t, Vx[:], reads=b_Vx))
        t = dbg_tensor("A1", [128, D])
        final_ops.append(P.dma("sp", t, A1[:], reads=[b_A1]))
        t = dbg_tensor("B1", [128, D])
        final_ops.append(P.dma("sp", t, B1[:], reads=[b_B1]))

    P.emit(final_wait_ops=final_ops)
    return nc, dbg_out


def _consts():
    c = np.zeros((128, 640), np.float32)
    c[:, 0:128] = np.eye(128, dtype=np.float32)
    q = np.arange(128)[:, None]
    k = np.arange(128)[None, :]
    c[:, 128:256] = np.where(k <= q, 0.0, NEG)
    s = np.arange(128)[:, None]
    t = np.arange(128)[None, :]
    c[:, 256:384] = ((t >= s) & ((t // 64) == (s // 64))).astype(np.float32)
    p = np.arange(128)
    c[:, 384] = np.power(10000.0, -(2.0 * (p % 64)) / 128.0)
    c[:, 385] = np.power(10000.0, -(2.0 * (p % 32)) / 64.0)
    c[:, 386] = np.where((p % 128) < 64, -1.0, 1.0)
    c[:, 387] = np.where((p % 64) < 32, -1.0, 1.0)
    c[:, 388:516] = 1.0
    for kk in range(NBIS):
        c[:, 516 + kk] = 2.0 ** (-kk)
    c[:, 540:604] = 1.0
    c[:, 540] = 0.0
    return c


def _swap_cols(w, nheads, hd):
    half = hd // 2
    w3 = w.reshape(w.shape[0], nheads, hd)
    return np.concatenate([w3[:, :, half:], w3[:, :, :half]], axis=2).reshape(w.shape[0], nheads * hd)


def prep_inputs(core, x, c, positions, w_ada, b_ada, g_norm1, w_in, g_head, hg_lower_bounds, w_attn_up, w_hgrn_up,
                w_out, g_norm2, w_router_group, b_router_group, w_router_expert, b_router_expert,
                w_exp_gate, w_exp_up, w_exp_down, g_final, shared):
    b, j = core // 4, core % 4
    pad = 3 - j
    xa = np.zeros((NU * 128, D), np.float32)
    nreal = (NU - pad) * 128
    xa[pad * 128:] = x[b][:nreal]
    own_rows = np.concatenate([np.arange(128 * (4 * i + j), 128 * (4 * i + j) + 128) for i in range(8)])
    xo = np.ascontiguousarray(x[b][own_rows])
    pos_pad = np.zeros((NU * 128,), np.int32)
    pos_pad[pad * 128:] = positions[b][:nreal]
    posr = np.ascontiguousarray(np.broadcast_to(pos_pad[None, :], (128, NU * 128)))
    poso = np.ascontiguousarray(np.broadcast_to(positions[b][own_rows][None, :], (128, 1024)))
    valid = np.zeros((NU * 128,), np.float32)
    valid[pad * 128:] = 1.0
    vmask = np.ascontiguousarray(valid.reshape(NU, 128).T)
    smask = np.ascontiguousarray(np.broadcast_to(np.where(valid[:512] > 0, 0.0, NEG)[None, :], (128, 512))).astype(np.float32)
    ccol = np.ascontiguousarray(c[b].reshape(16, 128).T)
    m = dict(xa=xa, xo=xo, posr=posr, poso=poso, vmask=vmask, smask=smask, ccol=ccol)
    m.update(shared)
    return m


def prep_shared(w_ada, b_ada, g_norm1, w_in, g_head, hg_lower_bounds, w_attn_up, w_hgrn_up, w_out, g_norm2,
                w_router_group, b_router_group, w_router_expert, b_router_expert, w_exp_gate, w_exp_up, w_exp_down,
                g_final):
    wi = w_in[0]
    ak = wi[:, O_AK:O_AK + 256]
    ik = wi[:, O_IK:O_IK + 64]
    ak_sw = _swap_cols(ak, 2, 128)
    ik_sw = _swap_cols(ik, 1, 64)
    w1a = np.ascontiguousarray(np.concatenate([ak, ak_sw, ik, ik, ik_sw, ik_sw], axis=1))
    aq_sw = _swap_cols(wi[:, O_AQ:O_AQ + 1024], 8, 128)
    iq_sw = _swap_cols(wi[:, O_IQ:O_IQ + 512], 8, 64)
    w2s = np.ascontiguousarray(np.concatenate([aq_sw, iq_sw], axis=1))
    grows = np.zeros((4, D), np.float32)
    grows[0] = g_norm1[0]
    grows[1] = g_norm2[0]
    grows[2] = g_final
    grows[3, :1024] = g_head[0].reshape(-1)
    lbc = np.zeros((128, 16), np.float32)
    lbc[:, 0:8] = hg_lower_bounds[0].reshape(8, 128).T
    lbc[:, 8:16] = hg_lower_bounds[1].reshape(8, 128).T
    return dict(cst=_consts(), lbc=lbc, w_ada=np.ascontiguousarray(w_ada[0]), b_ada=np.ascontiguousarray(b_ada[0:1]),
                grows=grows, w_in=np.ascontiguousarray(wi), w1a=w1a, w2s=w2s,
                w_au=np.ascontiguousarray(w_attn_up[0]), w_hu=np.ascontiguousarray(w_hgrn_up[0]),
                w_out=np.ascontiguousarray(w_out[0]),
                w_r=np.ascontiguousarray(np.concatenate([w_router_group[0], w_router_expert[0]], axis=1)),
                b_r=np.ascontiguousarray(np.concatenate([b_router_group[0], b_router_expert[0]])[None, :]),
                w_eg=np.ascontiguousarray(w_exp_gate[0]), w_eu=np.ascontiguousarray(w_exp_up[0]),
                w_ed=np.ascontiguousarray(w_exp_down[0]))


def kernel(**inputs):
    inp = {k: np.asarray(v) for k, v in inputs.items()}
    x = inp["x"]
    shared = prep_shared(**{k: inp[k] for k in inp if k not in ("x", "c", "positions")})
    nc, _ = build()
    in_maps = [prep_inputs(core, shared=shared, **inp) for core in range(8)]
    res = run_bass_kernel_spmd(nc, in_maps, core_ids=list(range(8)))
    out = np.zeros(x.shape, np.float32)
    for core in range(8):
        b, j = core // 4, core % 4
        o = res.results[core]["out"]
        for i in range(8):
            g = 4 * i + j
            out[b, 128 * g:128 * g + 128] = o[128 * i:128 * i + 128]
    return out
```

```python
import contextlib
import math
import numpy as np
import concourse.bass as bass
import concourse.mybir as mybir
from concourse.bass_utils import run_bass_kernel_spmd

F32 = mybir.dt.float32
BF16 = mybir.dt.bfloat16
I32 = mybir.dt.int32
AF = mybir.ActivationFunctionType
ALU = mybir.AluOpType
AX = mybir.AxisListType

D = 2048
NU = 32
NQ = 8
NOWN = 8
EPS = 1e-6
ATT_SCALE = 128 ** -0.5
IDX_SCALE = 512 ** -0.5
NEG = -1.0e30
NBIS = 14
COMB_OFF = 228288
TWO_PI = 2.0 * math.pi

O_AQ, O_AK, O_AV, O_IQ, O_IK, O_IW = 0, 1024, 1280, 1536, 2048, 2112
O_HQ, O_HF, O_HI, O_HOG, O_GA, O_GH = 2120, 3144, 4168, 5192, 6216, 8264


class Buf:
    __slots__ = ("name", "lw", "rd")

    def __init__(self, name=""):
        self.name = name
        self.lw = None
        self.rd = {}


def bufs(n, name=""):
    return [Buf(f"{name}{i}") for i in range(n)]


class Prog:
    STREAMS = ["pe", "act", "dve", "pool", "sp"]
    NDMASEM = 8

    def __init__(self, nc):
        self.nc = nc
        self.ops = []
        self.last = {}
        self.dma_open = []
        self.fence = {}

    def op(self, eng, fn, reads=(), writes=(), dma=False):
        idx = len(self.ops)
        deps = set()
        for b in reads:
            if b.lw is not None:
                self._dep(deps, idx, eng, dma, b.lw, "raw")
        for b in writes:
            if b.lw is not None:
                self._dep(deps, idx, eng, dma, b.lw, "waw")
            for r in b.rd.values():
                self._dep(deps, idx, eng, dma, r, "war")
        if eng in self.fence:
            deps |= self.fence.pop(eng)
        for b in reads:
            b.rd[("dma", idx) if dma else eng] = idx
        for b in writes:
            b.lw = idx
            b.rd = {}
        self.ops.append(dict(eng=eng, fn=fn, deps=deps, dma=dma, sig=False))
        if dma:
            self.dma_open.append(idx)
        else:
            self.last[eng] = idx
        return idx

    def _dep(self, deps, idx, eng, dma, pidx, kind):
        if pidx == idx:
            return
        p = self.ops[pidx]
        if (not dma) and (not p["dma"]) and p["eng"] == eng:
            if eng == "pe":
                return
        deps.add(pidx)

    def barrier(self):
        import os
        if os.environ.get("MK_NOBAR", "0") == "1":
            return
        f = set(self.last.values()) | set(self.dma_open)
        self.dma_open = []
        for s in self.STREAMS:
            self.fence[s] = set(f) | self.fence.get(s, set())

    def pe(self, fn, reads=(), writes=()):
        return self.op("pe", fn, reads, writes)

    def act(self, fn, reads=(), writes=()):
        return self.op("act", fn, reads, writes)

    def dve(self, fn, reads=(), writes=()):
        return self.op("dve", fn, reads, writes)

    def pool(self, fn, reads=(), writes=()):
        return self.op("pool", fn, reads, writes)

    def dma(self, eng, out, in_, reads=(), writes=(), **kw):
        return self.op(eng, lambda e: e.dma_start(out=out, in_=in_, **kw), reads, writes, dma=True)

    def emit(self, final_wait_ops=()):
        nc = self.nc
        ops = self.ops
        for o in ops:
            for d in o["deps"]:
                ops[d]["sig"] = True
        for i in final_wait_ops:
            ops[i]["sig"] = True
        ordn = {s: 0 for s in self.STREAMS}
        dcount = {s: 0 for s in self.STREAMS}
        for o in ops:
            s = o["eng"]
            if o["dma"]:
                k = dcount[s]
                dcount[s] += 1
                o["slot"] = k % self.NDMASEM
                o["use"] = k // self.NDMASEM + 1
            elif o["sig"]:
                ordn[s] += 1
                o["ord"] = ordn[s]
        with contextlib.ExitStack() as st:
            esem = {s: st.enter_context(nc.semaphore(f"e_{s}")) for s in self.STREAMS}
            dsem = {s: [st.enter_context(nc.semaphore(f"d_{s}{k}")) for k in range(self.NDMASEM)]
                    for s in self.STREAMS if dcount[s] > 0}
            block = st.enter_context(nc.Block())

            def target(pidx):
                p = ops[pidx]
                if p["dma"]:
                    return (dsem[p["eng"]][p["slot"]], 16 * p["use"], ("d", p["eng"], p["slot"]))
                return (esem[p["eng"]], p["ord"], ("e", p["eng"]))

            def run_stream(s, e):
                waited = {}
                for idx, o in enumerate(ops):
                    if o["eng"] != s:
                        continue
                    tg = {}
                    for d in o["deps"]:
                        sem, val, key = target(d)
                        if key not in tg or tg[key][1] < val:
                            tg[key] = (sem, val)
                    if o["dma"] and o["use"] > 1:
                        key = ("d", s, o["slot"])
                        val = 16 * (o["use"] - 1)
                        if key not in tg or tg[key][1] < val:
                            tg[key] = (dsem[s][o["slot"]], val)
                    for key, (sem, val) in tg.items():
                        if waited.get(key, 0) >= val:
                            continue
                        e.wait_ge(sem, val)
                        waited[key] = val
                    ins = o["fn"](e)
                    if o["dma"]:
                        ins.then_inc(dsem[s][o["slot"]], 16)
                    elif o["sig"]:
                        ins.then_inc(esem[s], 1)
                if s == "sp":
                    for i in final_wait_ops:
                        sem, val, key = target(i)
                        e.wait_ge(sem, val)

            @block.tensor
            def _(e):
                run_stream("pe", e)

            @block.scalar
            def _(e):
                run_stream("act", e)

            @block.vector
            def _(e):
                run_stream("dve", e)

            @block.gpsimd
            def _(e):
                run_stream("pool", e)

            @block.sync
            def _(e):
                run_stream("sp", e)


class Arena:
    BASE = 16640
    LIMIT = 228288

    def __init__(self, nc):
        self.nc = nc
        self.off = self.BASE
        self.n = 0

    def alloc(self, shape, dt, name="t"):
        esz = 4 if dt in (F32, I32) else 2
        size = esz * int(np.prod(shape[1:]))
        size = (size + 63) // 64 * 64
        t = self.nc.alloc_sbuf_tensor_at(f"a{self.n}_{name}", list(shape), dt, offset=self.off)
        self.n += 1
        self.off += size
        assert self.off <= self.LIMIT, f"SBUF arena overflow at {name}: {self.off}"
        return t

    def mark(self):
        return self.off

    def release(self, m):
        self.off = m


def build(upto=99, dbg=False):
    nc = bass.Bass("TRN2", target_bir_lowering=False)

    def din(name, shape, dt=F32):
        return nc.dram_tensor(name, list(shape), dt, kind="ExternalInput").ap()

    def dscr(name, shape, dt=F32):
        return nc.dram_tensor(name, list(shape), dt).ap()

    xa = din("xa", [NU * 128, D])
    xo = din("xo", [1024, D])
    posr = din("posr", [128, NU * 128], I32)
    poso = din("poso", [128, 1024], I32)
    vmask_d = din("vmask", [128, NU])
    smask_d = din("smask", [128, 512])
    cst_d = din("cst", [128, 640])
    ccol_d = din("ccol", [128, 16])
    lbc_d = din("lbc", [128, 16])
    w_ada = din("w_ada", [D, 6 * D])
    b_ada = din("b_ada", [1, 6 * D])
    grows = din("grows", [4, D])
    w_in = din("w_in", [D, 10312])
    w1a = din("w1a", [D, 768])
    w2s = din("w2s", [D, 1536])
    if upto >= 5:
        w_au = din("w_au", [1024, D])
        w_hu = din("w_hu", [1024, D])
        w_out = din("w_out", [D, D])
        w_r = din("w_r", [D, 36])
        b_r = din("b_r", [1, 36])
    if upto >= 6:
        w_eg = din("w_eg", [32, D, 512])
        w_eu = din("w_eu", [32, D, 512])
        w_ed = din("w_ed", [32, 512, D])
    out_d = nc.dram_tensor("out", [1024, D], F32, kind="ExternalOutput").ap()

    hTs = dscr("hTs", [NU, 128, D], BF16)
    modscr = dscr("modscr", [4, 128, D])
    x1s = dscr("x1s", [NOWN, 128, D])

    dbg_out = {}

    def dbg_tensor(name, shape, dt=F32):
        t = nc.dram_tensor("dbg_" + name, list(shape), dt, kind="ExternalOutput").ap()
        dbg_out[name] = t
        return t

    P = Prog(nc)
    A = Arena(nc)
    final_ops = []

    PS = [nc.alloc_psum_tensor(f"ps{k}", [128, 512], F32) for k in range(8)]
    PSB = bufs(8, "ps")

    def psf(k):
        return PS[k][:]

    def psb(k):
        return PS[k][:].bitcast(BF16)

    cst = A.alloc([128, 640], F32, "cst")
    b_cst = Buf("cst")
    P.dma("sp", cst[:], cst_d, writes=[b_cst])
    ident_f = cst[:, 0:128]
    cmask = cst[:, 128:256]
    tmask = cst[:, 256:384]
    inv_a = cst[:, 384:385]
    inv_i = cst[:, 385:386]
    sgn_a = cst[:, 386:387]
    sgn_i = cst[:, 387:388]
    ones_row = cst[0:1, 388:516]
    bisc = cst[:, 516:516 + NBIS]
    rmask = cst[:, 540:604]
    ident_b = A.alloc([128, 128], BF16, "identb")
    b_idb = Buf("identb")
    P.dve(lambda e: e.tensor_copy(out=ident_b[:], in_=ident_f), reads=[b_cst], writes=[b_idb])
    vmask = A.alloc([128, NU], F32, "vmask")
    b_vmask = Buf("vmask")
    P.dma("sp", vmask[:], vmask_d, writes=[b_vmask])

    def bcast_rows(dst_ap, b_dst, row_dram_ap, n, tmp_row, b_tmp, bank):
        for c0 in range(0, n, 512):
            w = min(512, n - c0)
            P.dma("sp", tmp_row[0:1, 0:w], row_dram_ap[:, c0:c0 + w], writes=[b_tmp])
            P.pe(lambda e, w=w: e.matmul(psf(bank)[:, 0:w], lhsT=ones_row, rhs=tmp_row[0:1, 0:w], start=True, stop=True),
                 reads=[b_cst, b_tmp], writes=[PSB[bank]])
            P.act(lambda e, c0=c0, w=w: e.activation(out=dst_ap[:, c0:c0 + w], in_=psf(bank)[:, 0:w], func=AF.Copy),
                  writes=[PSB[bank], b_dst])

    KT = A.alloc([128, 2, NU * 128], BF16, "KT")
    ikT = A.alloc([128, NU * 128], BF16, "ikT")
    Vx = A.alloc([128, NU, 2, 132], BF16, "Vx")
    b_KT, b_ikT, b_Vx = bufs(NQ, "KT"), bufs(NQ, "ikT"), bufs(NU, "Vx")
    m_p1 = A.mark()
    A1 = A.alloc([128, D], F32, "A1")
    B1 = A.alloc([128, D], F32, "B1")
    b_A1, b_B1 = Buf("A1"), Buf("B1")
    ccol = A.alloc([128, 16], F32, "ccol")
    csil = A.alloc([128, 16], F32, "csil")
    crep = A.alloc([128, 16, 128], BF16, "crep")
    b_ccol, b_csil, b_crep = Buf(), Buf(), Buf()
    P.dma("sp", ccol[:], ccol_d, writes=[b_ccol])
    P.act(lambda e: e.activation(out=csil[:], in_=ccol[:], func=AF.Silu), reads=[b_ccol], writes=[b_csil])
    for ck in range(16):
        P.dve(lambda e, ck=ck: e.tensor_copy(out=crep[:, ck, :], in_=csil[:, ck:ck + 1].to_broadcast([128, 128])),
              reads=[b_csil], writes=[b_crep])
    wada_t = [A.alloc([128, 16, 256], BF16, f"wada{i}") for i in range(2)]
    b_wada = bufs(2, "wada")
    brow = [A.alloc([1, 512], F32, f"brow{i}") for i in range(2)]
    b_brow = bufs(2, "brow")
    stage = [A.alloc([128, 512], F32, "stage0")] * 2
    b_stage = [Buf("stage")] * 2
    nst = [0]

    MW = 256
    NB8 = D // MW
    gsm = A.alloc([128, MW], F32, "gsm")
    b_gsm = Buf()
    mod_dma_done = set()

    def mod_dma(blk):
        if blk in mod_dma_done or blk >= 6 * NB8:
            return
        mod_dma_done.add(blk)
        s = blk % 2
        P.dma("pool", wada_t[s][:], w_ada[:, blk * MW:(blk + 1) * MW].rearrange("(c p) f -> p c f", p=128),
              writes=[b_wada[s]])

    def mod_block(blk, consume, grow=None):
        s = blk % 2
        mod_dma(blk)
        P.dma("sp", brow[s][0:1, 0:MW], b_ada[:, blk * MW:(blk + 1) * MW], writes=[b_brow[s]])
        bank = 4 + blk % 2
        for ck in range(16):
            P.pe(lambda e, ck=ck, s=s, bank=bank: e.matmul(psf(bank)[:, 0:MW], lhsT=crep[:, ck, :], rhs=wada_t[s][:, ck, :],
                                                            start=(ck == 0), stop=False),
                 reads=[b_crep, b_wada[s]], writes=[PSB[bank]])
        P.pe(lambda e, s=s, bank=bank: e.matmul(psf(bank)[:, 0:MW], lhsT=ones_row, rhs=brow[s][0:1, 0:MW], start=False, stop=True),
             reads=[b_cst, b_brow[s]], writes=[PSB[bank]])
        mod_dma(blk + 1)
        if grow is not None:
            c0 = (blk % NB8) * MW
            P.dma("sp", brow[s][0:1, MW:2 * MW], grows[grow:grow + 1, c0:c0 + MW], writes=[b_brow[s]])
            P.pe(lambda e, s=s: e.matmul(psf(3)[:, 0:MW], lhsT=ones_row, rhs=brow[s][0:1, MW:2 * MW], start=True, stop=True),
                 reads=[b_cst, b_brow[s]], writes=[PSB[3]])
            P.act(lambda e: e.activation(out=gsm[:], in_=psf(3)[:, 0:MW], func=AF.Copy), writes=[PSB[3], b_gsm])
        consume(bank)

    def to_scr(slot, c0, bank, with_g):
        k = nst[0] % 2
        nst[0] += 1
        if with_g:
            P.dve(lambda e: e.scalar_tensor_tensor(out=stage[k][:, 0:MW], in0=psf(bank)[:, 0:MW], scalar=1.0, in1=gsm[:],
                                                   op0=ALU.add, op1=ALU.mult),
                  reads=[b_gsm], writes=[PSB[bank], b_stage[k]])
        else:
            P.act(lambda e: e.activation(out=stage[k][:, 0:MW], in_=psf(bank)[:, 0:MW], func=AF.Copy), writes=[PSB[bank], b_stage[k]])
        P.dma("act", modscr[slot, :, c0:c0 + MW], stage[k][:, 0:MW], reads=[b_stage[k]])

    for q in range(NB8):
        mod_block(q, lambda bank, q=q: P.act(
            lambda e: e.activation(out=B1[:, q * MW:(q + 1) * MW], in_=psf(bank)[:, 0:MW], func=AF.Copy),
            writes=[PSB[bank], b_B1]))
    for q in range(NB8):
        mod_block(NB8 + q, lambda bank, q=q: P.dve(
            lambda e: e.scalar_tensor_tensor(out=A1[:, q * MW:(q + 1) * MW], in0=psf(bank)[:, 0:MW], scalar=1.0,
                                             in1=gsm[:], op0=ALU.add, op1=ALU.mult),
            reads=[b_gsm], writes=[PSB[bank], b_A1]), grow=0)
    pending_mod = []
    for q in range(NB8):
        pending_mod.append(lambda q=q: mod_block(2 * NB8 + q, lambda bank, q=q: to_scr(0, q * MW, bank, False)))
    for q in range(NB8):
        pending_mod.append(lambda q=q: mod_block(3 * NB8 + q, lambda bank, q=q: to_scr(2, q * MW, bank, False)))
    for q in range(NB8):
        pending_mod.append(lambda q=q: mod_block(4 * NB8 + q, lambda bank, q=q: to_scr(1, q * MW, bank, True), grow=1))
    for q in range(NB8):
        pending_mod.append(lambda q=q: mod_block(5 * NB8 + q, lambda bank, q=q: to_scr(3, q * MW, bank, False)))

    xt = [A.alloc([128, D], F32, f"xt{i}") for i in range(2)]
    b_xt = bufs(2, "xt")
    hb = [A.alloc([128, D], BF16, f"hb{i}") for i in range(4)]
    b_hb = bufs(4, "hb")
    NS0 = [dict(junk=hb[i], b_junk=b_hb[i], xn=None, b_xn=None,
                st1=A.alloc([128, 8], F32, f"st1_{i}"), b_ssq=Buf(), b_rstd=Buf()) for i in range(4)]

    def norm_tile(NS, x_ap, bx, Abc, bA, Bbc, bB, hb_ap, b_hbk):
        junk, b_junk, xn, b_xn, st1, b_ssq, b_rstd = (NS["junk"], NS["b_junk"], NS["xn"], NS["b_xn"], NS["st1"],
                                                      NS["b_ssq"], NS["b_rstd"])
        P.dve(lambda e: e.scalar_tensor_tensor(out=junk[:], in0=x_ap, scalar=1.0, in1=x_ap, op0=ALU.mult, op1=ALU.mult,
                                               accum_out=st1[:, 0:1]),
              reads=[bx], writes=[b_junk, b_ssq])
        P.act(lambda e: e.activation(out=st1[:, 1:2], in_=st1[:, 0:1], func=AF.Sqrt, scale=1.0 / D, bias=EPS),
              reads=[b_ssq], writes=[b_rstd])
        P.dve(lambda e: e.reciprocal(out=st1[:, 2:3], in_=st1[:, 1:2]), reads=[b_rstd], writes=[b_rstd])
        if xn is None:
            P.dve(lambda e: e.scalar_tensor_tensor(out=x_ap, in0=x_ap, scalar=st1[:, 2:3], in1=Abc, op0=ALU.mult, op1=ALU.mult),
                  reads=[bx, b_rstd, bA], writes=[bx])
            P.dve(lambda e: e.tensor_tensor(out=hb_ap, in0=x_ap, in1=Bbc, op=ALU.add), reads=[bx, bB], writes=[b_hbk])
        else:
            P.dve(lambda e: e.scalar_tensor_tensor(out=xn[:], in0=x_ap, scalar=st1[:, 2:3], in1=Abc, op0=ALU.mult, op1=ALU.mult),
                  reads=[bx, b_rstd, bA], writes=[b_xn])
            P.pool(lambda e: e.tensor_tensor(out=hb_ap, in0=xn[:], in1=Bbc, op=ALU.add), reads=[b_xn, bB], writes=[b_hbk])

    def transpose_tile(hb_ap, b_hbk, dst_ap, b_dst, banks):
        for half in range(2):
            bank = banks[half]
            for k in range(8):
                ck = half * 8 + k
                P.pe(lambda e, k=k, ck=ck, bank=bank: e.transpose(out=psb(bank)[:, k * 128:(k + 1) * 128],
                                                                   in_=hb_ap[:, ck * 128:(ck + 1) * 128], identity=ident_b[:]),
                     reads=[b_hbk, b_idb], writes=[PSB[bank]])
            P.act(lambda e, half=half, bank=bank: e.activation(out=dst_ap[:, half * 1024:(half + 1) * 1024], in_=psb(bank),
                                                                func=AF.Copy),
                  writes=[PSB[bank], b_dst])


    def rope_scratch():
        t0 = A.alloc([128, 512], F32, "rp_t0")
        t2 = A.alloc([128, 512], F32, "rp_t2")
        return dict(pos_i=t0[:].bitcast(I32), ang=A.alloc([128, 512], F32, "rp_ang")[:],
                    ki=t2[:].bitcast(I32), kf=t0[:],
                    r=A.alloc([128, 512], F32, "rp_r")[:], m=t2[:],
                    rc=A.alloc([128, 512], F32, "rp_rc")[:], b=Buf("rp"))

    def rope_tables(RS, pos_dram_ap, inv_col, sgn_col, cos_ap, sin_ap, b_tab):
        rp_pos_i, rp_ang, rp_ki, rp_kf, rp_r, rp_m, rp_rc, b_rp = (RS["pos_i"], RS["ang"], RS["ki"], RS["kf"], RS["r"],
                                                                    RS["m"], RS["rc"], RS["b"])
        P.dma("sp", rp_pos_i, pos_dram_ap, writes=[b_rp])
        P.dve(lambda e: e.tensor_copy(out=rp_ang, in_=rp_pos_i), reads=[b_rp], writes=[b_rp])
        P.dve(lambda e: e.tensor_scalar(out=rp_ang, in0=rp_ang, scalar1=inv_col, scalar2=None, op0=ALU.mult),
              reads=[b_rp, b_cst], writes=[b_rp])
        P.dve(lambda e: e.tensor_scalar(out=rp_ki, in0=rp_ang, scalar1=1.0 / TWO_PI, scalar2=None, op0=ALU.mult),
              reads=[b_rp], writes=[b_rp])
        P.dve(lambda e: e.tensor_copy(out=rp_kf, in_=rp_ki), reads=[b_rp], writes=[b_rp])
        P.dve(lambda e: e.scalar_tensor_tensor(out=rp_r, in0=rp_kf, scalar=-TWO_PI, in1=rp_ang, op0=ALU.mult, op1=ALU.add),
              reads=[b_rp], writes=[b_rp])
        P.dve(lambda e: e.tensor_scalar(out=rp_m, in0=rp_r, scalar1=math.pi, scalar2=-TWO_PI, op0=ALU.is_gt, op1=ALU.mult),
              reads=[b_rp], writes=[b_rp])
        P.dve(lambda e: e.tensor_tensor(out=rp_r, in0=rp_r, in1=rp_m, op=ALU.add), reads=[b_rp], writes=[b_rp])
        P.dve(lambda e: e.tensor_scalar(out=rp_m, in0=rp_r, scalar1=-math.pi, scalar2=TWO_PI, op0=ALU.is_lt, op1=ALU.mult),
              reads=[b_rp], writes=[b_rp])
        P.dve(lambda e: e.tensor_tensor(out=rp_r, in0=rp_r, in1=rp_m, op=ALU.add), reads=[b_rp], writes=[b_rp])
        P.dve(lambda e: e.tensor_scalar(out=rp_m, in0=rp_r, scalar1=math.pi / 2, scalar2=-TWO_PI, op0=ALU.is_gt, op1=ALU.mult),
              reads=[b_rp], writes=[b_rp])
        P.dve(lambda e: e.scalar_tensor_tensor(out=rp_rc, in0=rp_r, scalar=math.pi / 2, in1=rp_m, op0=ALU.add, op1=ALU.add),
              reads=[b_rp], writes=[b_rp])
        P.act(lambda e: e.activation(out=sin_ap, in_=rp_r, func=AF.Sin), reads=[b_rp], writes=[b_tab])
        P.act(lambda e: e.activation(out=cos_ap, in_=rp_rc, func=AF.Sin), reads=[b_rp], writes=[b_tab])
        P.dve(lambda e: e.tensor_scalar(out=sin_ap, in0=sin_ap, scalar1=sgn_col, scalar2=None, op0=ALU.mult),
              reads=[b_tab, b_cst], writes=[b_tab])

    def rope_evac(dst_ap, b_dst, bank_a, bank_s, cos_ap, sin_ap, b_tab, t1, t2, b_t):
        P.dve(lambda e: e.tensor_tensor(out=t1, in0=psf(bank_a), in1=cos_ap, op=ALU.mult), reads=[b_tab], writes=[PSB[bank_a], b_t[0]])
        P.dve(lambda e: e.tensor_tensor(out=t2, in0=psf(bank_s), in1=sin_ap, op=ALU.mult), reads=[b_tab], writes=[PSB[bank_s], b_t[1]])
        P.dve(lambda e: e.tensor_tensor(out=dst_ap, in0=t1, in1=t2, op=ALU.add), reads=[b_t[0], b_t[1]], writes=[b_dst])

    if upto >= 2:
        RS1 = rope_scratch()
        w1a_t = A.alloc([128, 16, 768], BF16, "w1a")
        wv_t = A.alloc([128, 16, 256], BF16, "wv")
        b_w1a, b_wv = Buf(), Buf()
        P.dma("pool", w1a_t[:], w1a.rearrange("(c p) f -> p c f", p=128), writes=[b_w1a])
        P.dma("pool", wv_t[:], w_in[:, O_AV:O_AV + 256].rearrange("(c p) f -> p c f", p=128), writes=[b_wv])
        hTq = [A.alloc([128, 4, 16, 128], BF16, f"hTq{i}") for i in range(2)]
        b_hTq4 = [bufs(4, f"hTq{i}_") for i in range(2)]
        tabs = [A.alloc([128, 512], F32, f"tab{i}") for i in range(4)]
        b_tabs = [Buf("tab_a"), Buf("tab_i")]
        rt = [A.alloc([128, 512], F32, f"rt{i}") for i in range(2)] * 2
        b_rt = bufs(2) * 2
        def norm_quad(qq):
            for r in range(4):
                u = 4 * qq + r
                k_ = u % 2
                P.dma("sp", xt[k_][:], xa[u * 128:(u + 1) * 128, :], writes=[b_xt[k_]])
                norm_tile(NS0[r], xt[k_][:], b_xt[k_], A1[:], b_A1, B1[:], b_B1, hb[r][:], b_hb[r])

        def trans_quad(qq):
            for r in range(4):
                u = 4 * qq + r
                transpose_tile(hb[r][:], b_hb[r], hTq[qq % 2][:, r, :, :].rearrange("p c t -> p (c t)"), b_hTq4[qq % 2][r], (6, 7))
                P.dma("act", hTs[u], hTq[qq % 2][:, r, :, :].rearrange("p c t -> p (c t)"), reads=[b_hTq4[qq % 2][r]])

        def norm_one(qq, r):
            u = 4 * qq + r
            k_ = u % 2
            P.dma("sp", xt[k_][:], xa[u * 128:(u + 1) * 128, :], writes=[b_xt[k_]])
            norm_tile(NS0[r], xt[k_][:], b_xt[k_], A1[:], b_A1, B1[:], b_B1, hb[r][:], b_hb[r])

        def projA(q, s, gi):
            ca, cs = [(0, 2), (1, 3), (4, 5)][gi]
            banks = (2 * (gi % 2), 2 * (gi % 2) + 1)
            for bi, cc in enumerate((ca, cs)):
                for ck in range(16):
                    P.pe(lambda e, ck=ck, cc=cc, bank=banks[bi]: e.matmul(
                        psf(bank), lhsT=w1a_t[:, ck, cc * 128:(cc + 1) * 128], rhs=hTq[s][:, :, ck, :],
                        start=(ck == 0), stop=(ck == 15)),
                        reads=[b_w1a] + b_hTq4[s], writes=[PSB[banks[bi]]])

        def evacA(q, gi):
            banks = (2 * (gi % 2), 2 * (gi % 2) + 1)
            k2 = 2 * (gi % 2)
            if gi < 2:
                rope_evac(KT[:, gi, q * 512:(q + 1) * 512], b_KT[q], banks[0], banks[1], tabs[0][:], tabs[1][:], b_tabs[0],
                          rt[k2][:], rt[k2 + 1][:], (b_rt[k2], b_rt[k2 + 1]))
            else:
                rope_evac(ikT[:, q * 512:(q + 1) * 512], b_ikT[q], banks[0], banks[1], tabs[2][:], tabs[3][:], b_tabs[1],
                          rt[k2][:], rt[k2 + 1][:], (b_rt[k2], b_rt[k2 + 1]))

        def projV(q, s):
            for r in range(4):
                u = 4 * q + r
                bank = 4 + (u % 2)
                for ck in range(16):
                    P.pe(lambda e, ck=ck, r=r, bank=bank: e.matmul(psf(bank)[:, 0:256], lhsT=hTq[s][:, r, ck, :], rhs=wv_t[:, ck, :],
                                                                    start=(ck == 0), stop=(ck == 15)),
                         reads=[b_wv, b_hTq4[s][r]], writes=[PSB[bank]])
                P.act(lambda e, u=u, bank=bank: e.activation(out=Vx[:, u, :, 0:128],
                                                             in_=psf(bank)[:, 0:256].rearrange("p (g d) -> p g d", g=2),
                                                             func=AF.Copy, scale=vmask[:, u:u + 1]),
                      reads=[b_vmask], writes=[PSB[bank], b_Vx[u]])
                P.pool(lambda e, u=u: e.tensor_copy(out=Vx[:, u, :, 128:129],
                                                    in_=vmask[:, u:u + 1].unsqueeze(1).to_broadcast([128, 2, 1])),
                       reads=[b_vmask], writes=[b_Vx[u]])

        norm_quad(0)
        trans_quad(0)
        for q in range(NQ):
            s = q % 2
            nxt = q + 1 < NQ
            rope_tables(RS1, posr[:, q * 512:(q + 1) * 512], inv_a, sgn_a, tabs[0][:], tabs[1][:], b_tabs[0])
            rope_tables(RS1, posr[:, q * 512:(q + 1) * 512], inv_i, sgn_i, tabs[2][:], tabs[3][:], b_tabs[1])
            projA(q, s, 0)
            if nxt:
                norm_one(q + 1, 0)
            projA(q, s, 1)
            evacA(q, 0)
            if nxt:
                norm_one(q + 1, 1)
            projA(q, s, 2)
            evacA(q, 1)
            if nxt:
                norm_one(q + 1, 2)
            projV(q, s)
            evacA(q, 2)
            if nxt:
                norm_one(q + 1, 3)
            for _ in range(5):
                if pending_mod:
                    pending_mod.pop(0)()
            if nxt:
                trans_quad(q + 1)

    while pending_mod:
        pending_mod.pop(0)()

    A.release(m_p1)
    P.barrier()
    o_n = A.alloc([128, NOWN, 1024], BF16, "o_n")
    b_on = bufs(NOWN, "o_n")
    m_p1b = A.mark()
    if upto >= 3:
        ghbc = A.alloc([128, 1024], F32, "ghbc")
        b_ghbc = Buf()
        tmp_row2 = A.alloc([1, 512], F32, "tmprow2")
        b_tmprow2 = Buf()
        import os
        _skip = os.environ.get("MK_SKIP", "")
        if "g" not in _skip:
            bcast_rows(ghbc[:], b_ghbc, grows[3:4, :], 1024, tmp_row2, b_tmprow2, 7)
        lbt = A.alloc([128, 40], F32, "lbt")
        b_lbt = Buf()
        P.dma("sp", lbt[:, 0:16], lbc_d, writes=[b_lbt])
        P.dve(lambda e: e.tensor_tensor(out=lbt[:, 16:24], in0=lbt[:, 0:8], in1=lbt[:, 8:16], op=ALU.subtract),
              reads=[b_lbt], writes=[b_lbt])
        P.act(lambda e: e.activation(out=lbt[:, 16:24], in_=lbt[:, 16:24], func=AF.Sigmoid), reads=[b_lbt], writes=[b_lbt])
        P.dve(lambda e: e.tensor_scalar(out=lbt[:, 24:32], in0=lbt[:, 16:24], scalar1=-1.0, scalar2=1.0, op0=ALU.mult, op1=ALU.add),
              reads=[b_lbt], writes=[b_lbt])
        P.dve(lambda e: e.tensor_scalar(out=lbt[:, 32:40], in0=lbt[:, 16:24], scalar1=-1.0, scalar2=None, op0=ALU.add),
              reads=[b_lbt], writes=[b_lbt])
        rm512 = A.alloc([128, 8, 64], F32, "rm512")
        b_rm = Buf()
        P.dve(lambda e: e.tensor_copy(out=rm512[:], in_=rmask.unsqueeze(1).to_broadcast([128, 8, 64])), reads=[b_cst], writes=[b_rm])
        whf = A.alloc([128, 16, 512], BF16, "whf")
        whi = A.alloc([128, 16, 512], BF16, "whi")
        whq = A.alloc([128, 16, 512], BF16, "whq")
        b_whf, b_whi, b_whq = Buf(), Buf(), Buf()
        hTqB = [A.alloc([128, 4, 16, 128], BF16, f"hTqb{i}") for i in range(2)]
        b_hTqB = bufs(2)
        bA = [A.alloc([128, 512], F32, f"hgA{i}") for i in range(4)]
        bK = [A.alloc([128, 512], F32, f"hgK{i}") for i in range(4)]
        bC = [A.alloc([128, 512], F32, f"hgC{i}") for i in range(4)]
        b_bA, b_bK, b_bC = bufs(4), bufs(4), bufs(4)
        kdT = A.alloc([128, 4, 512], BF16, "kdT")
        b_kdT = bufs(4)
        dec = A.alloc([128, 4, 8], F32, "dec")
        b_dec = Buf()
        ep = A.alloc([128, 4, 128], F32, "ep")
        b_ep = Buf()
        v_t = [A.alloc([128, 512], BF16, f"v_t{i}") for i in range(2)]
        b_vt = bufs(2)
        kd_t = [A.alloc([128, 4, 128], BF16, f"kd_t{i}") for i in range(2)]
        b_kdt = bufs(2)
        qs = A.alloc([128, 4, 128], F32, "qs")
        b_qs = Buf()
        qdT = A.alloc([128, 4, 128], BF16, "qdT")
        q0 = A.alloc([128, 4, 128], BF16, "q0")
        q1 = A.alloc([128, 4, 128], BF16, "q1")
        b_qd, b_q0, b_q1 = Buf(), Buf(), Buf()
        attm = A.alloc([128, 4, 128], BF16, "attm")
        b_attm = Buf()
        Sst = A.alloc([128, 4, 128], F32, "Sst")
        b_S = Buf()
        Sbf = [A.alloc([128, 4, 128], BF16, f"Sbf{i}") for i in range(2)]
        b_Sbf = bufs(2)
        tmpU = A.alloc([128, 4, 128], F32, "tmpU")
        b_tmpU = Buf()
        o_sb = A.alloc([128, 4, 128], F32, "o_sb")
        b_osb = Buf()
        junk4 = A.alloc([128, 128], BF16, "junk4")
        b_junk4 = Buf()
        st4 = A.alloc([128, 12], F32, "st4")
        b_st4 = Buf()
        if "m" not in _skip:
            P.pool(lambda e: e.memset(q0[:], 0.0), writes=[b_q0])
            P.pool(lambda e: e.memset(q1[:], 0.0), writes=[b_q1])
        import os
        for hg in range(int(os.environ.get("MK_NHG", 2))):
            P.dma("pool", whf[:], w_in[:, O_HF + hg * 512:O_HF + (hg + 1) * 512].rearrange("(c p) f -> p c f", p=128), writes=[b_whf])
            P.dma("pool", whi[:], w_in[:, O_HI + hg * 512:O_HI + (hg + 1) * 512].rearrange("(c p) f -> p c f", p=128), writes=[b_whi])
            P.dma("pool", whq[:], w_in[:, O_HQ + hg * 512:O_HQ + (hg + 1) * 512].rearrange("(c p) f -> p c f", p=128), writes=[b_whq])
            P.pool(lambda e: e.memset(Sst[:], 0.0), writes=[b_S])
            for q in range(int(os.environ.get("MK_NQ1B", NQ))):
                s = q % 2
                for r in range(4):
                    P.dma("sp", hTqB[s][:, r, :, :], hTs[4 * q + r].rearrange("p (c t) -> p c t", c=16), writes=[b_hTqB[s]])
                for h in range(4):
                    for ck in range(16):
                        P.pe(lambda e, ck=ck, h=h, s=s: e.matmul(psf(h), lhsT=whf[:, ck, h * 128:(h + 1) * 128], rhs=hTqB[s][:, :, ck, :],
                                                                  start=(ck == 0), stop=(ck == 15)),
                             reads=[b_whf, b_hTqB[s]], writes=[PSB[h]])
                for h in range(4):
                    P.act(lambda e, h=h: e.activation(out=bA[h][:], in_=psf(h), func=AF.Sigmoid), writes=[PSB[h], b_bA[h]])
                for h in range(4):
                    H = 4 * hg + h
                    P.dve(lambda e, h=h, H=H: e.tensor_scalar(out=bK[h][:], in0=bA[h][:], scalar1=-1.0, scalar2=lbt[:, 32 + H:33 + H],
                                                               op0=ALU.add, op1=ALU.mult),
                          reads=[b_bA[h], b_lbt], writes=[b_bK[h]])
                for h in range(4):
                    H = 4 * hg + h
                    P.act(lambda e, h=h, H=H: e.activation(out=bA[h][:], in_=bA[h][:], func=AF.Ln, scale=lbt[:, 24 + H:25 + H],
                                                            bias=lbt[:, 16 + H:17 + H]),
                          reads=[b_bA[h], b_lbt], writes=[b_bA[h]])
                for h in range(4):
                    P.dve(lambda e, h=h: e.tensor_tensor_scan(out=bC[h][:], data0=rm512[:].rearrange("p c t -> p (c t)"), data1=bA[h][:],
                                                              initial=0.0, op0=ALU.mult, op1=ALU.add),
                          reads=[b_bA[h], b_rm], writes=[b_bC[h]])
                for h in range(4):
                    P.act(lambda e, h=h: e.activation(out=bA[h][:], in_=bC[h][:], func=AF.Exp, scale=-1.0), reads=[b_bC[h]], writes=[b_bA[h]])
                for h in range(4):
                    P.act(lambda e, h=h: e.activation(out=dec[:, h, :], in_=bC[h][:].rearrange("p (c t) -> p c t", t=64)[:, :, 63],
                                                      func=AF.Exp),
                          reads=[b_bC[h]], writes=[b_dec])
                for h in range(4):
                    P.act(lambda e, h=h: e.activation(out=ep[:, h, :], in_=bC[h][:, 384:512], func=AF.Exp), reads=[b_bC[h]], writes=[b_ep])
                for h in range(4):
                    P.dve(lambda e, h=h: e.tensor_tensor(out=kdT[:, h, :], in0=bK[h][:], in1=bA[h][:], op=ALU.mult),
                          reads=[b_bK[h], b_bA[h]], writes=[b_kdT[h]])
                for r in range(4):
                    u = 4 * q + r
                    k2 = u % 2
                    own = (r == 3)
                    for ck in range(16):
                        P.pe(lambda e, ck=ck, r=r, s=s: e.matmul(psf(4), lhsT=hTqB[s][:, r, ck, :], rhs=whi[:, ck, :],
                                                                  start=(ck == 0), stop=(ck == 15)),
                             reads=[b_whi, b_hTqB[s]], writes=[PSB[4]])
                    P.act(lambda e, u=u, k2=k2: e.activation(out=v_t[k2][:], in_=psf(4), func=AF.Copy, scale=vmask[:, u:u + 1]),
                          reads=[b_vmask], writes=[PSB[4], b_vt[k2]])
                    for h in range(4):
                        P.pe(lambda e, h=h, r=r: e.transpose(out=psb(5)[:, h * 128:(h + 1) * 128], in_=kdT[:, h, r * 128:(r + 1) * 128],
                                                             identity=ident_b[:]),
                             reads=[b_kdT[h], b_idb], writes=[PSB[5]])
                    P.dve(lambda e, k2=k2: e.tensor_copy(out=kd_t[k2][:].rearrange("p h d -> p (h d)"), in_=psb(5)[:, 0:512]),
                          writes=[PSB[5], b_kdt[k2]])
                    if own:
                        i = q
                        for h in range(4):
                            for ck in range(16):
                                P.pe(lambda e, ck=ck, h=h, s=s: e.matmul(psf(7)[:, h * 128:(h + 1) * 128], lhsT=whq[:, ck, h * 128:(h + 1) * 128],
                                                                          rhs=hTqB[s][:, 3, ck, :], start=(ck == 0), stop=(ck == 15)),
                                     reads=[b_whq, b_hTqB[s]], writes=[PSB[7]])
                        P.act(lambda e: e.activation(out=qs[:].rearrange("p h d -> p (h d)"), in_=psf(7), func=AF.Silu),
                              writes=[PSB[7], b_qs])
                        P.dve(lambda e: e.tensor_tensor(out=qdT[:], in0=qs[:], in1=ep[:], op=ALU.mult), reads=[b_qs, b_ep], writes=[b_qd])
                        P.pool(lambda e: e.tensor_copy(out=q0[:, :, 0:64], in_=qdT[:, :, 0:64]), reads=[b_qd], writes=[b_q0])
                        P.pool(lambda e: e.tensor_copy(out=q1[:, :, 64:128], in_=qdT[:, :, 64:128]), reads=[b_qd], writes=[b_q1])
                        for h in range(4):
                            P.pe(lambda e, h=h: e.matmul(psf(7)[:, h * 128:(h + 1) * 128], lhsT=kdT[:, h, 384:512], rhs=qdT[:, h, :],
                                                         start=True, stop=True),
                                 reads=[b_kdT[h], b_qd], writes=[PSB[7]])
                        P.dve(lambda e: e.tensor_tensor(out=attm[:], in0=psf(7).rearrange("p (h d) -> p h d", h=4),
                                                        in1=tmask.unsqueeze(1).to_broadcast([128, 4, 128]), op=ALU.mult),
                              reads=[b_cst], writes=[PSB[7], b_attm])
                    for c in range(2):
                        if own:
                            P.act(lambda e, c=c: e.activation(out=Sbf[c][:], in_=Sst[:], func=AF.Copy), reads=[b_S], writes=[b_Sbf[c]])
                        for h in range(4):
                            P.pe(lambda e, h=h, c=c, k2=k2: e.matmul(psf(6)[:, h * 128:(h + 1) * 128], lhsT=kd_t[k2][64 * c:64 * c + 64, h, :],
                                                                      rhs=v_t[k2][64 * c:64 * c + 64, h * 128:(h + 1) * 128], start=True, stop=True),
                                 reads=[b_kdt[k2], b_vt[k2]], writes=[PSB[6]])
                        cq = 2 * r + c
                        P.dve(lambda e: e.tensor_tensor(out=tmpU[:], in0=psf(6).rearrange("p (h d) -> p h d", h=4), in1=Sst[:], op=ALU.add),
                              reads=[b_S], writes=[PSB[6], b_tmpU])
                        P.dve(lambda e, cq=cq: e.tensor_tensor(out=Sst[:], in0=tmpU[:], in1=dec[:, :, cq:cq + 1].to_broadcast([128, 4, 128]),
                                                               op=ALU.mult),
                              reads=[b_tmpU, b_dec], writes=[b_S])
                    if own:
                        for h in range(4):
                            P.pe(lambda e, h=h, k2=k2: e.matmul(psf(7)[:, h * 128:(h + 1) * 128], lhsT=attm[:, h, :],
                                                                rhs=v_t[k2][:, h * 128:(h + 1) * 128], start=True, stop=False),
                                 reads=[b_attm, b_vt[k2]], writes=[PSB[7]])
                            P.pe(lambda e, h=h: e.matmul(psf(7)[:, h * 128:(h + 1) * 128], lhsT=q0[:, h, :], rhs=Sbf[0][:, h, :],
                                                         start=False, stop=False),
                                 reads=[b_q0, b_Sbf[0]], writes=[PSB[7]])
                            P.pe(lambda e, h=h: e.matmul(psf(7)[:, h * 128:(h + 1) * 128], lhsT=q1[:, h, :], rhs=Sbf[1][:, h, :],
                                                         start=False, stop=True),
                                 reads=[b_q1, b_Sbf[1]], writes=[PSB[7]])
                        P.act(lambda e: e.activation(out=o_sb[:].rearrange("p h d -> p (h d)"), in_=psf(7), func=AF.Copy),
                              writes=[PSB[7], b_osb])
                        for h in range(4):
                            P.dve(lambda e, h=h: e.scalar_tensor_tensor(out=junk4[:], in0=o_sb[:, h, :], scalar=1.0, in1=o_sb[:, h, :],
                                                                        op0=ALU.mult, op1=ALU.mult, accum_out=st4[:, h:h + 1]),
                                  reads=[b_osb], writes=[b_junk4, b_st4])
                        P.act(lambda e: e.activation(out=st4[:, 4:8], in_=st4[:, 0:4], func=AF.Sqrt, scale=1.0 / 128, bias=EPS),
                              reads=[b_st4], writes=[b_st4])
                        P.dve(lambda e: e.reciprocal(out=st4[:, 8:12], in_=st4[:, 4:8]), reads=[b_st4], writes=[b_st4])
                        for h in range(4):
                            H = 4 * hg + h
                            P.dve(lambda e, h=h, H=H, i=i: e.scalar_tensor_tensor(out=o_n[:, i, H * 128:(H + 1) * 128], in0=o_sb[:, h, :],
                                                                                   scalar=st4[:, 8 + h:9 + h], in1=ghbc[:, H * 128:(H + 1) * 128],
                                                                                   op0=ALU.mult, op1=ALU.mult),
                                  reads=[b_osb, b_st4, b_ghbc], writes=[b_on[i]])

    if dbg and upto == 3:
        t = dbg_tensor("o_n", [128, NOWN, 1024], BF16)
        final_ops.append(P.dma("sp", t, o_n[:], reads=b_on))
        t = dbg_tensor("ikT", [128, NU * 128], BF16)
        final_ops.append(P.dma("sp", t, ikT[:], reads=b_ikT))
        t = dbg_tensor("KT", [128, 2, NU * 128], BF16)
        final_ops.append(P.dma("sp", t, KT[:], reads=b_KT))

    A.release(m_p1b)
    P.barrier()
    y_att = A.alloc([128, NOWN, 1024], BF16, "y_att")
    b_yatt = bufs(NOWN, "yatt")
    m_p2 = A.mark()
    if upto >= 4:
        QT = A.alloc([128, 8, 1024], BF16, "QT")
        iqT = A.alloc([128, 4, 1024], BF16, "iqT")
        b_QT, b_iqT = bufs(8, "QT"), bufs(4, "iqT")
        iwt = A.alloc([128, NOWN, 24], F32, "iwt")
        b_iwt = bufs(NOWN, "iwt")
        smask_t = A.alloc([128, 512], F32, "smask")
        b_smask = Buf()
        P.dma("sp", smask_t[:], smask_d, writes=[b_smask])
        m_p2a = A.mark()
        hTo = A.alloc([128, NOWN, 16, 128], BF16, "hTo")
        b_hTo = bufs(NOWN, "hTo")
        for i in range(NOWN):
            P.dma("sp", hTo[:, i, :, :], hTs[4 * i + 3].rearrange("p (c t) -> p c t", c=16), writes=[b_hTo[i]])
        RS2 = rope_scratch()
        otab = [A.alloc([128, 1024], F32, f"otab{i}") for i in range(4)]
        b_otab = [Buf("otab_a"), Buf("otab_i")]
        for hf2 in range(2):
            rope_tables(RS2, poso[:, hf2 * 512:(hf2 + 1) * 512], inv_a, sgn_a, otab[0][:, hf2 * 512:(hf2 + 1) * 512],
                        otab[1][:, hf2 * 512:(hf2 + 1) * 512], b_otab[0])
            rope_tables(RS2, poso[:, hf2 * 512:(hf2 + 1) * 512], inv_i, sgn_i, otab[2][:, hf2 * 512:(hf2 + 1) * 512],
                        otab[3][:, hf2 * 512:(hf2 + 1) * 512], b_otab[1])
        wiw = A.alloc([128, 16, 8], BF16, "wiw")
        b_wiw = Buf()
        P.dma("pool", wiw[:], w_in[:, O_IW:O_IW + 8].rearrange("(c p) f -> p c f", p=128), writes=[b_wiw])
        for i in range(NOWN):
            for ck in range(16):
                P.pe(lambda e, ck=ck, i=i: e.matmul(psf(7)[:, 0:8], lhsT=hTo[:, i, ck, :], rhs=wiw[:, ck, :], start=(ck == 0), stop=(ck == 15)),
                     reads=[b_hTo[i], b_wiw], writes=[PSB[7]])
            P.act(lambda e, i=i: e.activation(out=iwt[:, i, 0:8], in_=psf(7)[:, 0:8], func=AF.Copy), writes=[PSB[7], b_iwt[i]])
            P.act(lambda e, i=i: e.activation(out=iwt[:, i, 8:16], in_=iwt[:, i, 0:8], func=AF.Abs, scale=IDX_SCALE),
                  reads=[b_iwt[i]], writes=[b_iwt[i]])
            P.dve(lambda e, i=i: e.tensor_scalar(out=iwt[:, i, 16:24], in0=iwt[:, i, 0:8], scalar1=0.0, scalar2=2.0,
                                                 op0=ALU.is_ge, op1=ALU.mult), reads=[b_iwt[i]], writes=[b_iwt[i]])
            P.dve(lambda e, i=i: e.tensor_scalar(out=iwt[:, i, 16:24], in0=iwt[:, i, 16:24], scalar1=-1.0, scalar2=None,
                                                 op0=ALU.add), reads=[b_iwt[i]], writes=[b_iwt[i]])
        wq = [[A.alloc([128, 16, 256], BF16, f"wq{k}_{t}") for t in range(2)] for k in range(2)]
        b_wq = [[Buf(), Buf()] for k in range(2)]
        rt2 = [A.alloc([128, 512], F32, f"rt2_{i}") for i in range(4)]
        b_rt2 = bufs(4)
        groups = [("q", g) for g in range(4)] + [("i", g) for g in range(2)]
        cnt = 0
        for gi, (kind, g) in enumerate(groups):
            k = gi % 2
            if kind == "q":
                src_a = w_in[:, O_AQ + g * 256:O_AQ + (g + 1) * 256]
                src_s = w2s[:, g * 256:(g + 1) * 256]
            else:
                src_a = w_in[:, O_IQ + g * 256:O_IQ + (g + 1) * 256]
                src_s = w2s[:, 1024 + g * 256:1024 + (g + 1) * 256]
            P.dma("pool", wq[k][0][:], src_a.rearrange("(c p) f -> p c f", p=128), writes=[b_wq[k][0]])
            P.dma("pool", wq[k][1][:], src_s.rearrange("(c p) f -> p c f", p=128), writes=[b_wq[k][1]])
            for cc in range(2):
                for hf2 in range(2):
                    banks = (2 * (cnt % 2), 2 * (cnt % 2) + 1)
                    k2 = 2 * (cnt % 2)
                    cnt += 1
                    for t in range(2):
                        for ck in range(16):
                            P.pe(lambda e, ck=ck, cc=cc, hf2=hf2, t=t, k=k, bank=banks[t]: e.matmul(
                                psf(bank), lhsT=wq[k][t][:, ck, cc * 128:(cc + 1) * 128],
                                rhs=hTo[:, 4 * hf2:4 * hf2 + 4, ck, :], start=(ck == 0), stop=(ck == 15)),
                                reads=[b_wq[k][t]] + b_hTo[4 * hf2:4 * hf2 + 4], writes=[PSB[banks[t]]])
                    if kind == "q":
                        hd = 2 * g + cc
                        rope_evac(QT[:, hd, hf2 * 512:(hf2 + 1) * 512], b_QT[hd], banks[0], banks[1],
                                  otab[0][:, hf2 * 512:(hf2 + 1) * 512], otab[1][:, hf2 * 512:(hf2 + 1) * 512], b_otab[0],
                                  rt2[k2][:], rt2[k2 + 1][:], (b_rt2[k2], b_rt2[k2 + 1]))
                    else:
                        chn = 2 * g + cc
                        rope_evac(iqT[:, chn, hf2 * 512:(hf2 + 1) * 512], b_iqT[chn], banks[0], banks[1],
                                  otab[2][:, hf2 * 512:(hf2 + 1) * 512], otab[3][:, hf2 * 512:(hf2 + 1) * 512], b_otab[1],
                                  rt2[k2][:], rt2[k2 + 1][:], (b_rt2[k2], b_rt2[k2 + 1]))
        A.release(m_p2a)
        P.barrier()
        scoreL = [A.alloc([128, 4096], F32, f"score{i}") for i in range(2)]
        b_scoreL = bufs(2, "score")
        mask01L = [A.alloc([128, 4096], BF16, f"mask01_{i}") for i in range(2)]
        b_mask01L = bufs(2, "mask01")
        maskTL = [A.alloc([128, 32, 128], BF16, f"maskT{i}") for i in range(2)]
        b_maskTL = bufs(2, "maskT")
        bsL = [A.alloc([128, 8 + NBIS], F32, f"bs{i}") for i in range(2)]
        b_bsL = bufs(2, "bs")
        sacc = [A.alloc([128, 512], F32, f"sacc{i}") for i in range(2)]
        b_sacc = bufs(2)
        rl = [A.alloc([128, 512], F32, f"rl{i}") for i in range(2)]
        b_rl = bufs(2)
        Et = [A.alloc([128, 4, 128], BF16, f"Et{i}") for i in range(2)]
        b_Et = bufs(2)
        PT = [A.alloc([128, 4, 128], BF16, f"PT{i}") for i in range(2)]
        b_PT = bufs(2)
        rc = A.alloc([128, 4], F32, "rc")
        b_rc = Buf()
        cnts = dict(ndot=0, nst=0)

        def indexer(i):
            score, b_score, bs, b_bs = scoreL[i % 2], b_scoreL[i % 2], bsL[i % 2], b_bsL[i % 2]
            nk = 512 * (i + 1)
            for kq in range(i + 1):
                for h in range(8):
                    bank = cnts["ndot"] % 2
                    k = cnts["ndot"] % 2
                    cnts["ndot"] += 1
                    pb = (h % 2) * 64
                    P.pe(lambda e, h=h, kq=kq, bank=bank, pb=pb: e.matmul(
                        psf(bank), lhsT=iqT[pb:pb + 64, h // 2, i * 128:(i + 1) * 128], rhs=ikT[pb:pb + 64, kq * 512:(kq + 1) * 512],
                        start=True, stop=True),
                        reads=[b_iqT[h // 2], b_ikT[kq]], writes=[PSB[bank]])
                    P.act(lambda e, h=h, bank=bank, k=k: e.activation(out=rl[k][:], in_=psf(bank), func=AF.Relu,
                                                                       scale=iwt[:, i, 8 + h:9 + h]),
                          reads=[b_iwt[i]], writes=[PSB[bank], b_rl[k]])
                    dst = score[:, kq * 512:(kq + 1) * 512] if h == 7 else sacc[h % 2][:]
                    b_dst = b_score if h == 7 else b_sacc[h % 2]
                    if h == 0:
                        P.dve(lambda e, k=k, dst=dst: e.tensor_scalar(out=dst, in0=rl[k][:], scalar1=iwt[:, i, 16:17], scalar2=None,
                                                                       op0=ALU.mult),
                              reads=[b_rl[k], b_iwt[i]], writes=[b_dst])
                    else:
                        P.dve(lambda e, k=k, h=h, dst=dst: e.scalar_tensor_tensor(
                            out=dst, in0=rl[k][:], scalar=iwt[:, i, 16 + h:17 + h], in1=sacc[(h - 1) % 2][:], op0=ALU.mult, op1=ALU.add),
                            reads=[b_rl[k], b_iwt[i], b_sacc[(h - 1) % 2]], writes=[b_dst])
            P.dve(lambda e: e.tensor_reduce(out=bs[:, 0:1], in_=score[:, 0:nk], axis=AX.X, op=ALU.max, apply_absolute_value=True),
                  reads=[b_score], writes=[b_bs])
            P.dve(lambda e: e.tensor_tensor(out=score[:, nk - 128:nk], in0=score[:, nk - 128:nk], in1=cmask, op=ALU.add),
                  reads=[b_score, b_cst], writes=[b_score])
            P.dve(lambda e: e.tensor_tensor(out=score[:, 0:512], in0=score[:, 0:512], in1=smask_t[:], op=ALU.add),
                  reads=[b_score, b_smask], writes=[b_score])
            P.dve(lambda e: e.tensor_scalar(out=bs[:, 8:8 + NBIS], in0=bisc, scalar1=bs[:, 0:1], scalar2=None, op0=ALU.mult),
                  reads=[b_bs, b_cst], writes=[b_bs])
            P.dve(lambda e: e.tensor_scalar(out=bs[:, 1:2], in0=bs[:, 0:1], scalar1=-1.0, scalar2=None, op0=ALU.mult),
                  reads=[b_bs], writes=[b_bs])

        def bis_step(i, k, step):
            score, b_score, bs, b_bs = scoreL[i % 2], b_scoreL[i % 2], bsL[i % 2], b_bsL[i % 2]
            mask01, b_mask01 = mask01L[i % 2], b_mask01L[i % 2]
            nk = 512 * (i + 1)
            if step == 0:
                P.dve(lambda e: e.tensor_tensor(out=bs[:, 2:3], in0=bs[:, 1:2], in1=bs[:, 8 + k:9 + k], op=ALU.add),
                      reads=[b_bs], writes=[b_bs])
            elif step == 1:
                P.dve(lambda e: e.tensor_scalar(out=mask01[:, 0:nk], in0=score[:, 0:nk], scalar1=bs[:, 2:3], scalar2=0.0,
                                                op0=ALU.is_ge, op1=ALU.add, accum_out=bs[:, 3:4]),
                      reads=[b_score, b_bs], writes=[b_mask01, b_bs])
            elif step == 2:
                P.dve(lambda e: e.tensor_scalar(out=bs[:, 4:5], in0=bs[:, 3:4], scalar1=256.0, scalar2=bs[:, 8 + k:9 + k],
                                                op0=ALU.is_ge, op1=ALU.mult),
                      reads=[b_bs], writes=[b_bs])
            else:
                P.dve(lambda e: e.tensor_tensor(out=bs[:, 1:2], in0=bs[:, 1:2], in1=bs[:, 4:5], op=ALU.add), reads=[b_bs], writes=[b_bs])

        def make_mask(i):
            score, b_score, bs, b_bs = scoreL[i % 2], b_scoreL[i % 2], bsL[i % 2], b_bsL[i % 2]
            mask01, b_mask01 = mask01L[i % 2], b_mask01L[i % 2]
            maskT, b_maskT = maskTL[i % 2], b_maskTL[i % 2]
            nk = 512 * (i + 1)
            nkt = 4 * (i + 1)
            P.dve(lambda e: e.tensor_scalar(out=mask01[:, 0:nk], in0=score[:, 0:nk], scalar1=bs[:, 1:2], scalar2=None, op0=ALU.is_ge),
                  reads=[b_score, b_bs], writes=[b_mask01])
            for k0 in range(0, nkt, 8):
                n = min(8, nkt - k0)
                for kk in range(n):
                    kt = k0 + kk
                    P.pe(lambda e, kk=kk, kt=kt: e.transpose(out=psb(2)[:, kk * 128:(kk + 1) * 128], in_=mask01[:, kt * 128:(kt + 1) * 128],
                                                             identity=ident_b[:]),
                         reads=[b_mask01, b_idb], writes=[PSB[2]])
                P.act(lambda e, k0=k0, n=n: e.activation(out=maskT[:, k0:k0 + n, :].rearrange("p k q -> p (k q)"),
                                                         in_=psb(2)[:, 0:n * 128], func=AF.Copy),
                      writes=[PSB[2], b_maskT])

        lnr = A.alloc([128, 8], F32, "lnr")
        b_lnr = Buf()
        Et2 = [A.alloc([128, 2, 128], BF16, f"Et2_{i}") for i in range(3)]
        b_Et2 = bufs(3)
        PT2 = [A.alloc([128, 2, 128], BF16, f"PT2_{i}") for i in range(3)]
        b_PT2 = bufs(3)

        def attention(i, use_dve=False):
            maskT, b_maskT = maskTL[i % 2], b_maskTL[i % 2]
            nkt = 4 * (i + 1)
            for hp in range(4):
                g = hp // 2
                accb = (4, 5) if hp % 2 == 0 else (7, 2)
                steps = []
                for kt in range(nkt):
                    sb_ = (3, 6)[cnts["nst"] % 2]
                    k = cnts["nst"] % 3
                    cnts["nst"] += 1
                    steps.append((kt, sb_, k))
                for j in range(nkt + 2):
                    if j < nkt:
                        kt, sb_, k = steps[j]
                        P.pe(lambda e, kt=kt, g=g, hp=hp, sb_=sb_: e.matmul(psf(sb_)[:, 0:256], lhsT=KT[:, g, kt * 128:(kt + 1) * 128],
                                                                             rhs=QT[:, 2 * hp:2 * hp + 2, i * 128:(i + 1) * 128],
                                                                             start=True, stop=True),
                             reads=[b_KT[kt // 4]] + b_QT[2 * hp:2 * hp + 2], writes=[PSB[sb_]])
                        P.act(lambda e, sb_=sb_, k=k: e.activation(out=Et2[k][:].rearrange("p h q -> p (h q)"), in_=psf(sb_)[:, 0:256],
                                                                   func=AF.Exp, scale=ATT_SCALE),
                              writes=[PSB[sb_], b_Et2[k]])
                        P.op("dve" if (use_dve and kt % 2 == 1) else "pool",
                             lambda e, kt=kt, k=k: e.tensor_tensor(out=PT2[k][:], in0=Et2[k][:],
                                                                   in1=maskT[:, kt, :].unsqueeze(1).to_broadcast([128, 2, 128]), op=ALU.mult),
                             reads=[b_Et2[k], b_maskT], writes=[b_PT2[k]])
                    if j >= 2:
                        kt, sb_, k = steps[j - 2]
                        for hh in range(2):
                            P.pe(lambda e, hh=hh, kt=kt, g=g, k=k, ab=accb[hh]: e.matmul(
                                psf(ab)[:, 0:129], lhsT=PT2[k][:, hh, :], rhs=Vx[:, kt, g, 0:129], start=(kt == 0), stop=(kt == nkt - 1)),
                                reads=[b_PT2[k], b_Vx[kt]], writes=[PSB[accb[hh]]])
                for hh in range(2):
                    hd = 2 * hp + hh
                    ab = accb[hh]
                    P.act(lambda e, ab=ab, hd=hd: e.activation(out=lnr[:, hd:hd + 1], in_=psf(ab)[:, 128:129], func=AF.Ln),
                          writes=[PSB[ab], b_lnr])
                    P.act(lambda e, hd=hd: e.activation(out=lnr[:, hd:hd + 1], in_=lnr[:, hd:hd + 1], func=AF.Exp, scale=-1.0),
                          reads=[b_lnr], writes=[b_lnr])
                    P.act(lambda e, ab=ab, hd=hd: e.activation(out=y_att[:, i, hd * 128:(hd + 1) * 128], in_=psf(ab)[:, 0:128],
                                                               func=AF.Copy, scale=lnr[:, hd:hd + 1]),
                          reads=[b_lnr], writes=[PSB[ab], b_yatt[i]])

        NP2 = NOWN // 2
        for pr in range(NP2 + 1):
            if pr < NP2:
                for i in (2 * pr, 2 * pr + 1):
                    indexer(i)
            if pr >= 1:
                for i in (2 * pr - 2, 2 * pr - 1):
                    attention(i, use_dve=(pr == NP2))
            if pr < NP2:
                tiles = (2 * pr, 2 * pr + 1)
                for k in range(NBIS):
                    for step in range(4):
                        for i in tiles:
                            bis_step(i, k, step)
                for i in tiles:
                    make_mask(i)

    if dbg and upto == 4:
        t = dbg_tensor("y_att", [128, NOWN, 1024], BF16)
        final_ops.append(P.dma("sp", t, y_att[:], reads=b_yatt))
        if upto >= 4:
            t = dbg_tensor("ikT", [128, NU * 128], BF16)
            final_ops.append(P.dma("sp", t, ikT[:], reads=b_ikT))
            t = dbg_tensor("rl0", [128, 512])
            final_ops.append(P.dma("sp", t, rl[0][:], reads=[b_rl[0]]))
            t = dbg_tensor("rl1", [128, 512])
            final_ops.append(P.dma("sp", t, rl[1][:], reads=[b_rl[1]]))
            t = dbg_tensor("sacc0", [128, 512])
            final_ops.append(P.dma("sp", t, sacc[0][:], reads=[b_sacc[0]]))
            t = dbg_tensor("score", [128, 4096])
            final_ops.append(P.dma("sp", t, scoreL[1][:], reads=[b_scoreL[1]]))
            t = dbg_tensor("bs", [128, 8 + NBIS])
            final_ops.append(P.dma("sp", t, bsL[1][:], reads=[b_bsL[1]]))
            t = dbg_tensor("mask01", [128, 4096], BF16)
            final_ops.append(P.dma("sp", t, mask01L[1][:], reads=[b_mask01L[1]]))
            t = dbg_tensor("maskT", [128, 32, 128], BF16)
            final_ops.append(P.dma("sp", t, maskTL[1][:], reads=[b_maskTL[1]]))
            t = dbg_tensor("iwt", [128, NOWN, 24])
            final_ops.append(P.dma("sp", t, iwt[:], reads=b_iwt))
            t = dbg_tensor("QT", [128, 8, 1024], BF16)
            final_ops.append(P.dma("sp", t, QT[:], reads=b_QT))
            t = dbg_tensor("iqT", [128, 4, 1024], BF16)
            final_ops.append(P.dma("sp", t, iqT[:], reads=b_iqT))

    P.barrier()
    R_L, R_M, R_H = 19584, 61056, 93824
    h2T = None
    comb = None
    if upto >= 5:
        A.off = R_L
        yaT = A.alloc([128, 8, 1024], BF16, "yaT")
        yhT = A.alloc([128, 8, 1024], BF16, "yhT")
        G1bc = A.alloc([128, D], F32, "G1bc")
        b_yaT, b_yhT, b_G1 = bufs(NOWN, "yaT"), Buf("yhT"), Buf("G1")
        assert A.off <= R_M
        A.off = R_H
        onT = A.alloc([128, 8, 1024], BF16, "onT")
        b_onT = bufs(NOWN, "onT")
        hTo4 = A.alloc([128, NOWN, 16, 128], BF16, "hTo4")
        b_hTo4 = bufs(NOWN, "hTo4")
        whog = [A.alloc([128, 16, 256], BF16, f"whog{i}") for i in range(2)]
        b_whog = bufs(2)
        sil4 = [A.alloc([128, 512], F32, f"sil4_{i}") for i in range(2)]
        b_sil4 = bufs(2)
        wg4 = [dict(ga=A.alloc([128, 16, 256], BF16, f"wga{i}"), gh=A.alloc([128, 16, 256], BF16, f"wgh{i}"),
                    au=A.alloc([128, 8, 256], BF16, f"wau{i}"), hu=A.alloc([128, 8, 256], BF16, f"whu{i}")) for i in range(2)]
        b_wg4 = [dict(ga=Buf(), gh=Buf(), au=Buf(), hu=Buf()) for i in range(2)]
        sg4 = [sil4[0], sil4[1]] + [A.alloc([128, 512], F32, f"sg4_{i}") for i in range(2)]
        b_sg4 = bufs(4)
        mm4 = [A.alloc([128, 512], F32, f"mm4_{i}") for i in range(4)]
        b_mm4 = bufs(4)
        P.dma("sp", G1bc[:], modscr[0], writes=[b_G1])
        for i in range(NOWN):
            P.dma("sp", hTo4[:, i, :, :], hTs[4 * i + 3].rearrange("p (c t) -> p c t", c=16), writes=[b_hTo4[i]])
        for i in range(NOWN):
            for (src, dstT, b_src, b_dstT, bank) in ((y_att, yaT, b_yatt, b_yaT, 0), (o_n, onT, b_on, b_onT, 1)):
                for c in range(8):
                    P.pe(lambda e, c=c, i=i, src=src, bank=bank: e.transpose(out=psb(bank)[:, c * 128:(c + 1) * 128],
                                                                             in_=src[:, i, c * 128:(c + 1) * 128], identity=ident_b[:]),
                         reads=[b_src[i], b_idb], writes=[PSB[bank]])
                P.act(lambda e, i=i, dstT=dstT, bank=bank: e.activation(out=dstT[:, :, i * 128:(i + 1) * 128],
                                                                        in_=psb(bank).rearrange("p (c t) -> p c t", c=8), func=AF.Copy),
                      writes=[PSB[bank], b_dstT[i]])
        n4 = 0
        for g4 in range(4):
            k4 = g4 % 2
            P.dma("pool", whog[k4][:], w_in[:, O_HOG + g4 * 256:O_HOG + (g4 + 1) * 256].rearrange("(c p) f -> p c f", p=128),
                  writes=[b_whog[k4]])
            for cc in range(2):
                chn = 2 * g4 + cc
                for hf4 in range(2):
                    bank = 2 + n4 % 2
                    kk = n4 % 2
                    n4 += 1
                    for ck in range(16):
                        P.pe(lambda e, ck=ck, cc=cc, hf4=hf4, k4=k4, bank=bank: e.matmul(
                            psf(bank), lhsT=whog[k4][:, ck, cc * 128:(cc + 1) * 128], rhs=hTo4[:, 4 * hf4:4 * hf4 + 4, ck, :],
                            start=(ck == 0), stop=(ck == 15)),
                            reads=[b_whog[k4]] + b_hTo4[4 * hf4:4 * hf4 + 4], writes=[PSB[bank]])
                    P.act(lambda e, bank=bank, kk=kk: e.activation(out=sil4[kk][:], in_=psf(bank), func=AF.Silu),
                          writes=[PSB[bank], b_sil4[kk]])
                    P.dve(lambda e, kk=kk, chn=chn, hf4=hf4: e.tensor_tensor(out=yhT[:, chn, hf4 * 512:(hf4 + 1) * 512], in0=sil4[kk][:],
                                                                             in1=onT[:, chn, hf4 * 512:(hf4 + 1) * 512], op=ALU.mult),
                          reads=[b_sil4[kk]] + b_onT[4 * hf4:4 * hf4 + 4], writes=[b_yhT])
        P.barrier()
        mT_t = nc.alloc_sbuf_tensor_at("mergedT", [128, 16, 1024], BF16, offset=R_M)
        b_mT = bufs(NOWN, "mT")
        n4 = 0
        for g4 in range(8):
            k4 = g4 % 2
            W = wg4[k4]
            BW = b_wg4[k4]
            P.dma("pool", W["ga"][:], w_in[:, O_GA + g4 * 256:O_GA + (g4 + 1) * 256].rearrange("(c p) f -> p c f", p=128), writes=[BW["ga"]])
            P.dma("pool", W["gh"][:], w_in[:, O_GH + g4 * 256:O_GH + (g4 + 1) * 256].rearrange("(c p) f -> p c f", p=128), writes=[BW["gh"]])
            P.dma("pool", W["au"][:], w_au[:, g4 * 256:(g4 + 1) * 256].rearrange("(c p) f -> p c f", p=128), writes=[BW["au"]])
            P.dma("pool", W["hu"][:], w_hu[:, g4 * 256:(g4 + 1) * 256].rearrange("(c p) f -> p c f", p=128), writes=[BW["hu"]])
            for cc in range(2):
                Dc = 2 * g4 + cc
                for hf4 in range(2):
                    kk = n4 % 2
                    n4 += 1
                    bk = [4 * kk + t for t in range(4)]
                    for ck in range(16):
                        P.pe(lambda e, ck=ck, cc=cc, hf4=hf4, W=W, bank=bk[0]: e.matmul(
                            psf(bank), lhsT=W["ga"][:, ck, cc * 128:(cc + 1) * 128], rhs=hTo4[:, 4 * hf4:4 * hf4 + 4, ck, :],
                            start=(ck == 0), stop=(ck == 15)),
                            reads=[BW["ga"]] + b_hTo4[4 * hf4:4 * hf4 + 4], writes=[PSB[bk[0]]])
                    for ck in range(16):
                        P.pe(lambda e, ck=ck, cc=cc, hf4=hf4, W=W, bank=bk[1]: e.matmul(
                            psf(bank), lhsT=W["gh"][:, ck, cc * 128:(cc + 1) * 128], rhs=hTo4[:, 4 * hf4:4 * hf4 + 4, ck, :],
                            start=(ck == 0), stop=(ck == 15)),
                            reads=[BW["gh"]] + b_hTo4[4 * hf4:4 * hf4 + 4], writes=[PSB[bk[1]]])
                    for fc in range(8):
                        P.pe(lambda e, fc=fc, cc=cc, hf4=hf4, W=W, bank=bk[2]: e.matmul(
                            psf(bank), lhsT=W["au"][:, fc, cc * 128:(cc + 1) * 128], rhs=yaT[:, fc, hf4 * 512:(hf4 + 1) * 512],
                            start=(fc == 0), stop=(fc == 7)),
                            reads=[BW["au"]] + b_yaT[4 * hf4:4 * hf4 + 4], writes=[PSB[bk[2]]])
                    for fc in range(8):
                        P.pe(lambda e, fc=fc, cc=cc, hf4=hf4, W=W, bank=bk[3]: e.matmul(
                            psf(bank), lhsT=W["hu"][:, fc, cc * 128:(cc + 1) * 128], rhs=yhT[:, fc, hf4 * 512:(hf4 + 1) * 512],
                            start=(fc == 0), stop=(fc == 7)),
                            reads=[BW["hu"], b_yhT], writes=[PSB[bk[3]]])
                    P.act(lambda e, kk=kk, bank=bk[0]: e.activation(out=sg4[2 * kk][:], in_=psf(bank), func=AF.Sigmoid),
                          writes=[PSB[bk[0]], b_sg4[2 * kk]])
                    P.act(lambda e, kk=kk, bank=bk[1]: e.activation(out=sg4[2 * kk + 1][:], in_=psf(bank), func=AF.Sigmoid),
                          writes=[PSB[bk[1]], b_sg4[2 * kk + 1]])
                    P.dve(lambda e, kk=kk, bank=bk[2]: e.tensor_tensor(out=mm4[2 * kk][:], in0=psf(bank), in1=sg4[2 * kk][:], op=ALU.mult),
                          reads=[b_sg4[2 * kk]], writes=[PSB[bk[2]], b_mm4[2 * kk]])
                    P.dve(lambda e, kk=kk, bank=bk[3]: e.tensor_tensor(out=mm4[2 * kk + 1][:], in0=psf(bank), in1=sg4[2 * kk + 1][:], op=ALU.mult),
                          reads=[b_sg4[2 * kk + 1]], writes=[PSB[bk[3]], b_mm4[2 * kk + 1]])
                    P.pool(lambda e, kk=kk, Dc=Dc, hf4=hf4: e.tensor_tensor(out=mT_t[:, Dc, hf4 * 512:(hf4 + 1) * 512], in0=mm4[2 * kk][:],
                                                                            in1=mm4[2 * kk + 1][:], op=ALU.add),
                           reads=[b_mm4[2 * kk], b_mm4[2 * kk + 1]], writes=b_mT[4 * hf4:4 * hf4 + 4])
        P.barrier()
        A.off = R_L
        h2T = A.alloc([128, NOWN, 16, 128], BF16, "h2T")
        b_h2T = bufs(NOWN, "h2T")
        G1b = A.alloc([128, D], F32, "G1b")
        b_G1b = Buf()
        assert A.off <= R_M
        A.off = R_H
        wo_t = A.alloc([128, 16, D], BF16, "wo_t")
        b_wo = bufs(4, "wo")
        for n in range(4):
            P.dma("pool", wo_t[:, :, n * 512:(n + 1) * 512], w_out[:, n * 512:(n + 1) * 512].rearrange("(c p) f -> p c f", p=128),
                  writes=[b_wo[n]])
        P.dma("sp", G1b[:], modscr[0], writes=[b_G1b])
        A2bc = A.alloc([128, D], F32, "A2bc")
        B2bc = A.alloc([128, D], F32, "B2bc")
        b_A2, b_B2 = Buf(), Buf()
        P.dma("sp", A2bc[:], modscr[1], writes=[b_A2])
        P.dma("sp", B2bc[:], modscr[2], writes=[b_B2])
        xo_t = [A.alloc([128, D], F32, "xo_t0")] * 2
        b_xo = [Buf("xo")] * 2
        x1_t = [A.alloc([128, D], F32, f"x1_t{i}") for i in range(2)]
        b_x1 = bufs(2)
        hb4 = [A.alloc([128, D], BF16, f"hb4_{i}") for i in range(2)]
        b_hb4 = bufs(2)
        NS4c = dict(xn=A.alloc([128, D], F32, "xn4"), b_xn=Buf(), st1=A.alloc([128, 8], F32, "st14"), b_ssq=Buf(), b_rstd=Buf())
        NS4 = [dict(junk=hb4[k_], b_junk=b_hb4[k_], **NS4c) for k_ in range(2)]
        wr_t = A.alloc([128, 16, 36], BF16, "wr_t")
        b_wr = Buf()
        P.dma("pool", wr_t[:], w_r.rearrange("(c p) f -> p c f", p=128), writes=[b_wr])
        brbc = A.alloc([128, 36], F32, "brbc")
        b_brbc = Buf()
        tmp_row4 = A.alloc([1, 512], F32, "tmprow4")
        b_tmprow4 = Buf()
        bcast_rows(brbc[:], b_brbc, b_r, 36, tmp_row4, b_tmprow4, 7)
        comb = nc.alloc_sbuf_tensor_at("comb", [128, NOWN, 32], F32, offset=COMB_OFF)
        b_comb = bufs(NOWN, "comb")
        rt4 = A.alloc([128, 928], F32, "rt4")
        b_rt4 = Buf()
        lgall = A.alloc([128, NOWN, 36], F32, "lgall")
        b_lgall = Buf()
        n4 = 0
        for i in range(NOWN):
            s4 = i % 2
            P.dma("sp", xo_t[s4][:], xo[i * 128:(i + 1) * 128, :], writes=[b_xo[s4]])
            for n in range(4):
                bank = n4 % 2
                kk = n4 % 2
                n4 += 1
                for Dc in range(16):
                    P.pe(lambda e, Dc=Dc, i=i, n=n, bank=bank: e.matmul(psf(bank), lhsT=mT_t[:, Dc, i * 128:(i + 1) * 128],
                                                                         rhs=wo_t[:, Dc, n * 512:(n + 1) * 512], start=(Dc == 0), stop=(Dc == 15)),
                         reads=[b_mT[i], b_wo[n]], writes=[PSB[bank]])
                P.dve(lambda e, n=n, bank=bank, s4=s4: e.tensor_tensor(out=x1_t[s4][:, n * 512:(n + 1) * 512], in0=psf(bank),
                                                                       in1=G1b[:, n * 512:(n + 1) * 512], op=ALU.mult),
                      reads=[b_G1b], writes=[PSB[bank], b_x1[s4]])
                P.pool(lambda e, n=n, s4=s4: e.tensor_tensor(out=x1_t[s4][:, n * 512:(n + 1) * 512], in0=x1_t[s4][:, n * 512:(n + 1) * 512],
                                                             in1=xo_t[s4][:, n * 512:(n + 1) * 512], op=ALU.add),
                       reads=[b_x1[s4], b_xo[s4]], writes=[b_x1[s4]])
            P.dma("pool", x1s[i], x1_t[s4][:], reads=[b_x1[s4]])
            norm_tile(NS4[s4], x1_t[s4][:], b_x1[s4], A2bc[:], b_A2, B2bc[:], b_B2, hb4[s4][:], b_hb4[s4])
            transpose_tile(hb4[s4][:], b_hb4[s4], h2T[:, i, :, :].rearrange("p c t -> p (c t)"), b_h2T[i], (2, 3))
            for ck in range(16):
                P.pe(lambda e, ck=ck, i=i: e.matmul(psf(6)[:, 0:36], lhsT=h2T[:, i, ck, :], rhs=wr_t[:, ck, :], start=(ck == 0), stop=(ck == 15)),
                     reads=[b_h2T[i], b_wr], writes=[PSB[6]])
            P.dve(lambda e, i=i: e.tensor_tensor(out=lgall[:, i, :], in0=psf(6)[:, 0:36], in1=brbc[:], op=ALU.add),
                  reads=[b_brbc], writes=[PSB[6], b_lgall])
        T8 = NOWN
        gl = lgall[:, :, 0:4]
        el = lgall[:, :, 4:36]
        rB = lambda c0, n: rt4[:, c0:c0 + n]

        def R3(c0, a, b_):
            return rt4[:, c0:c0 + a * b_].rearrange("p (a b) -> p a b", a=a)
        gmax, gsum, pg, m1, m2, w1, w2 = rB(0, 8), rB(8, 8), rB(16, 8), rB(24, 8), rB(32, 8), rB(40, 8), rB(48, 8)
        gd, ge, pen = R3(64, 8, 4), R3(96, 8, 4), R3(128, 8, 4)
        elm, is1, is2 = R3(160, 8, 32), R3(416, 8, 32), R3(672, 8, 32)
        D1 = lambda fn, **kw: P.dve(fn, reads=[b_rt4, b_lgall], writes=[b_rt4])
        D1(lambda e: e.tensor_reduce(out=gmax, in_=gl, axis=AX.X, op=ALU.max))
        D1(lambda e: e.tensor_tensor(out=gd, in0=gl, in1=gmax.unsqueeze(2).to_broadcast([128, T8, 4]), op=ALU.subtract))
        P.act(lambda e: e.activation(out=ge, in_=gd, func=AF.Exp), reads=[b_rt4], writes=[b_rt4])
        D1(lambda e: e.tensor_reduce(out=gsum, in_=ge, axis=AX.X, op=ALU.add))
        D1(lambda e: e.reciprocal(out=pg, in_=gsum))
        D1(lambda e: e.tensor_scalar(out=pen, in0=gd, scalar1=0.0, scalar2=-NEG, op0=ALU.is_lt, op1=ALU.mult))
        D1(lambda e: e.tensor_tensor(out=elm.rearrange("p t (g x) -> p t g x", g=4), in0=el.rearrange("p t (g x) -> p t g x", g=4),
                                     in1=pen.unsqueeze(3).to_broadcast([128, T8, 4, 8]), op=ALU.subtract))
        D1(lambda e: e.tensor_reduce(out=m1, in_=elm, axis=AX.X, op=ALU.max))
        D1(lambda e: e.tensor_tensor(out=is1, in0=elm, in1=m1.unsqueeze(2).to_broadcast([128, T8, 32]), op=ALU.is_equal))
        D1(lambda e: e.scalar_tensor_tensor(out=elm, in0=is1, scalar=NEG, in1=elm, op0=ALU.mult, op1=ALU.add))
        D1(lambda e: e.tensor_reduce(out=m2, in_=elm, axis=AX.X, op=ALU.max))
        D1(lambda e: e.tensor_tensor(out=is2, in0=elm, in1=m2.unsqueeze(2).to_broadcast([128, T8, 32]), op=ALU.is_equal))
        D1(lambda e: e.tensor_tensor(out=w2, in0=m2, in1=m1, op=ALU.subtract))
        P.act(lambda e: e.activation(out=w2, in_=w2, func=AF.Exp), reads=[b_rt4], writes=[b_rt4])
        D1(lambda e: e.tensor_scalar(out=w2, in0=w2, scalar1=1.0, scalar2=None, op0=ALU.add))
        D1(lambda e: e.reciprocal(out=w1, in_=w2))
        D1(lambda e: e.tensor_scalar(out=w2, in0=w1, scalar1=-1.0, scalar2=1.0, op0=ALU.mult, op1=ALU.add))
        D1(lambda e: e.tensor_tensor(out=w1, in0=w1, in1=pg, op=ALU.mult))
        D1(lambda e: e.tensor_tensor(out=w2, in0=w2, in1=pg, op=ALU.mult))
        D1(lambda e: e.tensor_tensor(out=is1, in0=is1, in1=w1.unsqueeze(2).to_broadcast([128, T8, 32]), op=ALU.mult))
        D1(lambda e: e.tensor_tensor(out=is2, in0=is2, in1=w2.unsqueeze(2).to_broadcast([128, T8, 32]), op=ALU.mult))
        P.dve(lambda e: e.tensor_tensor(out=comb[:], in0=is1, in1=is2, op=ALU.add), reads=[b_rt4], writes=b_comb)

    if dbg and upto == 5:
        t = dbg_tensor("x1", [NOWN, 128, D])
        final_ops.append(P.dma("sp", t, x1s, reads=[]))
        t = dbg_tensor("h2T", [128, NOWN, 16, 128], BF16)
        final_ops.append(P.dma("sp", t, h2T[:], reads=b_h2T))
        t = dbg_tensor("comb", [128, NOWN, 32])
        final_ops.append(P.dma("sp", t, comb[:], reads=b_comb))
        t = dbg_tensor("mT", [128, 16, 1024], BF16)
        final_ops.append(P.dma("sp", t, mT_t[:], reads=b_mT))

    P.barrier()
    if upto >= 6:
        A.off = R_L + 32768
        G2bc = A.alloc([128, D], F32, "G2bc")
        b_G2 = Buf()
        P.dma("sp", G2bc[:], modscr[3], writes=[b_G2])
        A.off = R_M
        Y = A.alloc([128, NOWN, D], F32, "Y")
        b_Y = [[Buf() for n in range(4)] for i in range(NOWN)]
        m_p6 = A.mark()
        ring = [A.alloc([128, 8192], BF16, f"ring{i}") for i in range(5)]
        b_ring = bufs(5, "ring")
        actT = A.alloc([128, 4, 1024], BF16, "actT")
        b_act = bufs(2, "act")
        sa6 = [A.alloc([128, 512], F32, f"sa6_{i}") for i in range(2)]
        b_sa6 = bufs(2)
        import os
        NEXP = int(os.environ.get("MK_NEXP", 32))
        n6 = 0
        nd6 = 0
        for ex in range(NEXP):
            sl = [(3 * ex + t) % 5 for t in range(3)]
            Wg = ring[sl[0]][:].rearrange("p (c f) -> p c f", c=16)
            Wu = ring[sl[1]][:].rearrange("p (c f) -> p c f", c=16)
            Wd = ring[sl[2]][:].rearrange("p (c d) -> p c d", c=4)
            P.dma("pool", Wg, w_eg[ex].rearrange("(c p) f -> p c f", p=128), writes=[b_ring[sl[0]]])
            P.dma("pool", Wu, w_eu[ex].rearrange("(c p) f -> p c f", p=128), writes=[b_ring[sl[1]]])
            P.dma("pool", Wd, w_ed[ex].rearrange("(c p) d -> p c d", p=128), writes=[b_ring[sl[2]]])
            for hf6 in range(2):
                for fc in range(4):
                    kk = n6 % 2
                    n6 += 1
                    ba, bu = kk, 2 + kk
                    for ck in range(16):
                        P.pe(lambda e, ck=ck, fc=fc, hf6=hf6, Wg=Wg, ba=ba: e.matmul(
                            psf(ba), lhsT=Wg[:, ck, fc * 128:(fc + 1) * 128], rhs=h2T[:, 4 * hf6:4 * hf6 + 4, ck, :],
                            start=(ck == 0), stop=(ck == 15)),
                            reads=[b_ring[sl[0]]] + b_h2T[4 * hf6:4 * hf6 + 4], writes=[PSB[ba]])
                    for ck in range(16):
                        P.pe(lambda e, ck=ck, fc=fc, hf6=hf6, Wu=Wu, bu=bu: e.matmul(
                            psf(bu), lhsT=Wu[:, ck, fc * 128:(fc + 1) * 128], rhs=h2T[:, 4 * hf6:4 * hf6 + 4, ck, :],
                            start=(ck == 0), stop=(ck == 15)),
                            reads=[b_ring[sl[1]]] + b_h2T[4 * hf6:4 * hf6 + 4], writes=[PSB[bu]])
                    P.act(lambda e, ba=ba, kk=kk: e.activation(out=sa6[kk][:], in_=psf(ba), func=AF.Silu), writes=[PSB[ba], b_sa6[kk]])
                    P.dve(lambda e, bu=bu, kk=kk, fc=fc, hf6=hf6: e.tensor_tensor(out=actT[:, fc, hf6 * 512:(hf6 + 1) * 512], in0=psf(bu),
                                                                                  in1=sa6[kk][:], op=ALU.mult),
                          reads=[b_sa6[kk]], writes=[PSB[bu], b_act[hf6]])
            for i in range(NOWN):
                for n in range(4):
                    bd = 4 + nd6 % 4
                    nd6 += 1
                    for fc in range(4):
                        P.pe(lambda e, fc=fc, i=i, n=n, Wd=Wd, bd=bd: e.matmul(psf(bd), lhsT=actT[:, fc, i * 128:(i + 1) * 128],
                                                                                rhs=Wd[:, fc, n * 512:(n + 1) * 512], start=(fc == 0), stop=(fc == 3)),
                             reads=[b_act[i // 4], b_ring[sl[2]]], writes=[PSB[bd]])
                    if ex == 0:
                        P.dve(lambda e, i=i, n=n, bd=bd, ex=ex: e.tensor_scalar(out=Y[:, i, n * 512:(n + 1) * 512], in0=psf(bd),
                                                                                scalar1=comb[:, i, ex:ex + 1], scalar2=None, op0=ALU.mult),
                              reads=[b_comb[i]], writes=[PSB[bd], b_Y[i][n]])
                    else:
                        P.dve(lambda e, i=i, n=n, bd=bd, ex=ex: e.scalar_tensor_tensor(out=Y[:, i, n * 512:(n + 1) * 512], in0=psf(bd),
                                                                                       scalar=comb[:, i, ex:ex + 1], in1=Y[:, i, n * 512:(n + 1) * 512],
                                                                                       op0=ALU.mult, op1=ALU.add),
                              reads=[b_comb[i], b_Y[i][n]], writes=[PSB[bd], b_Y[i][n]])
        A.release(m_p6)
        P.barrier()
        gfbc = A.alloc([128, D], F32, "gfbc")
        b_gf = Buf()
        tmp_row6 = A.alloc([1, 512], F32, "tmprow6")
        b_tmprow6 = Buf()
        bcast_rows(gfbc[:], b_gf, grows[2:3, :], D, tmp_row6, b_tmprow6, 0)
        x1l = [A.alloc([128, D], F32, f"x1l{i}") for i in range(2)]
        b_x1l = bufs(2)
        of6 = [A.alloc([128, D], F32, f"of6_{i}") for i in range(2)]
        b_of6 = bufs(2)
        junk6 = A.alloc([128, D], BF16, "junk6")
        b_junk6 = Buf()
        st6 = A.alloc([128, 8], F32, "st6")
        b_st6 = Buf()
        for i in range(NOWN):
            k6 = i % 2
            P.dma("sp", x1l[k6][:], x1s[i], writes=[b_x1l[k6]])
            P.dve(lambda e, i=i: e.tensor_tensor(out=Y[:, i, :], in0=Y[:, i, :], in1=G2bc[:], op=ALU.mult),
                  reads=[b_G2] + b_Y[i], writes=b_Y[i])
            P.pool(lambda e, i=i, k6=k6: e.tensor_tensor(out=x1l[k6][:], in0=x1l[k6][:], in1=Y[:, i, :], op=ALU.add),
                   reads=[b_x1l[k6]] + b_Y[i], writes=[b_x1l[k6]])
            P.dve(lambda e, k6=k6: e.scalar_tensor_tensor(out=junk6[:], in0=x1l[k6][:], scalar=1.0, in1=x1l[k6][:], op0=ALU.mult, op1=ALU.mult,
                                                          accum_out=st6[:, 0:1]),
                  reads=[b_x1l[k6]], writes=[b_junk6, b_st6])
            P.act(lambda e: e.activation(out=st6[:, 1:2], in_=st6[:, 0:1], func=AF.Sqrt, scale=1.0 / D, bias=EPS), reads=[b_st6], writes=[b_st6])
            P.dve(lambda e: e.reciprocal(out=st6[:, 2:3], in_=st6[:, 1:2]), reads=[b_st6], writes=[b_st6])
            P.dve(lambda e, k6=k6: e.scalar_tensor_tensor(out=of6[k6][:], in0=x1l[k6][:], scalar=st6[:, 2:3], in1=gfbc[:], op0=ALU.mult, op1=ALU.mult),
                  reads=[b_x1l[k6], b_st6, b_gf], writes=[b_of6[k6]])
            final_ops.append(P.dma("act", out_d[i * 128:(i + 1) * 128, :], of6[k6][:], reads=[b_of6[k6]]))

    if dbg and upto == 2:
        t = dbg_tensor("KT", [128, 2, NU * 128], BF16)
        final_ops.append(P.dma("sp", t, KT[:], reads=b_KT))
        t = dbg_tensor("ikT", [128, NU * 128], BF16)
        final_ops.append(P.dma("sp", t, ikT[:], reads=b_ikT))
        t = dbg_tensor("Vx", [128, NU, 2, 132], BF16)
        final_ops.append(P.dma("sp", t, Vx[:], reads=b_Vx))
        t = dbg_tensor("A1", [128, D])
        final_ops.append(P.dma("sp", t, A1[:], reads=[b_A1]))
        t = dbg_tensor("B1", [128, D])
        final_ops.append(P.dma("sp", t, B1[:], reads=[b_B1]))

    P.emit(final_wait_ops=final_ops)
    return nc, dbg_out


def _consts():
    c = np.zeros((128, 640), np.float32)
    c[:, 0:128] = np.eye(128, dtype=np.float32)
    q = np.arange(128)[:, None]
    k = np.arange(128)[None, :]
    c[:, 128:256] = np.where(k <= q, 0.0, NEG)
    s = np.arange(128)[:, None]
    t = np.arange(128)[None, :]
    c[:, 256:384] = ((t >= s) & ((t // 64) == (s // 64))).astype(np.float32)
    p = np.arange(128)
    c[:, 384] = np.power(10000.0, -(2.0 * (p % 64)) / 128.0)
    c[:, 385] = np.power(10000.0, -(2.0 * (p % 32)) / 64.0)
    c[:, 386] = np.where((p % 128) < 64, -1.0, 1.0)
    c[:, 387] = np.where((p % 64) < 32, -1.0, 1.0)
    c[:, 388:516] = 1.0
    for kk in range(NBIS):
        c[:, 516 + kk] = 2.0 ** (-kk)
    c[:, 540:604] = 1.0
    c[:, 540] = 0.0
    return c


def _swap_cols(w, nheads, hd):
    half = hd // 2
    w3 = w.reshape(w.shape[0], nheads, hd)
    return np.concatenate([w3[:, :, half:], w3[:, :, :half]], axis=2).reshape(w.shape[0], nheads * hd)


def prep_inputs(core, x, c, positions, w_ada, b_ada, g_norm1, w_in, g_head, hg_lower_bounds, w_attn_up, w_hgrn_up,
                w_out, g_norm2, w_router_group, b_router_group, w_router_expert, b_router_expert,
                w_exp_gate, w_exp_up, w_exp_down, g_final, shared):
    b, j = core // 4, core % 4
    pad = 3 - j
    xa = np.zeros((NU * 128, D), np.float32)
    nreal = (NU - pad) * 128
    xa[pad * 128:] = x[b][:nreal]
    own_rows = np.concatenate([np.arange(128 * (4 * i + j), 128 * (4 * i + j) + 128) for i in range(8)])
    xo = np.ascontiguousarray(x[b][own_rows])
    pos_pad = np.zeros((NU * 128,), np.int32)
    pos_pad[pad * 128:] = positions[b][:nreal]
    posr = np.ascontiguousarray(np.broadcast_to(pos_pad[None, :], (128, NU * 128)))
    poso = np.ascontiguousarray(np.broadcast_to(positions[b][own_rows][None, :], (128, 1024)))
    valid = np.zeros((NU * 128,), np.float32)
    valid[pad * 128:] = 1.0
    vmask = np.ascontiguousarray(valid.reshape(NU, 128).T)
    smask = np.ascontiguousarray(np.broadcast_to(np.where(valid[:512] > 0, 0.0, NEG)[None, :], (128, 512))).astype(np.float32)
    ccol = np.ascontiguousarray(c[b].reshape(16, 128).T)
    m = dict(xa=xa, xo=xo, posr=posr, poso=poso, vmask=vmask, smask=smask, ccol=ccol)
    m.update(shared)
    return m


def prep_shared(w_ada, b_ada, g_norm1, w_in, g_head, hg_lower_bounds, w_attn_up, w_hgrn_up, w_out, g_norm2,
                w_router_group, b_router_group, w_router_expert, b_router_expert, w_exp_gate, w_exp_up, w_exp_down,
                g_final):
    wi = w_in[0]
    ak = wi[:, O_AK:O_AK + 256]
    ik = wi[:, O_IK:O_IK + 64]
    ak_sw = _swap_cols(ak, 2, 128)
    ik_sw = _swap_cols(ik, 1, 64)
    w1a = np.ascontiguousarray(np.concatenate([ak, ak_sw, ik, ik, ik_sw, ik_sw], axis=1))
    aq_sw = _swap_cols(wi[:, O_AQ:O_AQ + 1024], 8, 128)
    iq_sw = _swap_cols(wi[:, O_IQ:O_IQ + 512], 8, 64)
    w2s = np.ascontiguousarray(np.concatenate([aq_sw, iq_sw], axis=1))
    grows = np.zeros((4, D), np.float32)
    grows[0] = g_norm1[0]
    grows[1] = g_norm2[0]
    grows[2] = g_final
    grows[3, :1024] = g_head[0].reshape(-1)
    lbc = np.zeros((128, 16), np.float32)
    lbc[:, 0:8] = hg_lower_bounds[0].reshape(8, 128).T
    lbc[:, 8:16] = hg_lower_bounds[1].reshape(8, 128).T
    return dict(cst=_consts(), lbc=lbc, w_ada=np.ascontiguousarray(w_ada[0]), b_ada=np.ascontiguousarray(b_ada[0:1]),
                grows=grows, w_in=np.ascontiguousarray(wi), w1a=w1a, w2s=w2s,
                w_au=np.ascontiguousarray(w_attn_up[0]), w_hu=np.ascontiguousarray(w_hgrn_up[0]),
                w_out=np.ascontiguousarray(w_out[0]),
                w_r=np.ascontiguousarray(np.concatenate([w_router_group[0], w_router_expert[0]], axis=1)),
                b_r=np.ascontiguousarray(np.concatenate([b_router_group[0], b_router_expert[0]])[None, :]),
                w_eg=np.ascontiguousarray(w_exp_gate[0]), w_eu=np.ascontiguousarray(w_exp_up[0]),
                w_ed=np.ascontiguousarray(w_exp_down[0]))


def kernel(**inputs):
    inp = {k: np.asarray(v) for k, v in inputs.items()}
    x = inp["x"]
    shared = prep_shared(**{k: inp[k] for k in inp if k not in ("x", "c", "positions")})
    nc, _ = build()
    in_maps = [prep_inputs(core, shared=shared, **inp) for core in range(8)]
    res = run_bass_kernel_spmd(nc, in_maps, core_ids=list(range(8)))
    out = np.zeros(x.shape, np.float32)
    for core in range(8):
        b, j = core // 4, core % 4
        o = res.results[core]["out"]
        for i in range(8):
            g = 4 * i + j
            out[b, 128 * g:128 * g + 128] = o[128 * i:128 * i + 128]
    return out
```

```python
import contextlib
import math
import numpy as np
import concourse.bass as bass
import concourse.mybir as mybir
from concourse.bass_utils import run_bass_kernel_spmd

F32 = mybir.dt.float32
BF16 = mybir.dt.bfloat16
I32 = mybir.dt.int32
AF = mybir.ActivationFunctionType
ALU = mybir.AluOpType
AX = mybir.AxisListType

D = 2048
NU = 32
NQ = 8
NOWN = 8
EPS = 1e-6
ATT_SCALE = 128 ** -0.5
IDX_SCALE = 512 ** -0.5
NEG = -1.0e30
NBIS = 14
COMB_OFF = 228288
TWO_PI = 2.0 * math.pi

O_AQ, O_AK, O_AV, O_IQ, O_IK, O_IW = 0, 1024, 1280, 1536, 2048, 2112
O_HQ, O_HF, O_HI, O_HOG, O_GA, O_GH = 2120, 3144, 4168, 5192, 6216, 8264


class Buf:
    __slots__ = ("name", "lw", "rd")

    def __init__(self, name=""):
        self.name = name
        self.lw = None
        self.rd = {}


def bufs(n, name=""):
    return [Buf(f"{name}{i}") for i in range(n)]


class Prog:
    STREAMS = ["pe", "act", "dve", "pool", "sp"]
    NDMASEM = 8

    def __init__(self, nc):
        self.nc = nc
        self.ops = []
        self.last = {}
        self.dma_open = []
        self.fence = {}

    def op(self, eng, fn, reads=(), writes=(), dma=False):
        idx = len(self.ops)
        deps = set()
        for b in reads:
            if b.lw is not None:
                self._dep(deps, idx, eng, dma, b.lw, "raw")
        for b in writes:
            if b.lw is not None:
                self._dep(deps, idx, eng, dma, b.lw, "waw")
            for r in b.rd.values():
                self._dep(deps, idx, eng, dma, r, "war")
        if eng in self.fence:
            deps |= self.fence.pop(eng)
        for b in reads:
            b.rd[("dma", idx) if dma else eng] = idx
        for b in writes:
            b.lw = idx
            b.rd = {}
        self.ops.append(dict(eng=eng, fn=fn, deps=deps, dma=dma, sig=False))
        if dma:
            self.dma_open.append(idx)
        else:
            self.last[eng] = idx
        return idx

    def _dep(self, deps, idx, eng, dma, pidx, kind):
        if pidx == idx:
            return
        p = self.ops[pidx]
        if (not dma) and (not p["dma"]) and p["eng"] == eng:
            if eng == "pe":
                return
        deps.add(pidx)

    def barrier(self):
        import os
        if os.environ.get("MK_NOBAR", "0") == "1":
            return
        f = set(self.last.values()) | set(self.dma_open)
        self.dma_open = []
        for s in self.STREAMS:
            self.fence[s] = set(f) | self.fence.get(s, set())

    def pe(self, fn, reads=(), writes=()):
        return self.op("pe", fn, reads, writes)

    def act(self, fn, reads=(), writes=()):
        return self.op("act", fn, reads, writes)

    def dve(self, fn, reads=(), writes=()):
        return self.op("dve", fn, reads, writes)

    def pool(self, fn, reads=(), writes=()):
        return self.op("pool", fn, reads, writes)

    def dma(self, eng, out, in_, reads=(), writes=(), **kw):
        return self.op(eng, lambda e: e.dma_start(out=out, in_=in_, **kw), reads, writes, dma=True)

    def emit(self, final_wait_ops=()):
        nc = self.nc
        ops = self.ops
        for o in ops:
            for d in o["deps"]:
                ops[d]["sig"] = True
        for i in final_wait_ops:
            ops[i]["sig"] = True
        ordn = {s: 0 for s in self.STREAMS}
        dcount = {s: 0 for s in self.STREAMS}
        for o in ops:
            s = o["eng"]
            if o["dma"]:
                k = dcount[s]
                dcount[s] += 1
                o["slot"] = k % self.NDMASEM
                o["use"] = k // self.NDMASEM + 1
            elif o["sig"]:
                ordn[s] += 1
                o["ord"] = ordn[s]
        with contextlib.ExitStack() as st:
            esem = {s: st.enter_context(nc.semaphore(f"e_{s}")) for s in self.STREAMS}
            dsem = {s: [st.enter_context(nc.semaphore(f"d_{s}{k}")) for k in range(self.NDMASEM)]
                    for s in self.STREAMS if dcount[s] > 0}
            block = st.enter_context(nc.Block())

            def target(pidx):
                p = ops[pidx]
                if p["dma"]:
                    return (dsem[p["eng"]][p["slot"]], 16 * p["use"], ("d", p["eng"], p["slot"]))
                return (esem[p["eng"]], p["ord"], ("e", p["eng"]))

            def run_stream(s, e):
                waited = {}
                for idx, o in enumerate(ops):
                    if o["eng"] != s:
                        continue
                    tg = {}
                    for d in o["deps"]:
                        sem, val, key = target(d)
                        if key not in tg or tg[key][1] < val:
                            tg[key] = (sem, val)
                    if o["dma"] and o["use"] > 1:
                        key = ("d", s, o["slot"])
                        val = 16 * (o["use"] - 1)
                        if key not in tg or tg[key][1] < val:
                            tg[key] = (dsem[s][o["slot"]], val)
                    for key, (sem, val) in tg.items():
                        if waited.get(key, 0) >= val:
                            continue
                        e.wait_ge(sem, val)
                        waited[key] = val
                    ins = o["fn"](e)
                    if o["dma"]:
                        ins.then_inc(dsem[s][o["slot"]], 16)
                    elif o["sig"]:
                        ins.then_inc(esem[s], 1)
                if s == "sp":
                    for i in final_wait_ops:
                        sem, val, key = target(i)
                        e.wait_ge(sem, val)

            @block.tensor
            def _(e):
                run_stream("pe", e)

            @block.scalar
            def _(e):
                run_stream("act", e)

            @block.vector
            def _(e):
                run_stream("dve", e)

            @block.gpsimd
            def _(e):
                run_stream("pool", e)

            @block.sync
            def _(e):
                run_stream("sp", e)


class Arena:
    BASE = 16640
    LIMIT = 228288

    def __init__(self, nc):
        self.nc = nc
        self.off = self.BASE
        self.n = 0

    def alloc(self, shape, dt, name="t"):
        esz = 4 if dt in (F32, I32) else 2
        size = esz * int(np.prod(shape[1:]))
        size = (size + 63) // 64 * 64
        t = self.nc.alloc_sbuf_tensor_at(f"a{self.n}_{name}", list(shape), dt, offset=self.off)
        self.n += 1
        self.off += size
        assert self.off <= self.LIMIT, f"SBUF arena overflow at {name}: {self.off}"
        return t

    def mark(self):
        return self.off

    def release(self, m):
        self.off = m


def build(upto=99, dbg=False):
    nc = bass.Bass("TRN2", target_bir_lowering=False)

    def din(name, shape, dt=F32):
        return nc.dram_tensor(name, list(shape), dt, kind="ExternalInput").ap()

    def dscr(name, shape, dt=F32):
        return nc.dram_tensor(name, list(shape), dt).ap()

    xa = din("xa", [NU * 128, D])
    xo = din("xo", [1024, D])
    posr = din("posr", [128, NU * 128], I32)
    poso = din("poso", [128, 1024], I32)
    vmask_d = din("vmask", [128, NU])
    smask_d = din("smask", [128, 512])
    cst_d = din("cst", [128, 640])
    ccol_d = din("ccol", [128, 16])
    lbc_d = din("lbc", [128, 16])
    w_ada = din("w_ada", [D, 6 * D])
    b_ada = din("b_ada", [1, 6 * D])
    grows = din("grows", [4, D])
    w_in = din("w_in", [D, 10312])
    w1a = din("w1a", [D, 768])
    w2s = din("w2s", [D, 1536])
    if upto >= 5:
        w_au = din("w_au", [1024, D])
        w_hu = din("w_hu", [1024, D])
        w_out = din("w_out", [D, D])
        w_r = din("w_r", [D, 36])
        b_r = din("b_r", [1, 36])
    if upto >= 6:
        w_eg = din("w_eg", [32, D, 512])
        w_eu = din("w_eu", [32, D, 512])
        w_ed = din("w_ed", [32, 512, D])
    out_d = nc.dram_tensor("out", [1024, D], F32, kind="ExternalOutput").ap()

    hTs = dscr("hTs", [NU, 128, D], BF16)
    modscr = dscr("modscr", [4, 128, D])
    x1s = dscr("x1s", [NOWN, 128, D])

    dbg_out = {}

    def dbg_tensor(name, shape, dt=F32):
        t = nc.dram_tensor("dbg_" + name, list(shape), dt, kind="ExternalOutput").ap()
        dbg_out[name] = t
        return t

    P = Prog(nc)
    A = Arena(nc)
    final_ops = []

    PS = [nc.alloc_psum_tensor(f"ps{k}", [128, 512], F32) for k in range(8)]
    PSB = bufs(8, "ps")

    def psf(k):
        return PS[k][:]

    def psb(k):
        return PS[k][:].bitcast(BF16)

    cst = A.alloc([128, 640], F32, "cst")
    b_cst = Buf("cst")
    P.dma("sp", cst[:], cst_d, writes=[b_cst])
    ident_f = cst[:, 0:128]
    cmask = cst[:, 128:256]
    tmask = cst[:, 256:384]
    inv_a = cst[:, 384:385]
    inv_i = cst[:, 385:386]
    sgn_a = cst[:, 386:387]
    sgn_i = cst[:, 387:388]
    ones_row = cst[0:1, 388:516]
    bisc = cst[:, 516:516 + NBIS]
    rmask = cst[:, 540:604]
    ident_b = A.alloc([128, 128], BF16, "identb")
    b_idb = Buf("identb")
    P.dve(lambda e: e.tensor_copy(out=ident_b[:], in_=ident_f), reads=[b_cst], writes=[b_idb])
    vmask = A.alloc([128, NU], F32, "vmask")
    b_vmask = Buf("vmask")
    P.dma("sp", vmask[:], vmask_d, writes=[b_vmask])

    def bcast_rows(dst_ap, b_dst, row_dram_ap, n, tmp_row, b_tmp, bank):
        for c0 in range(0, n, 512):
            w = min(512, n - c0)
            P.dma("sp", tmp_row[0:1, 0:w], row_dram_ap[:, c0:c0 + w], writes=[b_tmp])
            P.pe(lambda e, w=w: e.matmul(psf(bank)[:, 0:w], lhsT=ones_row, rhs=tmp_row[0:1, 0:w], start=True, stop=True),
                 reads=[b_cst, b_tmp], writes=[PSB[bank]])
            P.act(lambda e, c0=c0, w=w: e.activation(out=dst_ap[:, c0:c0 + w], in_=psf(bank)[:, 0:w], func=AF.Copy),
                  writes=[PSB[bank], b_dst])

    KT = A.alloc([128, 2, NU * 128], BF16, "KT")
    ikT = A.alloc([128, NU * 128], BF16, "ikT")
    Vx = A.alloc([128, NU, 2, 132], BF16, "Vx")
    b_KT, b_ikT, b_Vx = bufs(NQ, "KT"), bufs(NQ, "ikT"), bufs(NU, "Vx")
    m_p1 = A.mark()
    A1 = A.alloc([128, D], F32, "A1")
    B1 = A.alloc([128, D], F32, "B1")
    b_A1, b_B1 = Buf("A1"), Buf("B1")
    ccol = A.alloc([128, 16], F32, "ccol")
    csil = A.alloc([128, 16], F32, "csil")
    crep = A.alloc([128, 16, 128], BF16, "crep")
    b_ccol, b_csil, b_crep = Buf(), Buf(), Buf()
    P.dma("sp", ccol[:], ccol_d, writes=[b_ccol])
    P.act(lambda e: e.activation(out=csil[:], in_=ccol[:], func=AF.Silu), reads=[b_ccol], writes=[b_csil])
    for ck in range(16):
        P.dve(lambda e, ck=ck: e.tensor_copy(out=crep[:, ck, :], in_=csil[:, ck:ck + 1].to_broadcast([128, 128])),
              reads=[b_csil], writes=[b_crep])
    wada_t = [A.alloc([128, 16, 256], BF16, f"wada{i}") for i in range(2)]
    b_wada = bufs(2, "wada")
    brow = [A.alloc([1, 512], F32, f"brow{i}") for i in range(2)]
    b_brow = bufs(2, "brow")
    stage = [A.alloc([128, 512], F32, "stage0")] * 2
    b_stage = [Buf("stage")] * 2
    nst = [0]

    MW = 256
    NB8 = D // MW
    gsm = A.alloc([128, MW], F32, "gsm")
    b_gsm = Buf()
    mod_dma_done = set()

    def mod_dma(blk):
        if blk in mod_dma_done or blk >= 6 * NB8:
            return
        mod_dma_done.add(blk)
        s = blk % 2
        P.dma("pool", wada_t[s][:], w_ada[:, blk * MW:(blk + 1) * MW].rearrange("(c p) f -> p c f", p=128),
              writes=[b_wada[s]])

    def mod_block(blk, consume, grow=None):
        s = blk % 2
        mod_dma(blk)
        P.dma("sp", brow[s][0:1, 0:MW], b_ada[:, blk * MW:(blk + 1) * MW], writes=[b_brow[s]])
        bank = 4 + blk % 2
        for ck in range(16):
            P.pe(lambda e, ck=ck, s=s, bank=bank: e.matmul(psf(bank)[:, 0:MW], lhsT=crep[:, ck, :], rhs=wada_t[s][:, ck, :],
                                                            start=(ck == 0), stop=False),
                 reads=[b_crep, b_wada[s]], writes=[PSB[bank]])
        P.pe(lambda e, s=s, bank=bank: e.matmul(psf(bank)[:, 0:MW], lhsT=ones_row, rhs=brow[s][0:1, 0:MW], start=False, stop=True),
             reads=[b_cst, b_brow[s]], writes=[PSB[bank]])
        mod_dma(blk + 1)
        if grow is not None:
            c0 = (blk % NB8) * MW
            P.dma("sp", brow[s][0:1, MW:2 * MW], grows[grow:grow + 1, c0:c0 + MW], writes=[b_brow[s]])
            P.pe(lambda e, s=s: e.matmul(psf(3)[:, 0:MW], lhsT=ones_row, rhs=brow[s][0:1, MW:2 * MW], start=True, stop=True),
                 reads=[b_cst, b_brow[s]], writes=[PSB[3]])
            P.act(lambda e: e.activation(out=gsm[:], in_=psf(3)[:, 0:MW], func=AF.Copy), writes=[PSB[3], b_gsm])
        consume(bank)

    def to_scr(slot, c0, bank, with_g):
        k = nst[0] % 2
        nst[0] += 1
        if with_g:
            P.dve(lambda e: e.scalar_tensor_tensor(out=stage[k][:, 0:MW], in0=psf(bank)[:, 0:MW], scalar=1.0, in1=gsm[:],
                                                   op0=ALU.add, op1=ALU.mult),
                  reads=[b_gsm], writes=[PSB[bank], b_stage[k]])
        else:
            P.act(lambda e: e.activation(out=stage[k][:, 0:MW], in_=psf(bank)[:, 0:MW], func=AF.Copy), writes=[PSB[bank], b_stage[k]])
        P.dma("act", modscr[slot, :, c0:c0 + MW], stage[k][:, 0:MW], reads=[b_stage[k]])

    for q in range(NB8):
        mod_block(q, lambda bank, q=q: P.act(
            lambda e: e.activation(out=B1[:, q * MW:(q + 1) * MW], in_=psf(bank)[:, 0:MW], func=AF.Copy),
            writes=[PSB[bank], b_B1]))
    for q in range(NB8):
        mod_block(NB8 + q, lambda bank, q=q: P.dve(
            lambda e: e.scalar_tensor_tensor(out=A1[:, q * MW:(q + 1) * MW], in0=psf(bank)[:, 0:MW], scalar=1.0,
                                             in1=gsm[:], op0=ALU.add, op1=ALU.mult),
            reads=[b_gsm], writes=[PSB[bank], b_A1]), grow=0)
    pending_mod = []
    for q in range(NB8):
        pending_mod.append(lambda q=q: mod_block(2 * NB8 + q, lambda bank, q=q: to_scr(0, q * MW, bank, False)))
    for q in range(NB8):
        pending_mod.append(lambda q=q: mod_block(3 * NB8 + q, lambda bank, q=q: to_scr(2, q * MW, bank, False)))
    for q in range(NB8):
        pending_mod.append(lambda q=q: mod_block(4 * NB8 + q, lambda bank, q=q: to_scr(1, q * MW, bank, True), grow=1))
    for q in range(NB8):
        pending_mod.append(lambda q=q: mod_block(5 * NB8 + q, lambda bank, q=q: to_scr(3, q * MW, bank, False)))

    xt = [A.alloc([128, D], F32, f"xt{i}") for i in range(2)]
    b_xt = bufs(2, "xt")
    hb = [A.alloc([128, D], BF16, f"hb{i}") for i in range(4)]
    b_hb = bufs(4, "hb")
    NS0 = [dict(junk=hb[i], b_junk=b_hb[i], xn=None, b_xn=None,
                st1=A.alloc([128, 8], F32, f"st1_{i}"), b_ssq=Buf(), b_rstd=Buf()) for i in range(4)]

    def norm_tile(NS, x_ap, bx, Abc, bA, Bbc, bB, hb_ap, b_hbk):
        junk, b_junk, xn, b_xn, st1, b_ssq, b_rstd = (NS["junk"], NS["b_junk"], NS["xn"], NS["b_xn"], NS["st1"],
                                                      NS["b_ssq"], NS["b_rstd"])
        P.dve(lambda e: e.scalar_tensor_tensor(out=junk[:], in0=x_ap, scalar=1.0, in1=x_ap, op0=ALU.mult, op1=ALU.mult,
                                               accum_out=st1[:, 0:1]),
              reads=[bx], writes=[b_junk, b_ssq])
        P.act(lambda e: e.activation(out=st1[:, 1:2], in_=st1[:, 0:1], func=AF.Sqrt, scale=1.0 / D, bias=EPS),
              reads=[b_ssq], writes=[b_rstd])
        P.dve(lambda e: e.reciprocal(out=st1[:, 2:3], in_=st1[:, 1:2]), reads=[b_rstd], writes=[b_rstd])
        if xn is None:
            P.dve(lambda e: e.scalar_tensor_tensor(out=x_ap, in0=x_ap, scalar=st1[:, 2:3], in1=Abc, op0=ALU.mult, op1=ALU.mult),
                  reads=[bx, b_rstd, bA], writes=[bx])
            P.dve(lambda e: e.tensor_tensor(out=hb_ap, in0=x_ap, in1=Bbc, op=ALU.add), reads=[bx, bB], writes=[b_hbk])
        else:
            P.dve(lambda e: e.scalar_tensor_tensor(out=xn[:], in0=x_ap, scalar=st1[:, 2:3], in1=Abc, op0=ALU.mult, op1=ALU.mult),
                  reads=[bx, b_rstd, bA], writes=[b_xn])
            P.dve(lambda e: e.tensor_tensor(out=hb_ap, in0=xn[:], in1=Bbc, op=ALU.add), reads=[b_xn, bB], writes=[b_hbk])

    def transpose_tile(hb_ap, b_hbk, dst_ap, b_dst, banks):
        for half in range(2):
            bank = banks[half]
            for k in range(8):
                ck = half * 8 + k
                P.pe(lambda e, k=k, ck=ck, bank=bank: e.transpose(out=psb(bank)[:, k * 128:(k + 1) * 128],
                                                                   in_=hb_ap[:, ck * 128:(ck + 1) * 128], identity=ident_b[:]),
                     reads=[b_hbk, b_idb], writes=[PSB[bank]])
            P.act(lambda e, half=half, bank=bank: e.activation(out=dst_ap[:, half * 1024:(half + 1) * 1024], in_=psb(bank),
                                                                func=AF.Copy),
                  writes=[PSB[bank], b_dst])


    def rope_scratch():
        t0 = A.alloc([128, 512], F32, "rp_t0")
        t2 = A.alloc([128, 512], F32, "rp_t2")
        return dict(pos_i=t0[:].bitcast(I32), ang=A.alloc([128, 512], F32, "rp_ang")[:],
                    ki=t2[:].bitcast(I32), kf=t0[:],
                    r=A.alloc([128, 512], F32, "rp_r")[:], m=t2[:],
                    rc=A.alloc([128, 512], F32, "rp_rc")[:], b=Buf("rp"))

    def rope_tables(RS, pos_dram_ap, inv_col, sgn_col, cos_ap, sin_ap, b_tab):
        rp_pos_i, rp_ang, rp_ki, rp_kf, rp_r, rp_m, rp_rc, b_rp = (RS["pos_i"], RS["ang"], RS["ki"], RS["kf"], RS["r"],
                                                                    RS["m"], RS["rc"], RS["b"])
        P.dma("sp", rp_pos_i, pos_dram_ap, writes=[b_rp])
        P.dve(lambda e: e.tensor_copy(out=rp_ang, in_=rp_pos_i), reads=[b_rp], writes=[b_rp])
        P.dve(lambda e: e.tensor_scalar(out=rp_ang, in0=rp_ang, scalar1=inv_col, scalar2=None, op0=ALU.mult),
              reads=[b_rp, b_cst], writes=[b_rp])
        P.dve(lambda e: e.tensor_scalar(out=rp_ki, in0=rp_ang, scalar1=1.0 / TWO_PI, scalar2=None, op0=ALU.mult),
              reads=[b_rp], writes=[b_rp])
        P.dve(lambda e: e.tensor_copy(out=rp_kf, in_=rp_ki), reads=[b_rp], writes=[b_rp])
        P.dve(lambda e: e.scalar_tensor_tensor(out=rp_r, in0=rp_kf, scalar=-TWO_PI, in1=rp_ang, op0=ALU.mult, op1=ALU.add),
              reads=[b_rp], writes=[b_rp])
        P.dve(lambda e: e.tensor_scalar(out=rp_m, in0=rp_r, scalar1=math.pi, scalar2=-TWO_PI, op0=ALU.is_gt, op1=ALU.mult),
              reads=[b_rp], writes=[b_rp])
        P.dve(lambda e: e.tensor_tensor(out=rp_r, in0=rp_r, in1=rp_m, op=ALU.add), reads=[b_rp], writes=[b_rp])
        P.dve(lambda e: e.tensor_scalar(out=rp_m, in0=rp_r, scalar1=-math.pi, scalar2=TWO_PI, op0=ALU.is_lt, op1=ALU.mult),
              reads=[b_rp], writes=[b_rp])
        P.dve(lambda e: e.tensor_tensor(out=rp_r, in0=rp_r, in1=rp_m, op=ALU.add), reads=[b_rp], writes=[b_rp])
        P.dve(lambda e: e.tensor_scalar(out=rp_m, in0=rp_r, scalar1=math.pi / 2, scalar2=-TWO_PI, op0=ALU.is_gt, op1=ALU.mult),
              reads=[b_rp], writes=[b_rp])
        P.dve(lambda e: e.scalar_tensor_tensor(out=rp_rc, in0=rp_r, scalar=math.pi / 2, in1=rp_m, op0=ALU.add, op1=ALU.add),
              reads=[b_rp], writes=[b_rp])
        P.act(lambda e: e.activation(out=sin_ap, in_=rp_r, func=AF.Sin), reads=[b_rp], writes=[b_tab])
        P.act(lambda e: e.activation(out=cos_ap, in_=rp_rc, func=AF.Sin), reads=[b_rp], writes=[b_tab])
        P.dve(lambda e: e.tensor_scalar(out=sin_ap, in0=sin_ap, scalar1=sgn_col, scalar2=None, op0=ALU.mult),
              reads=[b_tab, b_cst], writes=[b_tab])

    def rope_evac(dst_ap, b_dst, bank_a, bank_s, cos_ap, sin_ap, b_tab, t1, t2, b_t):
        P.dve(lambda e: e.tensor_tensor(out=t1, in0=psf(bank_a), in1=cos_ap, op=ALU.mult), reads=[b_tab], writes=[PSB[bank_a], b_t[0]])
        P.dve(lambda e: e.tensor_tensor(out=t2, in0=psf(bank_s), in1=sin_ap, op=ALU.mult), reads=[b_tab], writes=[PSB[bank_s], b_t[1]])
        P.dve(lambda e: e.tensor_tensor(out=dst_ap, in0=t1, in1=t2, op=ALU.add), reads=[b_t[0], b_t[1]], writes=[b_dst])

    if upto >= 2:
        RS1 = rope_scratch()
        w1a_t = A.alloc([128, 16, 768], BF16, "w1a")
        wv_t = A.alloc([128, 16, 256], BF16, "wv")
        b_w1a, b_wv = Buf(), Buf()
        P.dma("pool", w1a_t[:], w1a.rearrange("(c p) f -> p c f", p=128), writes=[b_w1a])
        P.dma("pool", wv_t[:], w_in[:, O_AV:O_AV + 256].rearrange("(c p) f -> p c f", p=128), writes=[b_wv])
        hTq = [A.alloc([128, 4, 16, 128], BF16, f"hTq{i}") for i in range(2)]
        b_hTq4 = [bufs(4, f"hTq{i}_") for i in range(2)]
        tabs = [A.alloc([128, 512], F32, f"tab{i}") for i in range(4)]
        b_tabs = [Buf("tab_a"), Buf("tab_i")]
        rt = [A.alloc([128, 512], F32, f"rt{i}") for i in range(2)] * 2
        b_rt = bufs(2) * 2
        def norm_quad(qq):
            for r in range(4):
                u = 4 * qq + r
                k_ = u % 2
                P.dma("sp", xt[k_][:], xa[u * 128:(u + 1) * 128, :], writes=[b_xt[k_]])
                norm_tile(NS0[r], xt[k_][:], b_xt[k_], A1[:], b_A1, B1[:], b_B1, hb[r][:], b_hb[r])

        def trans_quad(qq):
            for r in range(4):
                u = 4 * qq + r
                transpose_tile(hb[r][:], b_hb[r], hTq[qq % 2][:, r, :, :].rearrange("p c t -> p (c t)"), b_hTq4[qq % 2][r], (6, 7))
                P.dma("act", hTs[u], hTq[qq % 2][:, r, :, :].rearrange("p c t -> p (c t)"), reads=[b_hTq4[qq % 2][r]])

        def norm_one(qq, r):
            u = 4 * qq + r
            k_ = u % 2
            P.dma("sp", xt[k_][:], xa[u * 128:(u + 1) * 128, :], writes=[b_xt[k_]])
            norm_tile(NS0[r], xt[k_][:], b_xt[k_], A1[:], b_A1, B1[:], b_B1, hb[r][:], b_hb[r])

        def projA(q, s, gi):
            ca, cs = [(0, 2), (1, 3), (4, 5)][gi]
            banks = (2 * (gi % 2), 2 * (gi % 2) + 1)
            for bi, cc in enumerate((ca, cs)):
                for ck in range(16):
                    P.pe(lambda e, ck=ck, cc=cc, bank=banks[bi]: e.matmul(
                        psf(bank), lhsT=w1a_t[:, ck, cc * 128:(cc + 1) * 128], rhs=hTq[s][:, :, ck, :],
                        start=(ck == 0), stop=(ck == 15)),
                        reads=[b_w1a] + b_hTq4[s], writes=[PSB[banks[bi]]])

        def evacA(q, gi):
            banks = (2 * (gi % 2), 2 * (gi % 2) + 1)
            k2 = 2 * (gi % 2)
            if gi < 2:
                rope_evac(KT[:, gi, q * 512:(q + 1) * 512], b_KT[q], banks[0], banks[1], tabs[0][:], tabs[1][:], b_tabs[0],
                          rt[k2][:], rt[k2 + 1][:], (b_rt[k2], b_rt[k2 + 1]))
            else:
                rope_evac(ikT[:, q * 512:(q + 1) * 512], b_ikT[q], banks[0], banks[1], tabs[2][:], tabs[3][:], b_tabs[1],
                          rt[k2][:], rt[k2 + 1][:], (b_rt[k2], b_rt[k2 + 1]))

        def projV(q, s):
            for r in range(4):
                u = 4 * q + r
                bank = 4 + (u % 2)
                for ck in range(16):
                    P.pe(lambda e, ck=ck, r=r, bank=bank: e.matmul(psf(bank)[:, 0:256], lhsT=hTq[s][:, r, ck, :], rhs=wv_t[:, ck, :],
                                                                    start=(ck == 0), stop=(ck == 15)),
                         reads=[b_wv, b_hTq4[s][r]], writes=[PSB[bank]])
                P.act(lambda e, u=u, bank=bank: e.activation(out=Vx[:, u, :, 0:128],
                                                             in_=psf(bank)[:, 0:256].rearrange("p (g d) -> p g d", g=2),
                                                             func=AF.Copy, scale=vmask[:, u:u + 1]),
                      reads=[b_vmask], writes=[PSB[bank], b_Vx[u]])
                P.pool(lambda e, u=u: e.tensor_copy(out=Vx[:, u, :, 128:129],
                                                    in_=vmask[:, u:u + 1].unsqueeze(1).to_broadcast([128, 2, 1])),
                       reads=[b_vmask], writes=[b_Vx[u]])

        norm_quad(0)
        trans_quad(0)
        for q in range(NQ):
            s = q % 2
            nxt = q + 1 < NQ
            rope_tables(RS1, posr[:, q * 512:(q + 1) * 512], inv_a, sgn_a, tabs[0][:], tabs[1][:], b_tabs[0])
            rope_tables(RS1, posr[:, q * 512:(q + 1) * 512], inv_i, sgn_i, tabs[2][:], tabs[3][:], b_tabs[1])
            projA(q, s, 0)
            if nxt:
                norm_one(q + 1, 0)
            projA(q, s, 1)
            evacA(q, 0)
            if nxt:
                norm_one(q + 1, 1)
            projA(q, s, 2)
            evacA(q, 1)
            if nxt:
                norm_one(q + 1, 2)
            projV(q, s)
            evacA(q, 2)
            if nxt:
                norm_one(q + 1, 3)
            for _ in range(5):
                if pending_mod:
                    pending_mod.pop(0)()
            if nxt:
                trans_quad(q + 1)

    while pending_mod:
        pending_mod.pop(0)()

    A.release(m_p1)
    P.barrier()
    o_n = A.alloc([128, NOWN, 1024], BF16, "o_n")
    b_on = bufs(NOWN, "o_n")
    m_p1b = A.mark()
    if upto >= 3:
        ghbc = A.alloc([128, 1024], F32, "ghbc")
        b_ghbc = Buf()
        tmp_row2 = A.alloc([1, 512], F32, "tmprow2")
        b_tmprow2 = Buf()
        import os
        _skip = os.environ.get("MK_SKIP", "")
        if "g" not in _skip:
            bcast_rows(ghbc[:], b_ghbc, grows[3:4, :], 1024, tmp_row2, b_tmprow2, 7)
        lbt = A.alloc([128, 40], F32, "lbt")
        b_lbt = Buf()
        P.dma("sp", lbt[:, 0:16], lbc_d, writes=[b_lbt])
        P.dve(lambda e: e.tensor_tensor(out=lbt[:, 16:24], in0=lbt[:, 0:8], in1=lbt[:, 8:16], op=ALU.subtract),
              reads=[b_lbt], writes=[b_lbt])
        P.act(lambda e: e.activation(out=lbt[:, 16:24], in_=lbt[:, 16:24], func=AF.Sigmoid), reads=[b_lbt], writes=[b_lbt])
        P.dve(lambda e: e.tensor_scalar(out=lbt[:, 24:32], in0=lbt[:, 16:24], scalar1=-1.0, scalar2=1.0, op0=ALU.mult, op1=ALU.add),
              reads=[b_lbt], writes=[b_lbt])
        P.dve(lambda e: e.tensor_scalar(out=lbt[:, 32:40], in0=lbt[:, 16:24], scalar1=-1.0, scalar2=None, op0=ALU.add),
              reads=[b_lbt], writes=[b_lbt])
        rm512 = A.alloc([128, 8, 64], F32, "rm512")
        b_rm = Buf()
        P.dve(lambda e: e.tensor_copy(out=rm512[:], in_=rmask.unsqueeze(1).to_broadcast([128, 8, 64])), reads=[b_cst], writes=[b_rm])
        whf = A.alloc([128, 16, 512], BF16, "whf")
        whi = A.alloc([128, 16, 512], BF16, "whi")
        whq = A.alloc([128, 16, 512], BF16, "whq")
        b_whf, b_whi, b_whq = Buf(), Buf(), Buf()
        hTqB = [A.alloc([128, 4, 16, 128], BF16, f"hTqb{i}") for i in range(2)]
        b_hTqB = bufs(2)
        bA = [A.alloc([128, 512], F32, f"hgA{i}") for i in range(4)]
        bK = [A.alloc([128, 512], F32, f"hgK{i}") for i in range(4)]
        bC = [A.alloc([128, 512], F32, f"hgC{i}") for i in range(4)]
        b_bA, b_bK, b_bC = bufs(4), bufs(4), bufs(4)
        kdT = A.alloc([128, 4, 512], BF16, "kdT")
        b_kdT = bufs(4)
        dec = A.alloc([128, 4, 8], F32, "dec")
        b_dec = Buf()
        ep = A.alloc([128, 4, 128], F32, "ep")
        b_ep = Buf()
        v_t = [A.alloc([128, 512], BF16, f"v_t{i}") for i in range(2)]
        b_vt = bufs(2)
        kd_t = [A.alloc([128, 4, 128], BF16, f"kd_t{i}") for i in range(2)]
        b_kdt = bufs(2)
        qs = A.alloc([128, 4, 128], F32, "qs")
        b_qs = Buf()
        qdT = A.alloc([128, 4, 128], BF16, "qdT")
        q0 = A.alloc([128, 4, 128], BF16, "q0")
        q1 = A.alloc([128, 4, 128], BF16, "q1")
        b_qd, b_q0, b_q1 = Buf(), Buf(), Buf()
        attm = A.alloc([128, 4, 128], BF16, "attm")
        b_attm = Buf()
        Sst = A.alloc([128, 4, 128], F32, "Sst")
        b_S = Buf()
        Sbf = [A.alloc([128, 4, 128], BF16, f"Sbf{i}") for i in range(2)]
        b_Sbf = bufs(2)
        tmpU = A.alloc([128, 4, 128], F32, "tmpU")
        b_tmpU = Buf()
        o_sb = A.alloc([128, 4, 128], F32, "o_sb")
        b_osb = Buf()
        junk4 = A.alloc([128, 128], BF16, "junk4")
        b_junk4 = Buf()
        st4 = A.alloc([128, 12], F32, "st4")
        b_st4 = Buf()
        if "m" not in _skip:
            P.pool(lambda e: e.memset(q0[:], 0.0), writes=[b_q0])
            P.pool(lambda e: e.memset(q1[:], 0.0), writes=[b_q1])
        import os
        for hg in range(int(os.environ.get("MK_NHG", 2))):
            P.dma("pool", whf[:], w_in[:, O_HF + hg * 512:O_HF + (hg + 1) * 512].rearrange("(c p) f -> p c f", p=128), writes=[b_whf])
            P.dma("pool", whi[:], w_in[:, O_HI + hg * 512:O_HI + (hg + 1) * 512].rearrange("(c p) f -> p c f", p=128), writes=[b_whi])
            P.dma("pool", whq[:], w_in[:, O_HQ + hg * 512:O_HQ + (hg + 1) * 512].rearrange("(c p) f -> p c f", p=128), writes=[b_whq])
            P.pool(lambda e: e.memset(Sst[:], 0.0), writes=[b_S])
            for q in range(int(os.environ.get("MK_NQ1B", NQ))):
                s = q % 2
                for r in range(4):
                    P.dma("sp", hTqB[s][:, r, :, :], hTs[4 * q + r].rearrange("p (c t) -> p c t", c=16), writes=[b_hTqB[s]])
                for h in range(4):
                    for ck in range(16):
                        P.pe(lambda e, ck=ck, h=h, s=s: e.matmul(psf(h), lhsT=whf[:, ck, h * 128:(h + 1) * 128], rhs=hTqB[s][:, :, ck, :],
                                                                  start=(ck == 0), stop=(ck == 15)),
                             reads=[b_whf, b_hTqB[s]], writes=[PSB[h]])
                for h in range(4):
                    P.act(lambda e, h=h: e.activation(out=bA[h][:], in_=psf(h), func=AF.Sigmoid), writes=[PSB[h], b_bA[h]])
                for h in range(4):
                    H = 4 * hg + h
                    P.dve(lambda e, h=h, H=H: e.tensor_scalar(out=bK[h][:], in0=bA[h][:], scalar1=-1.0, scalar2=lbt[:, 32 + H:33 + H],
                                                               op0=ALU.add, op1=ALU.mult),
                          reads=[b_bA[h], b_lbt], writes=[b_bK[h]])
                for h in range(4):
                    H = 4 * hg + h
                    P.act(lambda e, h=h, H=H: e.activation(out=bA[h][:], in_=bA[h][:], func=AF.Ln, scale=lbt[:, 24 + H:25 + H],
                                                            bias=lbt[:, 16 + H:17 + H]),
                          reads=[b_bA[h], b_lbt], writes=[b_bA[h]])
                for h in range(4):
                    P.dve(lambda e, h=h: e.tensor_tensor_scan(out=bC[h][:], data0=rm512[:].rearrange("p c t -> p (c t)"), data1=bA[h][:],
                                                              initial=0.0, op0=ALU.mult, op1=ALU.add),
                          reads=[b_bA[h], b_rm], writes=[b_bC[h]])
                for h in range(4):
                    P.act(lambda e, h=h: e.activation(out=bA[h][:], in_=bC[h][:], func=AF.Exp, scale=-1.0), reads=[b_bC[h]], writes=[b_bA[h]])
                for h in range(4):
                    P.act(lambda e, h=h: e.activation(out=dec[:, h, :], in_=bC[h][:].rearrange("p (c t) -> p c t", t=64)[:, :, 63],
                                                      func=AF.Exp),
                          reads=[b_bC[h]], writes=[b_dec])
                for h in range(4):
                    P.act(lambda e, h=h: e.activation(out=ep[:, h, :], in_=bC[h][:, 384:512], func=AF.Exp), reads=[b_bC[h]], writes=[b_ep])
                for h in range(4):
                    P.dve(lambda e, h=h: e.tensor_tensor(out=kdT[:, h, :], in0=bK[h][:], in1=bA[h][:], op=ALU.mult),
                          reads=[b_bK[h], b_bA[h]], writes=[b_kdT[h]])
                for r in range(4):
                    u = 4 * q + r
                    k2 = u % 2
                    own = (r == 3)
                    for ck in range(16):
                        P.pe(lambda e, ck=ck, r=r, s=s: e.matmul(psf(4), lhsT=hTqB[s][:, r, ck, :], rhs=whi[:, ck, :],
                                                                  start=(ck == 0), stop=(ck == 15)),
                             reads=[b_whi, b_hTqB[s]], writes=[PSB[4]])
                    P.act(lambda e, u=u, k2=k2: e.activation(out=v_t[k2][:], in_=psf(4), func=AF.Copy, scale=vmask[:, u:u + 1]),
                          reads=[b_vmask], writes=[PSB[4], b_vt[k2]])
                    for h in range(4):
                        P.pe(lambda e, h=h, r=r: e.transpose(out=psb(5)[:, h * 128:(h + 1) * 128], in_=kdT[:, h, r * 128:(r + 1) * 128],
                                                             identity=ident_b[:]),
                             reads=[b_kdT[h], b_idb], writes=[PSB[5]])
                    P.dve(lambda e, k2=k2: e.tensor_copy(out=kd_t[k2][:].rearrange("p h d -> p (h d)"), in_=psb(5)[:, 0:512]),
                          writes=[PSB[5], b_kdt[k2]])
                    if own:
                        i = q
                        for h in range(4):
                            for ck in range(16):
                                P.pe(lambda e, ck=ck, h=h, s=s: e.matmul(psf(7)[:, h * 128:(h + 1) * 128], lhsT=whq[:, ck, h * 128:(h + 1) * 128],
                                                                          rhs=hTqB[s][:, 3, ck, :], start=(ck == 0), stop=(ck == 15)),
                                     reads=[b_whq, b_hTqB[s]], writes=[PSB[7]])
                        P.act(lambda e: e.activation(out=qs[:].rearrange("p h d -> p (h d)"), in_=psf(7), func=AF.Silu),
                              writes=[PSB[7], b_qs])
                        P.dve(lambda e: e.tensor_tensor(out=qdT[:], in0=qs[:], in1=ep[:], op=ALU.mult), reads=[b_qs, b_ep], writes=[b_qd])
                        P.pool(lambda e: e.tensor_copy(out=q0[:, :, 0:64], in_=qdT[:, :, 0:64]), reads=[b_qd], writes=[b_q0])
                        P.pool(lambda e: e.tensor_copy(out=q1[:, :, 64:128], in_=qdT[:, :, 64:128]), reads=[b_qd], writes=[b_q1])
                        for h in range(4):
                            P.pe(lambda e, h=h: e.matmul(psf(7)[:, h * 128:(h + 1) * 128], lhsT=kdT[:, h, 384:512], rhs=qdT[:, h, :],
                                                         start=True, stop=True),
                                 reads=[b_kdT[h], b_qd], writes=[PSB[7]])
                        P.dve(lambda e: e.tensor_tensor(out=attm[:], in0=psf(7).rearrange("p (h d) -> p h d", h=4),
                                                        in1=tmask.unsqueeze(1).to_broadcast([128, 4, 128]), op=ALU.mult),
                              reads=[b_cst], writes=[PSB[7], b_attm])
                    for c in range(2):
                        if own:
                            P.act(lambda e, c=c: e.activation(out=Sbf[c][:], in_=Sst[:], func=AF.Copy), reads=[b_S], writes=[b_Sbf[c]])
                        for h in range(4):
                            P.pe(lambda e, h=h, c=c, k2=k2: e.matmul(psf(6)[:, h * 128:(h + 1) * 128], lhsT=kd_t[k2][64 * c:64 * c + 64, h, :],
                                                                      rhs=v_t[k2][64 * c:64 * c + 64, h * 128:(h + 1) * 128], start=True, stop=True),
                                 reads=[b_kdt[k2], b_vt[k2]], writes=[PSB[6]])
                        cq = 2 * r + c
                        P.dve(lambda e: e.tensor_tensor(out=tmpU[:], in0=psf(6).rearrange("p (h d) -> p h d", h=4), in1=Sst[:], op=ALU.add),
                              reads=[b_S], writes=[PSB[6], b_tmpU])
                        P.dve(lambda e, cq=cq: e.tensor_tensor(out=Sst[:], in0=tmpU[:], in1=dec[:, :, cq:cq + 1].to_broadcast([128, 4, 128]),
                                                               op=ALU.mult),
                              reads=[b_tmpU, b_dec], writes=[b_S])
                    if own:
                        for h in range(4):
                            P.pe(lambda e, h=h, k2=k2: e.matmul(psf(7)[:, h * 128:(h + 1) * 128], lhsT=attm[:, h, :],
                                                                rhs=v_t[k2][:, h * 128:(h + 1) * 128], start=True, stop=False),
                                 reads=[b_attm, b_vt[k2]], writes=[PSB[7]])
                            P.pe(lambda e, h=h: e.matmul(psf(7)[:, h * 128:(h + 1) * 128], lhsT=q0[:, h, :], rhs=Sbf[0][:, h, :],
                                                         start=False, stop=False),
                                 reads=[b_q0, b_Sbf[0]], writes=[PSB[7]])
                            P.pe(lambda e, h=h: e.matmul(psf(7)[:, h * 128:(h + 1) * 128], lhsT=q1[:, h, :], rhs=Sbf[1][:, h, :],
                                                         start=False, stop=True),
                                 reads=[b_q1, b_Sbf[1]], writes=[PSB[7]])
                        P.act(lambda e: e.activation(out=o_sb[:].rearrange("p h d -> p (h d)"), in_=psf(7), func=AF.Copy),
                              writes=[PSB[7], b_osb])
                        for h in range(4):
                            P.dve(lambda e, h=h: e.scalar_tensor_tensor(out=junk4[:], in0=o_sb[:, h, :], scalar=1.0, in1=o_sb[:, h, :],
                                                                        op0=ALU.mult, op1=ALU.mult, accum_out=st4[:, h:h + 1]),
                                  reads=[b_osb], writes=[b_junk4, b_st4])
                        P.act(lambda e: e.activation(out=st4[:, 4:8], in_=st4[:, 0:4], func=AF.Sqrt, scale=1.0 / 128, bias=EPS),
                              reads=[b_st4], writes=[b_st4])
                        P.dve(lambda e: e.reciprocal(out=st4[:, 8:12], in_=st4[:, 4:8]), reads=[b_st4], writes=[b_st4])
                        for h in range(4):
                            H = 4 * hg + h
                            P.dve(lambda e, h=h, H=H, i=i: e.scalar_tensor_tensor(out=o_n[:, i, H * 128:(H + 1) * 128], in0=o_sb[:, h, :],
                                                                                   scalar=st4[:, 8 + h:9 + h], in1=ghbc[:, H * 128:(H + 1) * 128],
                                                                                   op0=ALU.mult, op1=ALU.mult),
                                  reads=[b_osb, b_st4, b_ghbc], writes=[b_on[i]])

    if dbg and upto == 3:
        t = dbg_tensor("o_n", [128, NOWN, 1024], BF16)
        final_ops.append(P.dma("sp", t, o_n[:], reads=b_on))
        t = dbg_tensor("ikT", [128, NU * 128], BF16)
        final_ops.append(P.dma("sp", t, ikT[:], reads=b_ikT))
        t = dbg_tensor("KT", [128, 2, NU * 128], BF16)
        final_ops.append(P.dma("sp", t, KT[:], reads=b_KT))

    A.release(m_p1b)
    P.barrier()
    y_att = A.alloc([128, NOWN, 1024], BF16, "y_att")
    b_yatt = bufs(NOWN, "yatt")
    m_p2 = A.mark()
    if upto >= 4:
        QT = A.alloc([128, 8, 1024], BF16, "QT")
        iqT = A.alloc([128, 4, 1024], BF16, "iqT")
        b_QT, b_iqT = bufs(8, "QT"), bufs(4, "iqT")
        iwt = A.alloc([128, NOWN, 24], F32, "iwt")
        b_iwt = bufs(NOWN, "iwt")
        smask_t = A.alloc([128, 512], F32, "smask")
        b_smask = Buf()
        P.dma("sp", smask_t[:], smask_d, writes=[b_smask])
        m_p2a = A.mark()
        hTo = A.alloc([128, NOWN, 16, 128], BF16, "hTo")
        b_hTo = bufs(NOWN, "hTo")
        for i in range(NOWN):
            P.dma("sp", hTo[:, i, :, :], hTs[4 * i + 3].rearrange("p (c t) -> p c t", c=16), writes=[b_hTo[i]])
        RS2 = rope_scratch()
        otab = [A.alloc([128, 1024], F32, f"otab{i}") for i in range(4)]
        b_otab = [Buf("otab_a"), Buf("otab_i")]
        for hf2 in range(2):
            rope_tables(RS2, poso[:, hf2 * 512:(hf2 + 1) * 512], inv_a, sgn_a, otab[0][:, hf2 * 512:(hf2 + 1) * 512],
                        otab[1][:, hf2 * 512:(hf2 + 1) * 512], b_otab[0])
            rope_tables(RS2, poso[:, hf2 * 512:(hf2 + 1) * 512], inv_i, sgn_i, otab[2][:, hf2 * 512:(hf2 + 1) * 512],
                        otab[3][:, hf2 * 512:(hf2 + 1) * 512], b_otab[1])
        wiw = A.alloc([128, 16, 8], BF16, "wiw")
        b_wiw = Buf()
        P.dma("pool", wiw[:], w_in[:, O_IW:O_IW + 8].rearrange("(c p) f -> p c f", p=128), writes=[b_wiw])
        for i in range(NOWN):
            for ck in range(16):
                P.pe(lambda e, ck=ck, i=i: e.matmul(psf(7)[:, 0:8], lhsT=hTo[:, i, ck, :], rhs=wiw[:, ck, :], start=(ck == 0), stop=(ck == 15)),
                     reads=[b_hTo[i], b_wiw], writes=[PSB[7]])
            P.act(lambda e, i=i: e.activation(out=iwt[:, i, 0:8], in_=psf(7)[:, 0:8], func=AF.Copy), writes=[PSB[7], b_iwt[i]])
            P.act(lambda e, i=i: e.activation(out=iwt[:, i, 8:16], in_=iwt[:, i, 0:8], func=AF.Abs, scale=IDX_SCALE),
                  reads=[b_iwt[i]], writes=[b_iwt[i]])
            P.dve(lambda e, i=i: e.tensor_scalar(out=iwt[:, i, 16:24], in0=iwt[:, i, 0:8], scalar1=0.0, scalar2=2.0,
                                                 op0=ALU.is_ge, op1=ALU.mult), reads=[b_iwt[i]], writes=[b_iwt[i]])
            P.dve(lambda e, i=i: e.tensor_scalar(out=iwt[:, i, 16:24], in0=iwt[:, i, 16:24], scalar1=-1.0, scalar2=None,
                                                 op0=ALU.add), reads=[b_iwt[i]], writes=[b_iwt[i]])
        wq = [[A.alloc([128, 16, 256], BF16, f"wq{k}_{t}") for t in range(2)] for k in range(2)]
        b_wq = [[Buf(), Buf()] for k in range(2)]
        rt2 = [A.alloc([128, 512], F32, f"rt2_{i}") for i in range(4)]
        b_rt2 = bufs(4)
        groups = [("q", g) for g in range(4)] + [("i", g) for g in range(2)]
        cnt = 0
        for gi, (kind, g) in enumerate(groups):
            k = gi % 2
            if kind == "q":
                src_a = w_in[:, O_AQ + g * 256:O_AQ + (g + 1) * 256]
                src_s = w2s[:, g * 256:(g + 1) * 256]
            else:
                src_a = w_in[:, O_IQ + g * 256:O_IQ + (g + 1) * 256]
                src_s = w2s[:, 1024 + g * 256:1024 + (g + 1) * 256]
            P.dma("pool", wq[k][0][:], src_a.rearrange("(c p) f -> p c f", p=128), writes=[b_wq[k][0]])
            P.dma("pool", wq[k][1][:], src_s.rearrange("(c p) f -> p c f", p=128), writes=[b_wq[k][1]])
            for cc in range(2):
                for hf2 in range(2):
                    banks = (2 * (cnt % 2), 2 * (cnt % 2) + 1)
                    k2 = 2 * (cnt % 2)
                    cnt += 1
                    for t in range(2):
                        for ck in range(16):
                            P.pe(lambda e, ck=ck, cc=cc, hf2=hf2, t=t, k=k, bank=banks[t]: e.matmul(
                                psf(bank), lhsT=wq[k][t][:, ck, cc * 128:(cc + 1) * 128],
                                rhs=hTo[:, 4 * hf2:4 * hf2 + 4, ck, :], start=(ck == 0), stop=(ck == 15)),
                                reads=[b_wq[k][t]] + b_hTo[4 * hf2:4 * hf2 + 4], writes=[PSB[banks[t]]])
                    if kind == "q":
                        hd = 2 * g + cc
                        rope_evac(QT[:, hd, hf2 * 512:(hf2 + 1) * 512], b_QT[hd], banks[0], banks[1],
                                  otab[0][:, hf2 * 512:(hf2 + 1) * 512], otab[1][:, hf2 * 512:(hf2 + 1) * 512], b_otab[0],
                                  rt2[k2][:], rt2[k2 + 1][:], (b_rt2[k2], b_rt2[k2 + 1]))
                    else:
                        chn = 2 * g + cc
                        rope_evac(iqT[:, chn, hf2 * 512:(hf2 + 1) * 512], b_iqT[chn], banks[0], banks[1],
                                  otab[2][:, hf2 * 512:(hf2 + 1) * 512], otab[3][:, hf2 * 512:(hf2 + 1) * 512], b_otab[1],
                                  rt2[k2][:], rt2[k2 + 1][:], (b_rt2[k2], b_rt2[k2 + 1]))
        A.release(m_p2a)
        P.barrier()
        scoreL = [A.alloc([128, 4096], F32, f"score{i}") for i in range(2)]
        b_scoreL = bufs(2, "score")
        mask01L = [A.alloc([128, 4096], BF16, f"mask01_{i}") for i in range(2)]
        b_mask01L = bufs(2, "mask01")
        maskTL = [A.alloc([128, 32, 128], BF16, f"maskT{i}") for i in range(2)]
        b_maskTL = bufs(2, "maskT")
        bsL = [A.alloc([128, 8 + NBIS], F32, f"bs{i}") for i in range(2)]
        b_bsL = bufs(2, "bs")
        sacc = [A.alloc([128, 512], F32, f"sacc{i}") for i in range(2)]
        b_sacc = bufs(2)
        rl = [A.alloc([128, 512], F32, f"rl{i}") for i in range(2)]
        b_rl = bufs(2)
        Et = [A.alloc([128, 4, 128], BF16, f"Et{i}") for i in range(2)]
        b_Et = bufs(2)
        PT = [A.alloc([128, 4, 128], BF16, f"PT{i}") for i in range(2)]
        b_PT = bufs(2)
        rc = A.alloc([128, 4], F32, "rc")
        b_rc = Buf()
        cnts = dict(ndot=0, nst=0)

        def indexer(i):
            score, b_score, bs, b_bs = scoreL[i % 2], b_scoreL[i % 2], bsL[i % 2], b_bsL[i % 2]
            nk = 512 * (i + 1)
            for kq in range(i + 1):
                for h in range(8):
                    bank = cnts["ndot"] % 2
                    k = cnts["ndot"] % 2
                    cnts["ndot"] += 1
                    pb = (h % 2) * 64
                    P.pe(lambda e, h=h, kq=kq, bank=bank, pb=pb: e.matmul(
                        psf(bank), lhsT=iqT[pb:pb + 64, h // 2, i * 128:(i + 1) * 128], rhs=ikT[pb:pb + 64, kq * 512:(kq + 1) * 512],
                        start=True, stop=True),
                        reads=[b_iqT[h // 2], b_ikT[kq]], writes=[PSB[bank]])
                    P.act(lambda e, h=h, bank=bank, k=k: e.activation(out=rl[k][:], in_=psf(bank), func=AF.Relu,
                                                                       scale=iwt[:, i, 8 + h:9 + h]),
                          reads=[b_iwt[i]], writes=[PSB[bank], b_rl[k]])
                    dst = score[:, kq * 512:(kq + 1) * 512] if h == 7 else sacc[h % 2][:]
                    b_dst = b_score if h == 7 else b_sacc[h % 2]
                    if h == 0:
                        P.dve(lambda e, k=k, dst=dst: e.tensor_scalar(out=dst, in0=rl[k][:], scalar1=iwt[:, i, 16:17], scalar2=None,
                                                                       op0=ALU.mult),
                              reads=[b_rl[k], b_iwt[i]], writes=[b_dst])
                    else:
                        P.dve(lambda e, k=k, h=h, dst=dst: e.scalar_tensor_tensor(
                            out=dst, in0=rl[k][:], scalar=iwt[:, i, 16 + h:17 + h], in1=sacc[(h - 1) % 2][:], op0=ALU.mult, op1=ALU.add),
                            reads=[b_rl[k], b_iwt[i], b_sacc[(h - 1) % 2]], writes=[b_dst])
            P.dve(lambda e: e.tensor_reduce(out=bs[:, 0:1], in_=score[:, 0:nk], axis=AX.X, op=ALU.max, apply_absolute_value=True),
                  reads=[b_score], writes=[b_bs])
            P.dve(lambda e: e.tensor_tensor(out=score[:, nk - 128:nk], in0=score[:, nk - 128:nk], in1=cmask, op=ALU.add),
                  reads=[b_score, b_cst], writes=[b_score])
            P.dve(lambda e: e.tensor_tensor(out=score[:, 0:512], in0=score[:, 0:512], in1=smask_t[:], op=ALU.add),
                  reads=[b_score, b_smask], writes=[b_score])
            P.dve(lambda e: e.tensor_scalar(out=bs[:, 8:8 + NBIS], in0=bisc, scalar1=bs[:, 0:1], scalar2=None, op0=ALU.mult),
                  reads=[b_bs, b_cst], writes=[b_bs])
            P.dve(lambda e: e.tensor_scalar(out=bs[:, 1:2], in0=bs[:, 0:1], scalar1=-1.0, scalar2=None, op0=ALU.mult),
                  reads=[b_bs], writes=[b_bs])

        def bis_step(i, k, step):
            score, b_score, bs, b_bs = scoreL[i % 2], b_scoreL[i % 2], bsL[i % 2], b_bsL[i % 2]
            mask01, b_mask01 = mask01L[i % 2], b_mask01L[i % 2]
            nk = 512 * (i + 1)
            if step == 0:
                P.dve(lambda e: e.tensor_tensor(out=bs[:, 2:3], in0=bs[:, 1:2], in1=bs[:, 8 + k:9 + k], op=ALU.add),
                      reads=[b_bs], writes=[b_bs])
            elif step == 1:
                P.dve(lambda e: e.tensor_scalar(out=mask01[:, 0:nk], in0=score[:, 0:nk], scalar1=bs[:, 2:3], scalar2=0.0,
                                                op0=ALU.is_ge, op1=ALU.add, accum_out=bs[:, 3:4]),
                      reads=[b_score, b_bs], writes=[b_mask01, b_bs])
            elif step == 2:
                P.dve(lambda e: e.tensor_scalar(out=bs[:, 4:5], in0=bs[:, 3:4], scalar1=256.0, scalar2=bs[:, 8 + k:9 + k],
                                                op0=ALU.is_ge, op1=ALU.mult),
                      reads=[b_bs], writes=[b_bs])
            else:
                P.dve(lambda e: e.tensor_tensor(out=bs[:, 1:2], in0=bs[:, 1:2], in1=bs[:, 4:5], op=ALU.add), reads=[b_bs], writes=[b_bs])

        def make_mask(i):
            score, b_score, bs, b_bs = scoreL[i % 2], b_scoreL[i % 2], bsL[i % 2], b_bsL[i % 2]
            mask01, b_mask01 = mask01L[i % 2], b_mask01L[i % 2]
            maskT, b_maskT = maskTL[i % 2], b_maskTL[i % 2]
            nk = 512 * (i + 1)
            nkt = 4 * (i + 1)
            P.dve(lambda e: e.tensor_scalar(out=mask01[:, 0:nk], in0=score[:, 0:nk], scalar1=bs[:, 1:2], scalar2=None, op0=ALU.is_ge),
                  reads=[b_score, b_bs], writes=[b_mask01])
            for k0 in range(0, nkt, 8):
                n = min(8, nkt - k0)
                for kk in range(n):
                    kt = k0 + kk
                    P.pe(lambda e, kk=kk, kt=kt: e.transpose(out=psb(2)[:, kk * 128:(kk + 1) * 128], in_=mask01[:, kt * 128:(kt + 1) * 128],
                                                             identity=ident_b[:]),
                         reads=[b_mask01, b_idb], writes=[PSB[2]])
                P.act(lambda e, k0=k0, n=n: e.activation(out=maskT[:, k0:k0 + n, :].rearrange("p k q -> p (k q)"),
                                                         in_=psb(2)[:, 0:n * 128], func=AF.Copy),
                      writes=[PSB[2], b_maskT])

        lnr = A.alloc([128, 8], F32, "lnr")
        b_lnr = Buf()
        Et2 = [A.alloc([128, 2, 128], BF16, f"Et2_{i}") for i in range(3)]
        b_Et2 = bufs(3)
        PT2 = [A.alloc([128, 2, 128], BF16, f"PT2_{i}") for i in range(3)]
        b_PT2 = bufs(3)

        def attention(i, use_dve=False):
            maskT, b_maskT = maskTL[i % 2], b_maskTL[i % 2]
            nkt = 4 * (i + 1)
            for hp in range(4):
                g = hp // 2
                accb = (4, 5) if hp % 2 == 0 else (7, 2)
                steps = []
                for kt in range(nkt):
                    sb_ = (3, 6)[cnts["nst"] % 2]
                    k = cnts["nst"] % 3
                    cnts["nst"] += 1
                    steps.append((kt, sb_, k))
                for j in range(nkt + 2):
                    if j < nkt:
                        kt, sb_, k = steps[j]
                        P.pe(lambda e, kt=kt, g=g, hp=hp, sb_=sb_: e.matmul(psf(sb_)[:, 0:256], lhsT=KT[:, g, kt * 128:(kt + 1) * 128],
                                                                             rhs=QT[:, 2 * hp:2 * hp + 2, i * 128:(i + 1) * 128],
                                                                             start=True, stop=True),
                             reads=[b_KT[kt // 4]] + b_QT[2 * hp:2 * hp + 2], writes=[PSB[sb_]])
                        P.act(lambda e, sb_=sb_, k=k: e.activation(out=Et2[k][:].rearrange("p h q -> p (h q)"), in_=psf(sb_)[:, 0:256],
                                                                   func=AF.Exp, scale=ATT_SCALE),
                              writes=[PSB[sb_], b_Et2[k]])
                        P.op("dve" if (use_dve and kt % 2 == 1) else "pool",
                             lambda e, kt=kt, k=k: e.tensor_tensor(out=PT2[k][:], in0=Et2[k][:],
                                                                   in1=maskT[:, kt, :].unsqueeze(1).to_broadcast([128, 2, 128]), op=ALU.mult),
                             reads=[b_Et2[k], b_maskT], writes=[b_PT2[k]])
                    if j >= 2:
                        kt, sb_, k = steps[j - 2]
                        for hh in range(2):
                            P.pe(lambda e, hh=hh, kt=kt, g=g, k=k, ab=accb[hh]: e.matmul(
                                psf(ab)[:, 0:129], lhsT=PT2[k][:, hh, :], rhs=Vx[:, kt, g, 0:129], start=(kt == 0), stop=(kt == nkt - 1)),
                                reads=[b_PT2[k], b_Vx[kt]], writes=[PSB[accb[hh]]])
                for hh in range(2):
                    hd = 2 * hp + hh
                    ab = accb[hh]
                    P.act(lambda e, ab=ab, hd=hd: e.activation(out=lnr[:, hd:hd + 1], in_=psf(ab)[:, 128:129], func=AF.Ln),
                          writes=[PSB[ab], b_lnr])
                    P.act(lambda e, hd=hd: e.activation(out=lnr[:, hd:hd + 1], in_=lnr[:, hd:hd + 1], func=AF.Exp, scale=-1.0),
                          reads=[b_lnr], writes=[b_lnr])
                    P.act(lambda e, ab=ab, hd=hd: e.activation(out=y_att[:, i, hd * 128:(hd + 1) * 128], in_=psf(ab)[:, 0:128],
                                                               func=AF.Copy, scale=lnr[:, hd:hd + 1]),
                          reads=[b_lnr], writes=[PSB[ab], b_yatt[i]])

        NP2 = NOWN // 2
        for pr in range(NP2 + 1):
            if pr < NP2:
                for i in (2 * pr, 2 * pr + 1):
                    indexer(i)
            if pr >= 1:
                for i in (2 * pr - 2, 2 * pr - 1):
                    attention(i, use_dve=(pr == NP2))
            if pr < NP2:
                tiles = (2 * pr, 2 * pr + 1)
                for k in range(NBIS):
                    for step in range(4):
                        for i in tiles:
                            bis_step(i, k, step)
                for i in tiles:
                    make_mask(i)

    if dbg and upto == 4:
        t = dbg_tensor("y_att", [128, NOWN, 1024], BF16)
        final_ops.append(P.dma("sp", t, y_att[:], reads=b_yatt))
        if upto >= 4:
            t = dbg_tensor("ikT", [128, NU * 128], BF16)
            final_ops.append(P.dma("sp", t, ikT[:], reads=b_ikT))
            t = dbg_tensor("rl0", [128, 512])
            final_ops.append(P.dma("sp", t, rl[0][:], reads=[b_rl[0]]))
            t = dbg_tensor("rl1", [128, 512])
            final_ops.append(P.dma("sp", t, rl[1][:], reads=[b_rl[1]]))
            t = dbg_tensor("sacc0", [128, 512])
            final_ops.append(P.dma("sp", t, sacc[0][:], reads=[b_sacc[0]]))
            t = dbg_tensor("score", [128, 4096])
            final_ops.append(P.dma("sp", t, scoreL[1][:], reads=[b_scoreL[1]]))
            t = dbg_tensor("bs", [128, 8 + NBIS])
            final_ops.append(P.dma("sp", t, bsL[1][:], reads=[b_bsL[1]]))
            t = dbg_tensor("mask01", [128, 4096], BF16)
            final_ops.append(P.dma("sp", t, mask01L[1][:], reads=[b_mask01L[1]]))
            t = dbg_tensor("maskT", [128, 32, 128], BF16)
            final_ops.append(P.dma("sp", t, maskTL[1][:], reads=[b_maskTL[1]]))
            t = dbg_tensor("iwt", [128, NOWN, 24])
            final_ops.append(P.dma("sp", t, iwt[:], reads=b_iwt))
            t = dbg_tensor("QT", [128, 8, 1024], BF16)
            final_ops.append(P.dma("sp", t, QT[:], reads=b_QT))
            t = dbg_tensor("iqT", [128, 4, 1024], BF16)
            final_ops.append(P.dma("sp", t, iqT[:], reads=b_iqT))

    P.barrier()
    R_L, R_M, R_H = 19584, 61056, 93824
    h2T = None
    comb = None
    if upto >= 5:
        A.off = R_L
        yaT = A.alloc([128, 8, 1024], BF16, "yaT")
        yhT = A.alloc([128, 8, 1024], BF16, "yhT")
        G1bc = A.alloc([128, D], F32, "G1bc")
        b_yaT, b_yhT, b_G1 = bufs(NOWN, "yaT"), Buf("yhT"), Buf("G1")
        assert A.off <= R_M
        A.off = R_H
        onT = A.alloc([128, 8, 1024], BF16, "onT")
        b_onT = bufs(NOWN, "onT")
        hTo4 = A.alloc([128, NOWN, 16, 128], BF16, "hTo4")
        b_hTo4 = bufs(NOWN, "hTo4")
        whog = [A.alloc([128, 16, 256], BF16, f"whog{i}") for i in range(2)]
        b_whog = bufs(2)
        sil4 = [A.alloc([128, 512], F32, f"sil4_{i}") for i in range(2)]
        b_sil4 = bufs(2)
        wg4 = [dict(ga=A.alloc([128, 16, 256], BF16, f"wga{i}"), gh=A.alloc([128, 16, 256], BF16, f"wgh{i}"),
                    au=A.alloc([128, 8, 256], BF16, f"wau{i}"), hu=A.alloc([128, 8, 256], BF16, f"whu{i}")) for i in range(2)]
        b_wg4 = [dict(ga=Buf(), gh=Buf(), au=Buf(), hu=Buf()) for i in range(2)]
        sg4 = [sil4[0], sil4[1]] + [A.alloc([128, 512], F32, f"sg4_{i}") for i in range(2)]
        b_sg4 = bufs(4)
        mm4 = [A.alloc([128, 512], F32, f"mm4_{i}") for i in range(4)]
        b_mm4 = bufs(4)
        P.dma("sp", G1bc[:], modscr[0], writes=[b_G1])
        for i in range(NOWN):
            P.dma("sp", hTo4[:, i, :, :], hTs[4 * i + 3].rearrange("p (c t) -> p c t", c=16), writes=[b_hTo4[i]])
        for i in range(NOWN):
            for (src, dstT, b_src, b_dstT, bank) in ((y_att, yaT, b_yatt, b_yaT, 0), (o_n, onT, b_on, b_onT, 1)):
                for c in range(8):
                    P.pe(lambda e, c=c, i=i, src=src, bank=bank: e.transpose(out=psb(bank)[:, c * 128:(c + 1) * 128],
                                                                             in_=src[:, i, c * 128:(c + 1) * 128], identity=ident_b[:]),
                         reads=[b_src[i], b_idb], writes=[PSB[bank]])
                P.act(lambda e, i=i, dstT=dstT, bank=bank: e.activation(out=dstT[:, :, i * 128:(i + 1) * 128],
                                                                        in_=psb(bank).rearrange("p (c t) -> p c t", c=8), func=AF.Copy),
                      writes=[PSB[bank], b_dstT[i]])
        n4 = 0
        for g4 in range(4):
            k4 = g4 % 2
            P.dma("pool", whog[k4][:], w_in[:, O_HOG + g4 * 256:O_HOG + (g4 + 1) * 256].rearrange("(c p) f -> p c f", p=128),
                  writes=[b_whog[k4]])
            for cc in range(2):
                chn = 2 * g4 + cc
                for hf4 in range(2):
                    bank = 2 + n4 % 2
                    kk = n4 % 2
                    n4 += 1
                    for ck in range(16):
                        P.pe(lambda e, ck=ck, cc=cc, hf4=hf4, k4=k4, bank=bank: e.matmul(
                            psf(bank), lhsT=whog[k4][:, ck, cc * 128:(cc + 1) * 128], rhs=hTo4[:, 4 * hf4:4 * hf4 + 4, ck, :],
                            start=(ck == 0), stop=(ck == 15)),
                            reads=[b_whog[k4]] + b_hTo4[4 * hf4:4 * hf4 + 4], writes=[PSB[bank]])
                    P.act(lambda e, bank=bank, kk=kk: e.activation(out=sil4[kk][:], in_=psf(bank), func=AF.Silu),
                          writes=[PSB[bank], b_sil4[kk]])
                    P.dve(lambda e, kk=kk, chn=chn, hf4=hf4: e.tensor_tensor(out=yhT[:, chn, hf4 * 512:(hf4 + 1) * 512], in0=sil4[kk][:],
                                                                             in1=onT[:, chn, hf4 * 512:(hf4 + 1) * 512], op=ALU.mult),
                          reads=[b_sil4[kk]] + b_onT[4 * hf4:4 * hf4 + 4], writes=[b_yhT])
        P.barrier()
        mT_t = nc.alloc_sbuf_tensor_at("mergedT", [128, 16, 1024], BF16, offset=R_M)
        b_mT = bufs(NOWN, "mT")
        n4 = 0
        for g4 in range(8):
            k4 = g4 % 2
            W = wg4[k4]
            BW = b_wg4[k4]
            P.dma("pool", W["ga"][:], w_in[:, O_GA + g4 * 256:O_GA + (g4 + 1) * 256].rearrange("(c p) f -> p c f", p=128), writes=[BW["ga"]])
            P.dma("pool", W["gh"][:], w_in[:, O_GH + g4 * 256:O_GH + (g4 + 1) * 256].rearrange("(c p) f -> p c f", p=128), writes=[BW["gh"]])
            P.dma("pool", W["au"][:], w_au[:, g4 * 256:(g4 + 1) * 256].rearrange("(c p) f -> p c f", p=128), writes=[BW["au"]])
            P.dma("pool", W["hu"][:], w_hu[:, g4 * 256:(g4 + 1) * 256].rearrange("(c p) f -> p c f", p=128), writes=[BW["hu"]])
            for cc in range(2):
                Dc = 2 * g4 + cc
                for hf4 in range(2):
                    kk = n4 % 2
                    n4 += 1
                    bk = [4 * kk + t for t in range(4)]
                    for ck in range(16):
                        P.pe(lambda e, ck=ck, cc=cc, hf4=hf4, W=W, bank=bk[0]: e.matmul(
                            psf(bank), lhsT=W["ga"][:, ck, cc * 128:(cc + 1) * 128], rhs=hTo4[:, 4 * hf4:4 * hf4 + 4, ck, :],
                            start=(ck == 0), stop=(ck == 15)),
                            reads=[BW["ga"]] + b_hTo4[4 * hf4:4 * hf4 + 4], writes=[PSB[bk[0]]])
                    for ck in range(16):
                        P.pe(lambda e, ck=ck, cc=cc, hf4=hf4, W=W, bank=bk[1]: e.matmul(
                            psf(bank), lhsT=W["gh"][:, ck, cc * 128:(cc + 1) * 128], rhs=hTo4[:, 4 * hf4:4 * hf4 + 4, ck, :],
                            start=(ck == 0), stop=(ck == 15)),
                            reads=[BW["gh"]] + b_hTo4[4 * hf4:4 * hf4 + 4], writes=[PSB[bk[1]]])
                    for fc in range(8):
                        P.pe(lambda e, fc=fc, cc=cc, hf4=hf4, W=W, bank=bk[2]: e.matmul(
                            psf(bank), lhsT=W["au"][:, fc, cc * 128:(cc + 1) * 128], rhs=yaT[:, fc, hf4 * 512:(hf4 + 1) * 512],
                            start=(fc == 0), stop=(fc == 7)),
                            reads=[BW["au"]] + b_yaT[4 * hf4:4 * hf4 + 4], writes=[PSB[bk[2]]])
                    for fc in range(8):
                        P.pe(lambda e, fc=fc, cc=cc, hf4=hf4, W=W, bank=bk[3]: e.matmul(
                            psf(bank), lhsT=W["hu"][:, fc, cc * 128:(cc + 1) * 128], rhs=yhT[:, fc, hf4 * 512:(hf4 + 1) * 512],
                            start=(fc == 0), stop=(fc == 7)),
                            reads=[BW["hu"], b_yhT], writes=[PSB[bk[3]]])
                    P.act(lambda e, kk=kk, bank=bk[0]: e.activation(out=sg4[2 * kk][:], in_=psf(bank), func=AF.Sigmoid),
                          writes=[PSB[bk[0]], b_sg4[2 * kk]])
                    P.act(lambda e, kk=kk, bank=bk[1]: e.activation(out=sg4[2 * kk + 1][:], in_=psf(bank), func=AF.Sigmoid),
                          writes=[PSB[bk[1]], b_sg4[2 * kk + 1]])
                    P.dve(lambda e, kk=kk, bank=bk[2]: e.tensor_tensor(out=mm4[2 * kk][:], in0=psf(bank), in1=sg4[2 * kk][:], op=ALU.mult),
                          reads=[b_sg4[2 * kk]], writes=[PSB[bk[2]], b_mm4[2 * kk]])
                    P.dve(lambda e, kk=kk, bank=bk[3]: e.tensor_tensor(out=mm4[2 * kk + 1][:], in0=psf(bank), in1=sg4[2 * kk + 1][:], op=ALU.mult),
                          reads=[b_sg4[2 * kk + 1]], writes=[PSB[bk[3]], b_mm4[2 * kk + 1]])
                    P.dve(lambda e, kk=kk, Dc=Dc, hf4=hf4: e.tensor_tensor(out=mT_t[:, Dc, hf4 * 512:(hf4 + 1) * 512], in0=mm4[2 * kk][:],
                                                                           in1=mm4[2 * kk + 1][:], op=ALU.add),
                          reads=[b_mm4[2 * kk], b_mm4[2 * kk + 1]], writes=b_mT[4 * hf4:4 * hf4 + 4])
        P.barrier()
        A.off = R_L
        h2T = A.alloc([128, NOWN, 16, 128], BF16, "h2T")
        b_h2T = bufs(NOWN, "h2T")
        G1b = A.alloc([128, D], F32, "G1b")
        b_G1b = Buf()
        assert A.off <= R_M
        A.off = R_H
        wo_t = A.alloc([128, 16, D], BF16, "wo_t")
        b_wo = bufs(4, "wo")
        for n in range(4):
            P.dma("pool", wo_t[:, :, n * 512:(n + 1) * 512], w_out[:, n * 512:(n + 1) * 512].rearrange("(c p) f -> p c f", p=128),
                  writes=[b_wo[n]])
        P.dma("sp", G1b[:], modscr[0], writes=[b_G1b])
        A2bc = A.alloc([128, D], F32, "A2bc")
        B2bc = A.alloc([128, D], F32, "B2bc")
        b_A2, b_B2 = Buf(), Buf()
        P.dma("sp", A2bc[:], modscr[1], writes=[b_A2])
        P.dma("sp", B2bc[:], modscr[2], writes=[b_B2])
        xo_t = [A.alloc([128, D], F32, "xo_t0")] * 2
        b_xo = [Buf("xo")] * 2
        x1_t = [A.alloc([128, D], F32, f"x1_t{i}") for i in range(2)]
        b_x1 = bufs(2)
        hb4 = [A.alloc([128, D], BF16, f"hb4_{i}") for i in range(2)]
        b_hb4 = bufs(2)
        NS4c = dict(xn=A.alloc([128, D], F32, "xn4"), b_xn=Buf(), st1=A.alloc([128, 8], F32, "st14"), b_ssq=Buf(), b_rstd=Buf())
        NS4 = [dict(junk=hb4[k_], b_junk=b_hb4[k_], **NS4c) for k_ in range(2)]
        wr_t = A.alloc([128, 16, 36], BF16, "wr_t")
        b_wr = Buf()
        P.dma("pool", wr_t[:], w_r.rearrange("(c p) f -> p c f", p=128), writes=[b_wr])
        brbc = A.alloc([128, 36], F32, "brbc")
        b_brbc = Buf()
        tmp_row4 = A.alloc([1, 512], F32, "tmprow4")
        b_tmprow4 = Buf()
        bcast_rows(brbc[:], b_brbc, b_r, 36, tmp_row4, b_tmprow4, 7)
        comb = nc.alloc_sbuf_tensor_at("comb", [128, NOWN, 32], F32, offset=COMB_OFF)
        b_comb = bufs(NOWN, "comb")
        rt4 = A.alloc([128, 928], F32, "rt4")
        b_rt4 = Buf()
        lgall = A.alloc([128, NOWN, 36], F32, "lgall")
        b_lgall = Buf()
        n4 = 0
        for i in range(NOWN):
            s4 = i % 2
            P.dma("sp", xo_t[s4][:], xo[i * 128:(i + 1) * 128, :], writes=[b_xo[s4]])
            for n in range(4):
                bank = n4 % 2
                kk = n4 % 2
                n4 += 1
                for Dc in range(16):
                    P.pe(lambda e, Dc=Dc, i=i, n=n, bank=bank: e.matmul(psf(bank), lhsT=mT_t[:, Dc, i * 128:(i + 1) * 128],
                                                                         rhs=wo_t[:, Dc, n * 512:(n + 1) * 512], start=(Dc == 0), stop=(Dc == 15)),
                         reads=[b_mT[i], b_wo[n]], writes=[PSB[bank]])
                P.dve(lambda e, n=n, bank=bank, s4=s4: e.tensor_tensor(out=x1_t[s4][:, n * 512:(n + 1) * 512], in0=psf(bank),
                                                                       in1=G1b[:, n * 512:(n + 1) * 512], op=ALU.mult),
                      reads=[b_G1b], writes=[PSB[bank], b_x1[s4]])
                P.pool(lambda e, n=n, s4=s4: e.tensor_tensor(out=x1_t[s4][:, n * 512:(n + 1) * 512], in0=x1_t[s4][:, n * 512:(n + 1) * 512],
                                                             in1=xo_t[s4][:, n * 512:(n + 1) * 512], op=ALU.add),
                       reads=[b_x1[s4], b_xo[s4]], writes=[b_x1[s4]])
            P.dma("pool", x1s[i], x1_t[s4][:], reads=[b_x1[s4]])
            norm_tile(NS4[s4], x1_t[s4][:], b_x1[s4], A2bc[:], b_A2, B2bc[:], b_B2, hb4[s4][:], b_hb4[s4])
            transpose_tile(hb4[s4][:], b_hb4[s4], h2T[:, i, :, :].rearrange("p c t -> p (c t)"), b_h2T[i], (2, 3))
            for ck in range(16):
                P.pe(lambda e, ck=ck, i=i: e.matmul(psf(6)[:, 0:36], lhsT=h2T[:, i, ck, :], rhs=wr_t[:, ck, :], start=(ck == 0), stop=(ck == 15)),
                     reads=[b_h2T[i], b_wr], writes=[PSB[6]])
            P.dve(lambda e, i=i: e.tensor_tensor(out=lgall[:, i, :], in0=psf(6)[:, 0:36], in1=brbc[:], op=ALU.add),
                  reads=[b_brbc], writes=[PSB[6], b_lgall])
        T8 = NOWN
        gl = lgall[:, :, 0:4]
        el = lgall[:, :, 4:36]
        rB = lambda c0, n: rt4[:, c0:c0 + n]

        def R3(c0, a, b_):
            return rt4[:, c0:c0 + a * b_].rearrange("p (a b) -> p a b", a=a)
        gmax, gsum, pg, m1, m2, w1, w2 = rB(0, 8), rB(8, 8), rB(16, 8), rB(24, 8), rB(32, 8), rB(40, 8), rB(48, 8)
        gd, ge, pen = R3(64, 8, 4), R3(96, 8, 4), R3(128, 8, 4)
        elm, is1, is2 = R3(160, 8, 32), R3(416, 8, 32), R3(672, 8, 32)
        D1 = lambda fn, **kw: P.dve(fn, reads=[b_rt4, b_lgall], writes=[b_rt4])
        D1(lambda e: e.tensor_reduce(out=gmax, in_=gl, axis=AX.X, op=ALU.max))
        D1(lambda e: e.tensor_tensor(out=gd, in0=gl, in1=gmax.unsqueeze(2).to_broadcast([128, T8, 4]), op=ALU.subtract))
        P.act(lambda e: e.activation(out=ge, in_=gd, func=AF.Exp), reads=[b_rt4], writes=[b_rt4])
        D1(lambda e: e.tensor_reduce(out=gsum, in_=ge, axis=AX.X, op=ALU.add))
        D1(lambda e: e.reciprocal(out=pg, in_=gsum))
        D1(lambda e: e.tensor_scalar(out=pen, in0=gd, scalar1=0.0, scalar2=-NEG, op0=ALU.is_lt, op1=ALU.mult))
        D1(lambda e: e.tensor_tensor(out=elm.rearrange("p t (g x) -> p t g x", g=4), in0=el.rearrange("p t (g x) -> p t g x", g=4),
                                     in1=pen.unsqueeze(3).to_broadcast([128, T8, 4, 8]), op=ALU.subtract))
        D1(lambda e: e.tensor_reduce(out=m1, in_=elm, axis=AX.X, op=ALU.max))
        D1(lambda e: e.tensor_tensor(out=is1, in0=elm, in1=m1.unsqueeze(2).to_broadcast([128, T8, 32]), op=ALU.is_equal))
        D1(lambda e: e.scalar_tensor_tensor(out=elm, in0=is1, scalar=NEG, in1=elm, op0=ALU.mult, op1=ALU.add))
        D1(lambda e: e.tensor_reduce(out=m2, in_=elm, axis=AX.X, op=ALU.max))
        D1(lambda e: e.tensor_tensor(out=is2, in0=elm, in1=m2.unsqueeze(2).to_broadcast([128, T8, 32]), op=ALU.is_equal))
        D1(lambda e: e.tensor_tensor(out=w2, in0=m2, in1=m1, op=ALU.subtract))
        P.act(lambda e: e.activation(out=w2, in_=w2, func=AF.Exp), reads=[b_rt4], writes=[b_rt4])
        D1(lambda e: e.tensor_scalar(out=w2, in0=w2, scalar1=1.0, scalar2=None, op0=ALU.add))
        D1(lambda e: e.reciprocal(out=w1, in_=w2))
        D1(lambda e: e.tensor_scalar(out=w2, in0=w1, scalar1=-1.0, scalar2=1.0, op0=ALU.mult, op1=ALU.add))
        D1(lambda e: e.tensor_tensor(out=w1, in0=w1, in1=pg, op=ALU.mult))
        D1(lambda e: e.tensor_tensor(out=w2, in0=w2, in1=pg, op=ALU.mult))
        D1(lambda e: e.tensor_tensor(out=is1, in0=is1, in1=w1.unsqueeze(2).to_broadcast([128, T8, 32]), op=ALU.mult))
        D1(lambda e: e.tensor_tensor(out=is2, in0=is2, in1=w2.unsqueeze(2).to_broadcast([128, T8, 32]), op=ALU.mult))
        P.dve(lambda e: e.tensor_tensor(out=comb[:], in0=is1, in1=is2, op=ALU.add), reads=[b_rt4], writes=b_comb)

    if dbg and upto == 5:
        t = dbg_tensor("x1", [NOWN, 128, D])
        final_ops.append(P.dma("sp", t, x1s, reads=[]))
        t = dbg_tensor("h2T", [128, NOWN, 16, 128], BF16)
        final_ops.append(P.dma("sp", t, h2T[:], reads=b_h2T))
        t = dbg_tensor("comb", [128, NOWN, 32])
        final_ops.append(P.dma("sp", t, comb[:], reads=b_comb))
        t = dbg_tensor("mT", [128, 16, 1024], BF16)
        final_ops.append(P.dma("sp", t, mT_t[:], reads=b_mT))

    P.barrier()
    if upto >= 6:
        A.off = R_L + 32768
        G2bc = A.alloc([128, D], F32, "G2bc")
        b_G2 = Buf()
        P.dma("sp", G2bc[:], modscr[3], writes=[b_G2])
        A.off = R_M
        Y = A.alloc([128, NOWN, D], F32, "Y")
        b_Y = [[Buf() for n in range(4)] for i in range(NOWN)]
        m_p6 = A.mark()
        ring = [A.alloc([128, 8192], BF16, f"ring{i}") for i in range(5)]
        b_ring = bufs(5, "ring")
        actT = A.alloc([128, 4, 1024], BF16, "actT")
        b_act = bufs(2, "act")
        sa6 = [A.alloc([128, 512], F32, f"sa6_{i}") for i in range(2)]
        b_sa6 = bufs(2)
        import os
        NEXP = int(os.environ.get("MK_NEXP", 32))
        n6 = 0
        nd6 = 0
        for ex in range(NEXP):
            sl = [(3 * ex + t) % 5 for t in range(3)]
            Wg = ring[sl[0]][:].rearrange("p (c f) -> p c f", c=16)
            Wu = ring[sl[1]][:].rearrange("p (c f) -> p c f", c=16)
            Wd = ring[sl[2]][:].rearrange("p (c d) -> p c d", c=4)
            P.dma("pool", Wg, w_eg[ex].rearrange("(c p) f -> p c f", p=128), writes=[b_ring[sl[0]]])
            P.dma("pool", Wu, w_eu[ex].rearrange("(c p) f -> p c f", p=128), writes=[b_ring[sl[1]]])
            P.dma("pool", Wd, w_ed[ex].rearrange("(c p) d -> p c d", p=128), writes=[b_ring[sl[2]]])
            for hf6 in range(2):
                for fc in range(4):
                    kk = n6 % 2
                    n6 += 1
                    ba, bu = kk, 2 + kk
                    for ck in range(16):
                        P.pe(lambda e, ck=ck, fc=fc, hf6=hf6, Wg=Wg, ba=ba: e.matmul(
                            psf(ba), lhsT=Wg[:, ck, fc * 128:(fc + 1) * 128], rhs=h2T[:, 4 * hf6:4 * hf6 + 4, ck, :],
                            start=(ck == 0), stop=(ck == 15)),
                            reads=[b_ring[sl[0]]] + b_h2T[4 * hf6:4 * hf6 + 4], writes=[PSB[ba]])
                    for ck in range(16):
                        P.pe(lambda e, ck=ck, fc=fc, hf6=hf6, Wu=Wu, bu=bu: e.matmul(
                            psf(bu), lhsT=Wu[:, ck, fc * 128:(fc + 1) * 128], rhs=h2T[:, 4 * hf6:4 * hf6 + 4, ck, :],
                            start=(ck == 0), stop=(ck == 15)),
                            reads=[b_ring[sl[1]]] + b_h2T[4 * hf6:4 * hf6 + 4], writes=[PSB[bu]])
                    P.act(lambda e, ba=ba, kk=kk: e.activation(out=sa6[kk][:], in_=psf(ba), func=AF.Silu), writes=[PSB[ba], b_sa6[kk]])
                    P.dve(lambda e, bu=bu, kk=kk, fc=fc, hf6=hf6: e.tensor_tensor(out=actT[:, fc, hf6 * 512:(hf6 + 1) * 512], in0=psf(bu),
                                                                                  in1=sa6[kk][:], op=ALU.mult),
                          reads=[b_sa6[kk]], writes=[PSB[bu], b_act[hf6]])
            for i in range(NOWN):
                for n in range(4):
                    bd = 4 + nd6 % 4
                    nd6 += 1
                    for fc in range(4):
                        P.pe(lambda e, fc=fc, i=i, n=n, Wd=Wd, bd=bd: e.matmul(psf(bd), lhsT=actT[:, fc, i * 128:(i + 1) * 128],
                                                                                rhs=Wd[:, fc, n * 512:(n + 1) * 512], start=(fc == 0), stop=(fc == 3)),
                             reads=[b_act[i // 4], b_ring[sl[2]]], writes=[PSB[bd]])
                    if ex == 0:
                        P.dve(lambda e, i=i, n=n, bd=bd, ex=ex: e.tensor_scalar(out=Y[:, i, n * 512:(n + 1) * 512], in0=psf(bd),
                                                                                scalar1=comb[:, i, ex:ex + 1], scalar2=None, op0=ALU.mult),
                              reads=[b_comb[i]], writes=[PSB[bd], b_Y[i][n]])
                    else:
                        P.dve(lambda e, i=i, n=n, bd=bd, ex=ex: e.scalar_tensor_tensor(out=Y[:, i, n * 512:(n + 1) * 512], in0=psf(bd),
                                                                                       scalar=comb[:, i, ex:ex + 1], in1=Y[:, i, n * 512:(n + 1) * 512],
                                                                                       op0=ALU.mult, op1=ALU.add),
                              reads=[b_comb[i], b_Y[i][n]], writes=[PSB[bd], b_Y[i][n]])
        A.release(m_p6)
        P.barrier()
        gfbc = A.alloc([128, D], F32, "gfbc")
        b_gf = Buf()
        tmp_row6 = A.alloc([1, 512], F32, "tmprow6")
        b_tmprow6 = Buf()
        bcast_rows(gfbc[:], b_gf, grows[2:3, :], D, tmp_row6, b_tmprow6, 0)
        x1l = [A.alloc([128, D], F32, f"x1l{i}") for i in range(2)]
        b_x1l = bufs(2)
        of6 = [A.alloc([128, D], F32, f"of6_{i}") for i in range(2)]
        b_of6 = bufs(2)
        junk6 = A.alloc([128, D], BF16, "junk6")
        b_junk6 = Buf()
        st6 = A.alloc([128, 8], F32, "st6")
        b_st6 = Buf()
        for i in range(NOWN):
            k6 = i % 2
            P.dma("sp", x1l[k6][:], x1s[i], writes=[b_x1l[k6]])
            P.dve(lambda e, i=i: e.tensor_tensor(out=Y[:, i, :], in0=Y[:, i, :], in1=G2bc[:], op=ALU.mult),
                  reads=[b_G2] + b_Y[i], writes=b_Y[i])
            P.pool(lambda e, i=i, k6=k6: e.tensor_tensor(out=x1l[k6][:], in0=x1l[k6][:], in1=Y[:, i, :], op=ALU.add),
                   reads=[b_x1l[k6]] + b_Y[i], writes=[b_x1l[k6]])
            P.dve(lambda e, k6=k6: e.scalar_tensor_tensor(out=junk6[:], in0=x1l[k6][:], scalar=1.0, in1=x1l[k6][:], op0=ALU.mult, op1=ALU.mult,
                                                          accum_out=st6[:, 0:1]),
                  reads=[b_x1l[k6]], writes=[b_junk6, b_st6])
            P.act(lambda e: e.activation(out=st6[:, 1:2], in_=st6[:, 0:1], func=AF.Sqrt, scale=1.0 / D, bias=EPS), reads=[b_st6], writes=[b_st6])
            P.dve(lambda e: e.reciprocal(out=st6[:, 2:3], in_=st6[:, 1:2]), reads=[b_st6], writes=[b_st6])
            P.dve(lambda e, k6=k6: e.scalar_tensor_tensor(out=of6[k6][:], in0=x1l[k6][:], scalar=st6[:, 2:3], in1=gfbc[:], op0=ALU.mult, op1=ALU.mult),
                  reads=[b_x1l[k6], b_st6, b_gf], writes=[b_of6[k6]])
            final_ops.append(P.dma("act", out_d[i * 128:(i + 1) * 128, :], of6[k6][:], reads=[b_of6[k6]]))

    if dbg and upto == 2:
        t = dbg_tensor("KT", [128, 2, NU * 128], BF16)
        final_ops.append(P.dma("sp", t, KT[:], reads=b_KT))
        t = dbg_tensor("ikT", [128, NU * 128], BF16)
        final_ops.append(P.dma("sp", t, ikT[:], reads=b_ikT))
        t = dbg_tensor("Vx", [128, NU, 2, 132], BF16)
        final_ops.append(P.dma("sp", t, Vx[:], reads=b_Vx))
        t = dbg_tensor("A1", [128, D])
        final_ops.append(P.dma("sp", t, A1[:], reads=[b_A1]))
        t = dbg_tensor("B1", [128, D])
        final_ops.append(P.dma("sp", t, B1[:], reads=[b_B1]))

    P.emit(final_wait_ops=final_ops)
    return nc, dbg_out


def _consts():
    c = np.zeros((128, 640), np.float32)
    c[:, 0:128] = np.eye(128, dtype=np.float32)
    q = np.arange(128)[:, None]
    k = np.arange(128)[None, :]
    c[:, 128:256] = np.where(k <= q, 0.0, NEG)
    s = np.arange(128)[:, None]
    t = np.arange(128)[None, :]
    c[:, 256:384] = ((t >= s) & ((t // 64) == (s // 64))).astype(np.float32)
    p = np.arange(128)
    c[:, 384] = np.power(10000.0, -(2.0 * (p % 64)) / 128.0)
    c[:, 385] = np.power(10000.0, -(2.0 * (p % 32)) / 64.0)
    c[:, 386] = np.where((p % 128) < 64, -1.0, 1.0)
    c[:, 387] = np.where((p % 64) < 32, -1.0, 1.0)
    c[:, 388:516] = 1.0
    for kk in range(NBIS):
        c[:, 516 + kk] = 2.0 ** (-kk)
    c[:, 540:604] = 1.0
    c[:, 540] = 0.0
    return c


def _swap_cols(w, nheads, hd):
    half = hd // 2
    w3 = w.reshape(w.shape[0], nheads, hd)
    return np.concatenate([w3[:, :, half:], w3[:, :, :half]], axis=2).reshape(w.shape[0], nheads * hd)


def prep_inputs(core, x, c, positions, w_ada, b_ada, g_norm1, w_in, g_head, hg_lower_bounds, w_attn_up, w_hgrn_up,
                w_out, g_norm2, w_router_group, b_router_group, w_router_expert, b_router_expert,
                w_exp_gate, w_exp_up, w_exp_down, g_final, shared):
    b, j = core // 4, core % 4
    pad = 3 - j
    xa = np.zeros((NU * 128, D), np.float32)
    nreal = (NU - pad) * 128
    xa[pad * 128:] = x[b][:nreal]
    own_rows = np.concatenate([np.arange(128 * (4 * i + j), 128 * (4 * i + j) + 128) for i in range(8)])
    xo = np.ascontiguousarray(x[b][own_rows])
    pos_pad = np.zeros((NU * 128,), np.int32)
    pos_pad[pad * 128:] = positions[b][:nreal]
    posr = np.ascontiguousarray(np.broadcast_to(pos_pad[None, :], (128, NU * 128)))
    poso = np.ascontiguousarray(np.broadcast_to(positions[b][own_rows][None, :], (128, 1024)))
    valid = np.zeros((NU * 128,), np.float32)
    valid[pad * 128:] = 1.0
    vmask = np.ascontiguousarray(valid.reshape(NU, 128).T)
    smask = np.ascontiguousarray(np.broadcast_to(np.where(valid[:512] > 0, 0.0, NEG)[None, :], (128, 512))).astype(np.float32)
    ccol = np.ascontiguousarray(c[b].reshape(16, 128).T)
    m = dict(xa=xa, xo=xo, posr=posr, poso=poso, vmask=vmask, smask=smask, ccol=ccol)
    m.update(shared)
    return m


def prep_shared(w_ada, b_ada, g_norm1, w_in, g_head, hg_lower_bounds, w_attn_up, w_hgrn_up, w_out, g_norm2,
                w_router_group, b_router_group, w_router_expert, b_router_expert, w_exp_gate, w_exp_up, w_exp_down,
                g_final):
    wi = w_in[0]
    ak = wi[:, O_AK:O_AK + 256]
    ik = wi[:, O_IK:O_IK + 64]
    ak_sw = _swap_cols(ak, 2, 128)
    ik_sw = _swap_cols(ik, 1, 64)
    w1a = np.ascontiguousarray(np.concatenate([ak, ak_sw, ik, ik, ik_sw, ik_sw], axis=1))
    aq_sw = _swap_cols(wi[:, O_AQ:O_AQ + 1024], 8, 128)
    iq_sw = _swap_cols(wi[:, O_IQ:O_IQ + 512], 8, 64)
    w2s = np.ascontiguousarray(np.concatenate([aq_sw, iq_sw], axis=1))
    grows = np.zeros((4, D), np.float32)
    grows[0] = g_norm1[0]
    grows[1] = g_norm2[0]
    grows[2] = g_final
    grows[3, :1024] = g_head[0].reshape(-1)
    lbc = np.zeros((128, 16), np.float32)
    lbc[:, 0:8] = hg_lower_bounds[0].reshape(8, 128).T
    lbc[:, 8:16] = hg_lower_bounds[1].reshape(8, 128).T
    return dict(cst=_consts(), lbc=lbc, w_ada=np.ascontiguousarray(w_ada[0]), b_ada=np.ascontiguousarray(b_ada[0:1]),
                grows=grows, w_in=np.ascontiguousarray(wi), w1a=w1a, w2s=w2s,
                w_au=np.ascontiguousarray(w_attn_up[0]), w_hu=np.ascontiguousarray(w_hgrn_up[0]),
                w_out=np.ascontiguousarray(w_out[0]),
                w_r=np.ascontiguousarray(np.concatenate([w_router_group[0], w_router_expert[0]], axis=1)),
                b_r=np.ascontiguousarray(np.concatenate([b_router_group[0], b_router_expert[0]])[None, :]),
                w_eg=np.ascontiguousarray(w_exp_gate[0]), w_eu=np.ascontiguousarray(w_exp_up[0]),
                w_ed=np.ascontiguousarray(w_exp_down[0]))


def kernel(**inputs):
    inp = {k: np.asarray(v) for k, v in inputs.items()}
    x = inp["x"]
    shared = prep_shared(**{k: inp[k] for k in inp if k not in ("x", "c", "positions")})
    nc, _ = build()
    in_maps = [prep_inputs(core, shared=shared, **inp) for core in range(8)]
    res = run_bass_kernel_spmd(nc, in_maps, core_ids=list(range(8)))
    out = np.zeros(x.shape, np.float32)
    for core in range(8):
        b, j = core // 4, core % 4
        o = res.results[core]["out"]
        for i in range(8):
            g = 4 * i + j
            out[b, 128 * g:128 * g + 128] = o[128 * i:128 * i + 128]
    return out
```
